# Optimizing a Trainium2 kernel written in Bass

```python
import math
import jax
import jax.numpy as jnp
from jax import lax
import numpy as np


D_MODEL = 4096
BATCH = 4
SEQ = 4096
DEPTH = 1

MIX_WIDTH = D_MODEL
ATTN_WIDTH = MIX_WIDTH // 2
SSM_WIDTH = MIX_WIDTH - ATTN_WIDTH
ATTN_HEAD_DIM = 128
ATTN_HEADS = ATTN_WIDTH // (2 * ATTN_HEAD_DIM)
Q_BLOCK = 128
NUM_BUCKETS = 32
MAX_DISTANCE = 128
SSM_HEAD_DIM = 64
SSM_HEADS = SSM_WIDTH // SSM_HEAD_DIM
SSM_GROUPS = 8
SSM_STATE = 128
CONV_WIDTH = 5
CONV_CHANNELS = SSM_WIDTH + 2 * SSM_GROUPS * SSM_STATE
CHUNK = 128
IN_PROJ_WIDTH = 3 * ATTN_WIDTH + SSM_WIDTH + CONV_CHANNELS + 2 * SSM_HEADS
PROJ_SPLITS = (ATTN_WIDTH, 2 * ATTN_WIDTH, 3 * ATTN_WIDTH, 3 * ATTN_WIDTH + SSM_WIDTH,
               3 * ATTN_WIDTH + SSM_WIDTH + CONV_CHANNELS,
               3 * ATTN_WIDTH + SSM_WIDTH + CONV_CHANNELS + SSM_HEADS)
N_EXPERT_GROUPS = 4
EXPERTS_PER_GROUP = 8
N_EXPERTS = N_EXPERT_GROUPS * EXPERTS_PER_GROUP
TOP_K = 2
D_FF = D_MODEL // 4
EXPERT_BLOCK = 128
EPS = 1e-6

kernel_name = 'hymba_diffattn_ssd_hmoe_encoder'


def lambda_init_at(layer):
    return 0.8 - 0.6 * math.exp(-0.3 * layer)


def rmsnorm(x, w):
    xf = x.astype(jnp.float32)
    y = xf * lax.rsqrt(jnp.mean(xf * xf, axis=-1, keepdims=True) + EPS)
    return (y * w.astype(jnp.float32)).astype(x.dtype)


def t5_bucket(rel):
    half = NUM_BUCKETS // 2
    max_exact = half // 2
    n = jnp.abs(rel)
    large = max_exact + (jnp.log(jnp.maximum(n, 1).astype(jnp.float32) / max_exact)
                         / math.log(MAX_DISTANCE / max_exact) * (half - max_exact)).astype(jnp.int32)
    large = jnp.minimum(large, half - 1)
    return jnp.where(rel > 0, half, 0) + jnp.where(n < max_exact, n, large)


def diff_attention(q, k, v, rel_bias, lam, subln_w, lambda_init):
    b, s, h, _, dh = q.shape
    nqb = s // Q_BLOCK
    q_blocks = jnp.moveaxis(q.reshape(b, nqb, Q_BLOCK, h, 2, dh), 1, 0)
    k_pos = jnp.arange(s, dtype=jnp.int32)
    scale = dh ** -0.5

    def one_block(args):
        q_blk, start = args
        logits = jnp.einsum('bqhmd,bkhmd->bhmqk', q_blk, k,
                            preferred_element_type=jnp.float32) * scale
        q_pos = start + jnp.arange(Q_BLOCK, dtype=jnp.int32)
        bias = rel_bias[t5_bucket(k_pos[None, :] - q_pos[:, None])].astype(jnp.float32)
        logits = logits + jnp.transpose(bias, (2, 0, 1))[None, :, None]
        p = jax.nn.softmax(logits, axis=-1)
        a = p[:, :, 0] - lam * p[:, :, 1]
        return jnp.einsum('bhqk,bkhe->bqhe', a.astype(v.dtype), v)

    starts = jnp.arange(nqb, dtype=jnp.int32) * Q_BLOCK
    out = lax.map(one_block, (q_blocks, starts))
    out = jnp.moveaxis(out, 0, 1).reshape(b, s, h, 2 * dh)
    out = rmsnorm(out, subln_w) * (1.0 - lambda_init)
    return out.reshape(b, s, h * 2 * dh)


def centred_dwconv(u, w, bias):
    y = lax.conv_general_dilated(u, w[:, None, :].astype(u.dtype), window_strides=(1,),
                                 padding=[(CONV_WIDTH // 2, CONV_WIDTH // 2)],
                                 dimension_numbers=('NWC', 'WIO', 'NWC'),
                                 feature_group_count=u.shape[-1])
    return y + bias.astype(u.dtype)


def ssd_chunked(xs, a, bm, cm):
    b, s, h, p = xs.shape
    g, n = bm.shape[2], bm.shape[3]
    r = h // g
    nc = s // CHUNK
    xc = xs.reshape(b, nc, CHUNK, g, r, p)
    ac = jnp.transpose(a.reshape(b, nc, CHUNK, g, r), (0, 3, 4, 1, 2))
    bc = bm.reshape(b, nc, CHUNK, g, n)
    cc = cm.reshape(b, nc, CHUNK, g, n)
    a_cs = jnp.cumsum(ac, axis=-1)
    lower = jnp.tril(jnp.ones((CHUNK, CHUNK), dtype=bool))
    decay_in = jnp.exp(jnp.where(lower, a_cs[..., :, None] - a_cs[..., None, :], -jnp.inf))
    cb = jnp.einsum('bclgn,bcsgn->bcgls', cc, bc)
    y_diag = jnp.einsum('bcgls,bgrcls,bcsgrp->bclgrp', cb, decay_in, xc)
    decay_to_end = jnp.exp(a_cs[..., -1:] - a_cs)
    states = jnp.einsum('bclgn,bgrcl,bclgrp->bcgrpn', bc, decay_to_end, xc)
    chunk_decay = jnp.exp(a_cs[..., -1])

    def carry_state(state, inp):
        s_c, d_c = inp
        return state * d_c[..., None, None] + s_c, state

    init = jnp.zeros((b, g, r, p, n), xs.dtype)
    _, prev = lax.scan(carry_state, init,
                       (jnp.moveaxis(states, 1, 0), jnp.moveaxis(chunk_decay, -1, 0)))
    y_off = jnp.einsum('bclgn,cbgrpn,bgrcl->bclgrp', cc, prev, jnp.exp(a_cs))
    return (y_diag + y_off).reshape(b, s, h, p)


def ssd_direction(xs, bm, cm, dt_raw, dt_bias, a_log):
    dt = jax.nn.softplus(dt_raw.astype(jnp.float32) + dt_bias.astype(jnp.float32))
    a = -jnp.exp(a_log.astype(jnp.float32))
    return ssd_chunked(xs * dt[..., None], a * dt, bm, cm)


def flip_seq(t):
    return jnp.flip(t, axis=1)


def ssd_mixer(z, xbc, dt_f, dt_b, conv_w, conv_b, dt_bias_f, dt_bias_b, a_log_f, a_log_b, d_skip, norm_w):
    b, s, _ = xbc.shape
    xbc = jax.nn.silu(centred_dwconv(xbc, conv_w, conv_b)).astype(jnp.float32)
    gn = SSM_GROUPS * SSM_STATE
    xs = xbc[..., :SSM_WIDTH].reshape(b, s, SSM_HEADS, SSM_HEAD_DIM)
    bm = xbc[..., SSM_WIDTH:SSM_WIDTH + gn].reshape(b, s, SSM_GROUPS, SSM_STATE)
    cm = xbc[..., SSM_WIDTH + gn:].reshape(b, s, SSM_GROUPS, SSM_STATE)
    y_fwd = ssd_direction(xs, bm, cm, dt_f, dt_bias_f, a_log_f)
    y_bwd = flip_seq(ssd_direction(flip_seq(xs), flip_seq(bm), flip_seq(cm), flip_seq(dt_b), dt_bias_b, a_log_b))
    y = y_fwd + y_bwd + d_skip.astype(jnp.float32)[:, None] * xs
    gw = SSM_WIDTH // SSM_GROUPS
    y = y.reshape(b, s, SSM_GROUPS, gw) * jax.nn.silu(z.astype(jnp.float32)).reshape(b, s, SSM_GROUPS, gw)
    y = y * lax.rsqrt(jnp.mean(y * y, axis=-1, keepdims=True) + EPS)
    return (y.reshape(b, s, SSM_WIDTH) * norm_w.astype(jnp.float32)).astype(z.dtype)


def hierarchical_moe(h, w_group_router, b_group_router, w_expert_router, b_expert_router, w_gate, w_up, w_down):
    b, s, d = h.shape
    t = b * s
    hf = h.reshape(t, d)
    tok = jnp.arange(t)
    g_prob = jax.nn.softmax((hf @ w_group_router).astype(jnp.float32) + b_group_router.astype(jnp.float32), axis=-1)
    g_sel = jnp.argmax(g_prob, axis=-1)
    p_group = jnp.max(g_prob, axis=-1)
    e_logits = ((hf @ w_expert_router).astype(jnp.float32) + b_expert_router.astype(jnp.float32)).reshape(
        t, N_EXPERT_GROUPS, EXPERTS_PER_GROUP)
    e_prob = jax.nn.softmax(e_logits[tok, g_sel], axis=-1)
    top_p, top_i = lax.top_k(e_prob, TOP_K)
    gate = p_group[:, None] * top_p / jnp.sum(top_p, axis=-1, keepdims=True)
    expert = g_sel[:, None] * EXPERTS_PER_GROUP + top_i
    n = t * TOP_K
    flat_e = expert.reshape(n).astype(jnp.int32)
    order = jnp.argsort(flat_e)
    se = flat_e[order]
    stok = (order // TOP_K).astype(jnp.int32)
    sgate = gate.reshape(n)[order]
    counts = jnp.bincount(flat_e, length=N_EXPERTS)
    starts = jnp.cumsum(counts) - counts
    padded = (counts + EXPERT_BLOCK - 1) // EXPERT_BLOCK * EXPERT_BLOCK
    padded_end = jnp.cumsum(padded)
    dest = padded_end[se] - padded[se] + jnp.arange(n) - starts[se]
    n_blocks = -(-n // EXPERT_BLOCK) + N_EXPERTS
    rows = n_blocks * EXPERT_BLOCK
    row_tok = jnp.zeros((rows,), jnp.int32).at[dest].set(stok)
    row_gate = jnp.zeros((rows,), jnp.float32).at[dest].set(sgate)
    block_expert = jnp.minimum(
        jnp.searchsorted(padded_end, jnp.arange(n_blocks) * EXPERT_BLOCK, side='right'), N_EXPERTS - 1)

    def expert_block(args):
        e, toks, gts = args
        xb = hf[toks]
        act = jax.nn.silu(xb @ w_gate[e]) * (xb @ w_up[e])
        return (act @ w_down[e]) * gts[:, None].astype(hf.dtype)

    y = lax.map(expert_block, (block_expert, row_tok.reshape(n_blocks, EXPERT_BLOCK),
                               row_gate.reshape(n_blocks, EXPERT_BLOCK)))
    out = jnp.zeros((t, d), hf.dtype).at[row_tok].add(y.reshape(rows, d))
    return out.reshape(b, s, d)


def setup_inputs(seed: int = 0) -> dict:
    key = jax.random.key(seed)
    ks = jax.random.split(key, 32)
    f32 = jnp.float32

    def nrm(k, shape, scale):
        return jax.random.normal(k, shape, f32) * scale

    L = DEPTH
    u = jax.random.uniform(ks[10], (L, SSM_HEADS), f32)
    dt0 = jnp.exp(u * (math.log(0.1) - math.log(0.001)) + math.log(0.001))
    u2 = jax.random.uniform(ks[11], (L, SSM_HEADS), f32)
    dt1 = jnp.exp(u2 * (math.log(0.1) - math.log(0.001)) + math.log(0.001))
    return {
        'x': nrm(ks[0], (BATCH, SEQ, D_MODEL), 1.0),
        'rel_bias': nrm(ks[1], (NUM_BUCKETS, ATTN_HEADS), 0.5),
        'norm1_w': 1.0 + nrm(ks[2], (L, D_MODEL), 0.02),
        'w_in': nrm(ks[3], (L, D_MODEL, IN_PROJ_WIDTH), D_MODEL ** -0.5),
        'lambda_q1': nrm(ks[4], (L, ATTN_HEAD_DIM), 0.1),
        'lambda_k1': nrm(ks[5], (L, ATTN_HEAD_DIM), 0.1),
        'lambda_q2': nrm(ks[6], (L, ATTN_HEAD_DIM), 0.1),
        'lambda_k2': nrm(ks[7], (L, ATTN_HEAD_DIM), 0.1),
        'subln_w': 1.0 + nrm(ks[8], (L, 2 * ATTN_HEAD_DIM), 0.02),
        'conv_w': nrm(ks[9], (L, CONV_WIDTH, CONV_CHANNELS), CONV_WIDTH ** -0.5),
        'conv_b': nrm(ks[12], (L, CONV_CHANNELS), 0.01),
        'dt_bias_f': dt0 + jnp.log(-jnp.expm1(-dt0)),
        'dt_bias_b': dt1 + jnp.log(-jnp.expm1(-dt1)),
        'a_log_f': jnp.log(jax.random.uniform(ks[13], (L, SSM_HEADS), f32, 1.0, 16.0)),
        'a_log_b': jnp.log(jax.random.uniform(ks[14], (L, SSM_HEADS), f32, 1.0, 16.0)),
        'd_skip': 1.0 + nrm(ks[15], (L, SSM_HEADS), 0.02),
        'ssm_norm_w': 1.0 + nrm(ks[16], (L, SSM_WIDTH), 0.02),
        'w_out': nrm(ks[17], (L, MIX_WIDTH, D_MODEL), MIX_WIDTH ** -0.5),
        'norm2_w': 1.0 + nrm(ks[18], (L, D_MODEL), 0.02),
        'w_group_router': nrm(ks[19], (L, D_MODEL, N_EXPERT_GROUPS), D_MODEL ** -0.5),
        'b_group_router': nrm(ks[20], (L, N_EXPERT_GROUPS), 0.01),
        'w_expert_router': nrm(ks[21], (L, D_MODEL, N_EXPERTS), D_MODEL ** -0.5),
        'b_expert_router': nrm(ks[22], (L, N_EXPERTS), 0.01),
        'w_gate': nrm(ks[23], (L, N_EXPERTS, D_MODEL, D_FF), D_MODEL ** -0.5),
        'w_up': nrm(ks[24], (L, N_EXPERTS, D_MODEL, D_FF), D_MODEL ** -0.5),
        'w_down': nrm(ks[25], (L, N_EXPERTS, D_FF, D_MODEL), D_FF ** -0.5),
        'final_norm_w': 1.0 + nrm(ks[26], (D_MODEL,), 0.02),
    }


def reference(x, rel_bias, norm1_w, w_in, lambda_q1, lambda_k1, lambda_q2, lambda_k2, subln_w, conv_w, conv_b,
              dt_bias_f, dt_bias_b, a_log_f, a_log_b, d_skip, ssm_norm_w, w_out, norm2_w, w_group_router,
              b_group_router, w_expert_router, b_expert_router, w_gate, w_up, w_down, final_norm_w):
    b, s, _ = x.shape
    for layer in range(DEPTH):
        h = rmsnorm(x, norm1_w[layer])
        proj = h @ w_in[layer]
        q, k, v, z, xbc, dt_f, dt_b = jnp.split(proj, PROJ_SPLITS, axis=-1)
        lam_init = lambda_init_at(layer)
        lam = (jnp.exp(jnp.sum(lambda_q1[layer].astype(jnp.float32) * lambda_k1[layer].astype(jnp.float32)))
               - jnp.exp(jnp.sum(lambda_q2[layer].astype(jnp.float32) * lambda_k2[layer].astype(jnp.float32)))
               + lam_init)
        attn_out = diff_attention(q.reshape(b, s, ATTN_HEADS, 2, ATTN_HEAD_DIM),
                                  k.reshape(b, s, ATTN_HEADS, 2, ATTN_HEAD_DIM),
                                  v.reshape(b, s, ATTN_HEADS, 2 * ATTN_HEAD_DIM),
                                  rel_bias, lam, subln_w[layer], lam_init)
        ssm_out = ssd_mixer(z, xbc, dt_f, dt_b, conv_w[layer], conv_b[layer], dt_bias_f[layer], dt_bias_b[layer],
                            a_log_f[layer], a_log_b[layer], d_skip[layer], ssm_norm_w[layer])
        x = x + jnp.concatenate([attn_out, ssm_out], axis=-1) @ w_out[layer]
        x = x + hierarchical_moe(rmsnorm(x, norm2_w[layer]), w_group_router[layer], b_group_router[layer],
                                 w_expert_router[layer], b_expert_router[layer], w_gate[layer], w_up[layer],
                                 w_down[layer])
    return rmsnorm(x, final_norm_w)
```

```python
import math
from contextlib import ExitStack
import numpy as np
import concourse.bass as bass
import concourse.mybir as mybir
from concourse.bass_utils import run_bass_kernel_spmd

F32 = mybir.dt.float32
BF16 = mybir.dt.bfloat16
I32 = mybir.dt.int32
AF = mybir.ActivationFunctionType
ALU = mybir.AluOpType
AX = mybir.AxisListType

T = 4096
TO = 2048
D = 4096
NCH = 32
EPS = 1e-6
SCALE = 128 ** -0.5
LAM_INIT = 0.8 - 0.6 * math.exp(-0.3 * 0)
NE = 32
CAP = 512
NSLOT = NE * CAP
DFF = 1024
ENG = ['tensor', 'vector', 'scalar', 'gpsimd', 'sync']


class Sem:
    def __init__(self, h, name):
        self.h = h
        self.n = 0
        self.name = name


class Slot:
    def __init__(self, name=''):
        self.name = name
        self.wr = None
        self.rd = []


class Prog:
    def __init__(self, nc, stack):
        self.nc = nc
        self.stack = stack
        self.q = {e: [] for e in ENG}
        self.waited = {e: {} for e in ENG}
        self.sems = {}
        self.dma_evs = {e: [] for e in ENG}
        self.phase = 0
        self.engsem = {}
        self.new_phase_sems()

    def _raw_sem(self, name):
        if name not in self.sems:
            h = self.stack.enter_context(self.nc.semaphore(name))
            self.sems[name] = Sem(h, name)
        return self.sems[name]

    def sem(self, name):
        if not hasattr(self, 'pmap'):
            self.pmap = {}
        if name not in self.pmap:
            self.pmap[name] = self._raw_sem('dma%d' % len(self.pmap))
        return self.pmap[name]

    def new_phase_sems(self):
        self.pmap = {}
        self.semslot = {}
        if not self.engsem or max(s.n for s in self.engsem.values()) > 20000:
            self.gen = getattr(self, 'gen', -1) + 1
            self.engsem = {e: self._raw_sem('g%d_%s' % (self.gen, e)) for e in ENG}

    def op(self, eng, fn, reads=(), writes=(), dma=None, nsig=1, rw=()):
        waits = []
        writes = list(writes) + list(rw)
        for s in reads:
            if s.wr is not None:
                waits.append(s.wr)
        for s in writes:
            if s.wr is not None:
                waits.append(s.wr)
            waits.extend(s.rd)
        if dma is not None:
            sem, unit = dma, 16
            key_ = (list(writes) + list(reads))[0].name.split('/')[0] if (list(writes) + list(reads)) else None
            ss_ = self.__dict__.setdefault('semslot', {})
            if ss_.get(sem.name, key_) != key_:
                print("WARNING: DMA semaphore", sem.name, "shared by slots", ss_[sem.name], key_)
            ss_[sem.name] = key_
        else:
            sem, unit = self.engsem[eng], 1
        sem.n += unit * nsig
        assert sem.n < 60000, sem.name
        ev = (sem, sem.n, eng)
        w2 = []
        for (ws, wv, weng) in waits:
            if eng == 'tensor' and weng == 'tensor' and ws is self.engsem['tensor']:
                continue
            if self.waited[eng].get(ws.name, 0) >= wv:
                continue
            self.waited[eng][ws.name] = wv
            w2.append((ws, wv))
        self.q[eng].append((w2, fn, sem, unit, nsig))
        if dma is not None:
            self.dma_evs[eng].append(ev)
        for s in reads:
            s.rd.append(ev)
        for s in writes:
            s.wr = ev
            s.rd = []
        return ev

    def flush(self, name):
        for eng in ENG:
            best = {}
            for (s, v, _) in self.dma_evs[eng]:
                best[s.name] = (s, max(v, best.get(s.name, (s, 0))[1]))
            w = [(s, v) for (s, v) in best.values() if self.waited[eng].get(s.name, 0) < v]
            for (s, v) in w:
                self.waited[eng][s.name] = v
            if w:
                self.q[eng].append((w, None, None, 0, 0))
            self.dma_evs[eng] = []
        with self.nc.Block(name) as blk:
            for en in ENG:
                ops = self.q[en]
                if not ops:
                    continue

                def body(e, ops=ops):
                    for (waits, fn, sem, unit, nsig) in ops:
                        for (ws, wv) in waits:
                            e.wait_ge(ws.h, wv)
                        if fn is None:
                            continue
                        r = fn(e)
                        lst = list(r) if isinstance(r, (list, tuple)) else [r]
                        assert len(lst) == nsig, (len(lst), nsig)
                        for ins in lst:
                            ins.then_inc(sem.h, unit)
                getattr(blk, en)(body)
        self.q = {e: [] for e in ENG}
        self.phase += 1
        self.new_phase_sems()


class Tile:
    def __init__(self, t, name):
        self.t = t
        self.s = Slot(name)

    def __getitem__(self, k):
        return self.t[k]

    def sub(self, key):
        if not hasattr(self, '_sub'):
            self._sub = {}
        if key not in self._sub:
            self._sub[key] = Slot('%s/%s' % (self.s.name, key))
        return self._sub[key]

    def all(self):
        return [self.s] + list(getattr(self, '_sub', {}).values())


def build_nc(upto=99, debug=False):
    nc = bass.Bass("TRN2", target_bir_lowering=False)
    top = ExitStack()
    P = Prog(nc, top)

    def din(name, shape, dt=F32):
        return nc.dram_tensor(name, list(shape), dt, kind="ExternalInput").ap()

    def dscr(name, shape, dt, out=False):
        kind = "ExternalOutput" if (out and debug) else "Internal"
        return nc.dram_tensor(name, list(shape), dt, kind=kind).ap()

    x_d = din("xs", [T, D])
    w_main = din("w_main", [D, 12288])
    w_dt = din("w_dt", [D, 64])
    norm1_w = din("norm1_w", [1, D])
    band_d = din("band", [8, 6, 128, 512])
    cfar_d = din("cfar", [128, 16])
    lam_d = din("lamv", [4, 128])
    subln_d = din("subln_w", [1, 256])
    convw_d = din("convw", [128, 32, 5])
    convb_d = din("convb", [128, 32])
    dtb_d = din("dt_bias", [1, 64])
    alog_d = din("a_log", [1, 64])
    dskip_d = din("d_skip", [1, 32])
    ssmn_d = din("ssm_norm_w", [1, 2048])
    if upto >= 4:
        w_out = din("w_out", [D, D])
    if upto >= 5:
        norm2_w = din("norm2_w", [1, D])
        w_rt = din("w_rt", [D, 36])
        b_rt = din("b_rt", [1, 36])
    if upto >= 6:
        w_gate = din("w_gate", [NE, D, DFF])
        w_up = din("w_up", [NE, D, DFF])
        w_down = din("w_down", [NE, DFF, D])
    if upto >= 7:
        fnorm_w = din("final_norm_w", [1, D])
        out_d = nc.dram_tensor("out", [TO, D], F32, kind="ExternalOutput").ap()

    hT_d = dscr("hT_d", [8, 128, NCH, 512], BF16)
    qT_d = dscr("qT_d", [16, 128, TO], BF16, out=True)
    kT_d = dscr("kT_d", [16, 128, T], BF16, out=True)
    v_d = dscr("v_d", [T, 2048], BF16, out=True)
    zs_d = dscr("zs_d", [TO, 2048], BF16, out=True)
    uT_d = dscr("uT_d", [32, 128, T], BF16, out=True)
    dt_d = dscr("dt_d", [T, 64], F32, out=True)
    mix_d = dscr("mix_d", [TO, D], BF16, out=True)
    xs_d = dscr("xsc_d", [T, 2048], BF16, out=True)
    bt_d = dscr("bt_d", [T, 1024], BF16)
    yf_d = dscr("yf_d", [TO, 2048], F32, out=True)
    x1_d = dscr("x1_d", [TO, D], F32, out=True)
    xg_d = dscr("xg_d", [NSLOT + 128, D], BF16)
    y_d = dscr("y_d", [NSLOT + 128, D], BF16)

    def sb(stack, name, shape, dt):
        return Tile(stack.enter_context(nc.sbuf_tensor("s_" + name, list(shape), dt)), name)

    def ps(stack, name, shape, dt=F32):
        return Tile(stack.enter_context(nc.psum_tensor("p_" + name, list(shape), dt)), name)

    idf = sb(top, "idf", [128, 128], F32)
    identb = sb(top, "identb", [128, 128], BF16)
    identf = sb(top, "identf", [128, 128], F32)
    U_f = sb(top, "U_f", [128, 128], F32)
    L_f = sb(top, "L_f", [128, 128], F32)
    ones_f = sb(top, "ones_f", [128, 128], F32)
    mneg_f = sb(top, "mneg_f", [128, 128], F32)
    mneg_b = sb(top, "mneg_b", [128, 128], F32)
    Ls_b = sb(top, "Ls_b", [128, 128], BF16)
    ones_b = sb(top, "ones_b", [128, 128], BF16)

    P.op('gpsimd', lambda g: g.iota(idf[:], pattern=[[1, 128]], base=0, channel_multiplier=-1,
                                    allow_small_or_imprecise_dtypes=True), writes=[idf.s])
    P.op('vector', lambda v: v.tensor_single_scalar(out=identb[:], in_=idf[:], scalar=0.0, op=ALU.is_equal),
         reads=[idf.s], writes=[identb.s])
    P.op('vector', lambda v: v.tensor_single_scalar(out=identf[:], in_=idf[:], scalar=0.0, op=ALU.is_equal),
         reads=[idf.s], writes=[identf.s])
    P.op('vector', lambda v: v.tensor_single_scalar(out=U_f[:], in_=idf[:], scalar=0.0, op=ALU.is_ge),
         reads=[idf.s], writes=[U_f.s])
    P.op('vector', lambda v: v.tensor_single_scalar(out=L_f[:], in_=idf[:], scalar=0.0, op=ALU.is_le),
         reads=[idf.s], writes=[L_f.s])
    P.op('vector', lambda v: v.tensor_single_scalar(out=Ls_b[:], in_=idf[:], scalar=0.0, op=ALU.is_gt),
         reads=[idf.s], writes=[Ls_b.s])
    P.op('vector', lambda v: v.memset(ones_f[:], 1.0), writes=[ones_f.s])
    P.op('vector', lambda v: v.memset(ones_b[:], 1.0), writes=[ones_b.s])
    P.op('vector', lambda v: v.tensor_scalar(out=mneg_f[:], in0=U_f[:], scalar1=1.0, scalar2=30000.0,
                                             op0=ALU.subtract, op1=ALU.mult), reads=[U_f.s], writes=[mneg_f.s])
    P.op('vector', lambda v: v.tensor_scalar(out=mneg_b[:], in0=L_f[:], scalar1=1.0, scalar2=30000.0,
                                             op0=ALU.subtract, op1=ALU.mult), reads=[L_f.s], writes=[mneg_b.s])

    def bcast_load(stack, name, src_row, n, eng='sync'):
        t = sb(stack, name, [128, n], F32)
        P.op(eng, lambda e: e.dma_start(out=t[:], in_=src_row.partition_broadcast(128)), writes=[t.s],
             dma=P.sem('ld_' + name))
        return t

    def rmsnorm_tile(xt, wb, hb, junk, ss, std, rstd, ncols):
        P.op('scalar', lambda a: a.activation(out=junk[:], in_=xt[:], func=AF.Square, accum_out=ss[:]),
             reads=[xt.s], writes=[junk.s, ss.s])
        P.op('scalar', lambda a: a.activation(out=std[:], in_=ss[:], func=AF.Sqrt, scale=1.0 / ncols, bias=EPS),
             reads=[ss.s], writes=[std.s])
        P.op('vector', lambda v: v.reciprocal(out=rstd[:], in_=std[:]), reads=[std.s], writes=[rstd.s])
        P.op('vector', lambda v: v.scalar_tensor_tensor(out=hb[:], in0=xt[:], scalar=rstd[:], in1=wb[:],
                                                        op0=ALU.mult, op1=ALU.mult),
             reads=[xt.s, rstd.s, wb.s], writes=[hb.s])

    def transpose_tile(src, dst_fn, ncols, pTs, cnt, ident, dslot_fn):
        nblk = ncols // 128
        for g8 in range((nblk + 7) // 8):
            nb = min(8, nblk - g8 * 8)
            pt = pTs[cnt[0] % len(pTs)]
            cnt[0] += 1

            def tr(t, pt=pt, g8=g8, nb=nb):
                r = None
                for k in range(nb):
                    c = g8 * 8 + k
                    r = t.transpose(out=pt[:, k, :], in_=src[:, c * 128:(c + 1) * 128], identity=ident[:])
                return r
            P.op('tensor', tr, reads=[src.s, ident.s], writes=[pt.s])
            if cnt[0] % 2 == 0:
                P.op('scalar', lambda a, pt=pt, g8=g8, nb=nb: a.copy(out=dst_fn(g8 * 8, nb), in_=pt[:, 0:nb, :]),
                     reads=[pt.s], writes=[dslot_fn(g8)])
            else:
                P.op('vector', lambda v, pt=pt, g8=g8, nb=nb: v.tensor_copy(out=dst_fn(g8 * 8, nb), in_=pt[:, 0:nb, :]),
                     reads=[pt.s], writes=[dslot_fn(g8)])

    with ExitStack() as st:
        w1b = bcast_load(st, "w1b", norm1_w[0:1, :], D)
        xts = [sb(st, "xt%d" % i, [128, D], F32) for i in range(2)]
        junk = sb(st, "junk0", [128, D], BF16)
        hbs = [sb(st, "hb%d" % i, [128, D], BF16) for i in range(2)]
        hTs = [sb(st, "hTs%d" % i, [128, NCH, 512], BF16) for i in range(2)]
        ss = [sb(st, "ss%d" % i, [128, 1], F32) for i in range(2)]
        std = [sb(st, "std%d" % i, [128, 1], F32) for i in range(2)]
        rstd = [sb(st, "rstd%d" % i, [128, 1], F32) for i in range(2)]
        pT = [ps(st, "pT%d" % i, [128, 8, 128], BF16) for i in range(4)]
        ldx = [P.sem("ld_xt%d" % i) for i in range(2)]
        sthT = [P.sem("st_hT%d" % i) for i in range(2)]

        def load_x(i):
            xt = xts[i % 2]
            P.op('sync', lambda e: e.dma_start(out=xt[:], in_=x_d[i * 128:(i + 1) * 128, :]), writes=[xt.s],
                 dma=ldx[i % 2])
        load_x(0)
        cnt = [0]
        for i in range(32):
            if i + 1 < 32:
                load_x(i + 1)
            xt, hb = xts[i % 2], hbs[i % 2]
            rmsnorm_tile(xt, w1b, hb, junk, ss[i % 2], std[i % 2], rstd[i % 2], D)
            tb, j = i // 4, i % 4
            hT = hTs[tb % 2]
            transpose_tile(hb, lambda c0, nb, hT=hT, j=j: hT[:, c0:c0 + nb, j * 128:(j + 1) * 128], D, pT, cnt,
                           identb, lambda g, hT=hT, j=j: hT.sub((j, g)))
            if j == 3:
                P.op('sync', lambda e, hT=hT, tb=tb: e.dma_start(out=hT_d[tb], in_=hT[:]), reads=hT.all(),
                     dma=sthT[tb % 2])
        P.flush("ph0")

    with ExitStack() as st:
        Ws = [sb(st, "Wsl%d" % i, [128, NCH, 512], BF16) for i in range(2)]
        Wdt = sb(st, "Wdt", [128, NCH, 64], BF16)
        hTt = [sb(st, "hTt%d" % i, [128, NCH, 512], BF16) for i in range(2)]
        osb = [sb(st, "osb%d" % i, [128, 512], BF16) for i in range(4)]
        odt = [sb(st, "odt%d" % i, [128, 64], F32) for i in range(2)]
        pacc = [ps(st, "pacc%d" % i, [128, 512], F32) for i in range(6)]
        ldW = [P.sem("ld_W%d" % i) for i in range(2)]
        ldWdt = P.sem("ld_Wdt")
        ldh = [P.sem("ld_hTt%d" % i) for i in range(2)]
        sto = [P.sem("st_osb%d" % i) for i in range(4)]
        stdt = [P.sem("st_odt%d" % i) for i in range(2)]
        wv = w_main.rearrange("(c p) n -> p c n", p=128)
        wdtv = w_dt.rearrange("(c p) n -> p c n", p=128)
        slices = []
        for s_ in range(4):
            slices.append(('q', s_ * 512, 4))
        for s_ in range(4):
            slices.append(('k', 2048 + s_ * 512, 8))
        for s_ in range(4):
            slices.append(('v', 4096 + s_ * 512, 8))
        for s_ in range(4):
            slices.append(('z', 6144 + s_ * 512, 4))
        for s_ in range(8):
            slices.append(('u', 8192 + s_ * 512, 8))
        P.op('gpsimd', lambda g: g.dma_start(out=Wdt[:], in_=wdtv), writes=[Wdt.s], dma=ldWdt)

        def load_W(si):
            W = Ws[si % 2]
            c0 = slices[si][1]
            P.op('gpsimd', lambda g: g.dma_start(out=W[:], in_=wv[:, :, c0:c0 + 512]), writes=[W.s], dma=ldW[si % 2])
        its = [(si, tb) for si in range(len(slices)) for tb in range(slices[si][2])]

        def load_h(k):
            si, tb = its[k]
            h = hTt[k % 2]
            P.op('sync', lambda e: e.dma_start(out=h[:], in_=hT_d[tb]), writes=[h.s], dma=ldh[k % 2])
        load_W(0)
        load_h(0)
        npa = [0]
        nos = [0]
        nev = [0]

        def evac(pa, ob, silu=False):
            nev[0] += 1
            if silu:
                P.op('scalar', lambda a: a.activation(out=ob[:], in_=pa[:], func=AF.Silu), reads=[pa.s], writes=[ob.s])
            elif nev[0] % 2 == 0:
                P.op('scalar', lambda a: a.copy(out=ob[:], in_=pa[:]), reads=[pa.s], writes=[ob.s])
            else:
                P.op('vector', lambda v: v.tensor_copy(out=ob[:], in_=pa[:]), reads=[pa.s], writes=[ob.s])

        for k, (si, tb) in enumerate(its):
            kind, c0, ntb = slices[si]
            if tb == 0 and si + 1 < len(slices):
                load_W(si + 1)
            if k + 1 < len(its):
                load_h(k + 1)
            W, h = Ws[si % 2], hTt[k % 2]
            t0 = tb * 512
            if kind in ('q', 'k', 'u'):
                for j in range(4):
                    pa = pacc[npa[0] % 6]
                    npa[0] += 1

                    def mm(t, pa=pa, W=W, h=h, j=j):
                        r = None
                        for c in range(NCH):
                            r = t.matmul(pa[:], lhsT=W[:, c, j * 128:(j + 1) * 128], rhs=h[:, c, :],
                                         start=(c == 0), stop=(c == NCH - 1))
                        return r
                    P.op('tensor', mm, reads=[W.s, h.s], writes=[pa.s])
                    ob = osb[nos[0] % 4]
                    osem = sto[nos[0] % 4]
                    nos[0] += 1
                    evac(pa, ob)
                    blk = (c0 - {'q': 0, 'k': 2048, 'u': 8192}[kind]) // 128 + j
                    dst = {'q': qT_d, 'k': kT_d, 'u': uT_d}[kind]
                    P.op('gpsimd', lambda g, ob=ob, dst=dst, blk=blk, t0=t0: g.dma_start(
                        out=dst[blk, :, t0:t0 + 512], in_=ob[:]), reads=[ob.s], dma=osem)
            else:
                for t4 in range(4):
                    pa = pacc[npa[0] % 6]
                    npa[0] += 1

                    def mm(t, pa=pa, W=W, h=h, t4=t4):
                        r = None
                        for c in range(NCH):
                            r = t.matmul(pa[:], lhsT=h[:, c, t4 * 128:(t4 + 1) * 128], rhs=W[:, c, :],
                                         start=(c == 0), stop=(c == NCH - 1))
                        return r
                    P.op('tensor', mm, reads=[W.s, h.s], writes=[pa.s])
                    ob = osb[nos[0] % 4]
                    osem = sto[nos[0] % 4]
                    nos[0] += 1
                    evac(pa, ob, silu=(kind == 'z'))
                    cc = c0 - {'v': 4096, 'z': 6144}[kind]
                    dst = {'v': v_d, 'z': zs_d}[kind]
                    r0 = t0 + t4 * 128
                    P.op('gpsimd', lambda g, ob=ob, dst=dst, cc=cc, r0=r0: g.dma_start(
                        out=dst[r0:r0 + 128, cc:cc + 512], in_=ob[:]), reads=[ob.s], dma=osem)
            if kind == 'v' and c0 == 4096:
                for t4 in range(4):
                    pa = pacc[npa[0] % 6]
                    npa[0] += 1

                    def mm(t, pa=pa, h=h, t4=t4):
                        r = None
                        for c in range(NCH):
                            r = t.matmul(pa[:, 0:64], lhsT=h[:, c, t4 * 128:(t4 + 1) * 128], rhs=Wdt[:, c, :],
                                         start=(c == 0), stop=(c == NCH - 1))
                        return r
                    P.op('tensor', mm, reads=[Wdt.s, h.s], writes=[pa.s])
                    od = odt[t4 % 2]
                    P.op('vector', lambda v, od=od, pa=pa: v.tensor_copy(out=od[:], in_=pa[:, 0:64]), reads=[pa.s],
                         writes=[od.s])
                    r0 = t0 + t4 * 128
                    P.op('gpsimd', lambda g, od=od, r0=r0: g.dma_start(out=dt_d[r0:r0 + 128, :], in_=od[:]),
                         reads=[od.s], dma=stdt[t4 % 2])
        P.flush("ph1")
    if upto >= 2:
      with ExitStack() as st:
        qTs = [[sb(st, "qTs%d_%d" % (i, m), [128, TO], BF16) for m in range(2)] for i in range(2)]
        kTs = [[sb(st, "kTs%d_%d" % (i, m), [128, T], BF16) for m in range(2)] for i in range(2)]
        vs = [sb(st, "vs%d" % i, [128, 32, 257], BF16) for i in range(2)]
        bands = [sb(st, "band%d" % i, [128, 6, 512], F32) for i in range(2)]
        cfar = sb(st, "cfar", [128, 16], F32)
        lamv = [bcast_load(st, "lamv%d" % i, lam_d[i:i + 1, :], 128) for i in range(4)]
        sublnb = bcast_load(st, "sublnb", subln_d[0:1, :], 256)
        ldq = [[P.sem("ld_q%d_%d" % (i, m)) for m in range(2)] for i in range(2)]
        ldk = [[P.sem("ld_k%d_%d" % (i, m)) for m in range(2)] for i in range(2)]
        ldv = [P.sem("ld_v%d" % i) for i in range(2)]
        ldb = [P.sem("ld_band%d" % i) for i in range(2)]
        P.op('sync', lambda e: e.dma_start(out=cfar[:], in_=cfar_d), writes=[cfar.s], dma=P.sem("ld_cfar"))
        for i in range(2):
            P.op('vector', lambda v, i=i: v.memset(vs[i][:, :, 256:257], 1.0), writes=[vs[i].sub('ones')])
        lj = sb(st, "lj", [128, 128], F32)
        d1 = sb(st, "d1", [128, 1], F32)
        d2 = sb(st, "d2", [128, 1], F32)
        nlam = sb(st, "nlam", [128, 1], F32)
        for (ia, ib, dd) in ((0, 1, d1), (2, 3, d2)):
            P.op('vector', lambda v, ia=ia, ib=ib: v.tensor_tensor(out=lj[:], in0=lamv[ia][:], in1=lamv[ib][:], op=ALU.mult),
                 reads=[lamv[ia].s, lamv[ib].s, lj.s], writes=[lj.s])
            P.op('vector', lambda v, dd=dd: v.tensor_reduce(out=dd[:], in_=lj[:], axis=AX.X, op=ALU.add), reads=[lj.s],
                 writes=[dd.s])
        P.op('scalar', lambda a: a.activation(out=d1[:], in_=d1[:], func=AF.Exp), reads=[d1.s], writes=[d1.s])
        P.op('scalar', lambda a: a.activation(out=d2[:], in_=d2[:], func=AF.Exp), reads=[d2.s], writes=[d2.s])
        P.op('vector', lambda v: v.tensor_tensor(out=nlam[:], in0=d2[:], in1=d1[:], op=ALU.subtract),
             reads=[d1.s, d2.s], writes=[nlam.s])
        P.op('vector', lambda v: v.tensor_scalar(out=nlam[:], in0=nlam[:], scalar1=-LAM_INIT, scalar2=None, op0=ALU.add),
             reads=[nlam.s], writes=[nlam.s])
        P.op('vector', lambda v: v.tensor_scalar(out=sublnb[:], in0=sublnb[:], scalar1=(1.0 - LAM_INIT), scalar2=None,
                                                 op0=ALU.mult), reads=[sublnb.s], writes=[sublnb.s])

        pS = [ps(st, "pS%d" % i, [128, 512], F32) for i in range(4)]
        pA = [ps(st, "pA%d" % i, [128, 512], F32) for i in range(4)]
        Es = [sb(st, "Es%d" % i, [128, 512], BF16) for i in range(4)]
        tmps = [sb(st, "tmpS%d" % i, [128, 512], F32) for i in range(2)]
        osb2 = [[sb(st, "o%d_%d" % (m, qb), [128, 257], F32) for qb in range(4)] for m in range(2)]
        fin = {n: [sb(st, "fin_%s%d" % (n, i), [128, 1], F32) for i in range(2)] for n in ('r0', 'r1', 'ss', 'sd', 'rs')}
        ao = [sb(st, "ao%d" % i, [128, 256], F32) for i in range(2)]
        aj = sb(st, "aj", [128, 256], F32)
        ay = [sb(st, "ay%d" % i, [128, 256], BF16) for i in range(2)]
        sty = [P.sem("st_ay%d" % i) for i in range(2)]

        def load_head(h):
            i = h % 2
            for m in range(2):
                P.op('sync', lambda e, m=m: e.dma_start(out=qTs[i][m][:], in_=qT_d[2 * h + m]), writes=[qTs[i][m].s],
                     dma=ldq[i][m], nsig=1)
                P.op('sync', lambda e, m=m: e.dma_start(out=kTs[i][m][:], in_=kT_d[2 * h + m]), writes=[kTs[i][m].s],
                     dma=ldk[i][m], nsig=1)
            P.op('sync', lambda e: e.dma_start(out=vs[i][:, :, 0:256],
                                               in_=v_d[:, h * 256:(h + 1) * 256].rearrange("(c p) e -> p c e", p=128)),
                 writes=[vs[i].s], dma=ldv[i])
            P.op('sync', lambda e: e.dma_start(out=bands[i][:], in_=band_d[h].rearrange("r k q -> k r q")),
                 writes=[bands[i].s], dma=ldb[i])
        load_head(0)
        nS = [0]
        nE = [0]
        nT = [0]
        nF = [0]
        ss4 = [sb(st, "ss4_%d" % i, [128, 4], F32) for i in range(2)]
        sd4 = [sb(st, "sd4_%d" % i, [128, 4], F32) for i in range(2)]
        rs4 = [sb(st, "rs4_%d" % i, [128, 4], F32) for i in range(2)]
        ao4 = [[sb(st, "ao4_%d_%d" % (i, qb), [128, 256], F32) for qb in range(4)] for i in range(2)]
        ay4 = [[sb(st, "ay4_%d_%d" % (i, qb), [128, 256], BF16) for qb in range(4)] for i in range(2)]
        sty4 = [[P.sem("st_ay4_%d_%d" % (i, qb)) for qb in range(4)] for i in range(2)]
        pending = []

        def make_finalize(h, qg, f):
            def fin_():
                for qb in range(4):
                    o0, o1 = osb2[0][qb], osb2[1][qb]
                    r0, r1 = fin['r0'][qb % 2], fin['r1'][qb % 2]
                    a_o = ao4[f][qb]
                    P.op('vector', lambda v, r0=r0, o0=o0: v.reciprocal(out=r0[:], in_=o0[:, 256:257]), reads=[o0.s],
                         writes=[r0.s])
                    P.op('vector', lambda v, r1=r1, o1=o1: v.reciprocal(out=r1[:], in_=o1[:, 256:257]), reads=[o1.s],
                         writes=[r1.s])
                    P.op('vector', lambda v, r1=r1: v.tensor_tensor(out=r1[:], in0=r1[:], in1=nlam[:], op=ALU.mult),
                         reads=[r1.s, nlam.s], writes=[r1.s])
                    P.op('vector', lambda v, a_o=a_o, o0=o0, r0=r0: v.tensor_scalar(
                        out=a_o[:], in0=o0[:, 0:256], scalar1=r0[:], scalar2=None, op0=ALU.mult),
                        reads=[o0.s, r0.s], writes=[a_o.s])
                    P.op('vector', lambda v, a_o=a_o, o1=o1, r1=r1: v.scalar_tensor_tensor(
                        out=a_o[:], in0=o1[:, 0:256], scalar=r1[:], in1=a_o[:], op0=ALU.mult, op1=ALU.add),
                        reads=[o1.s, r1.s, a_o.s], writes=[a_o.s])
                    P.op('gpsimd', lambda g, a_o=a_o: g.tensor_tensor(out=aj[:], in0=a_o[:], in1=a_o[:], op=ALU.mult),
                         reads=[a_o.s, aj.s], writes=[aj.s])
                    P.op('vector', lambda v, qb=qb: v.tensor_reduce(out=ss4[f][:, qb:qb + 1], in_=aj[:], axis=AX.X, op=ALU.add),
                         reads=[aj.s], writes=[ss4[f].sub(qb)])
                P.op('scalar', lambda a: a.activation(out=sd4[f][:], in_=ss4[f][:], func=AF.Sqrt, scale=1.0 / 256, bias=EPS),
                     reads=ss4[f].all(), writes=[sd4[f].s])
                P.op('vector', lambda v: v.reciprocal(out=rs4[f][:], in_=sd4[f][:]), reads=[sd4[f].s], writes=[rs4[f].s])
                for qb in range(4):
                    a_o, a_y = ao4[f][qb], ay4[f][qb]
                    P.op('vector', lambda v, a_y=a_y, a_o=a_o, qb=qb: v.scalar_tensor_tensor(
                        out=a_y[:], in0=a_o[:], scalar=rs4[f][:, qb:qb + 1], in1=sublnb[:], op0=ALU.mult, op1=ALU.mult),
                        reads=[a_o.s, rs4[f].s, sublnb.s], writes=[a_y.s])
                    q0 = qg * 512 + qb * 128
                    P.op('gpsimd', lambda g, a_y=a_y, q0=q0: g.dma_start(
                        out=mix_d[q0:q0 + 128, h * 256:(h + 1) * 256], in_=a_y[:]), reads=[a_y.s], dma=sty4[f][qb])
            return fin_

        LOOK = 3
        for h in range(8):
            if h + 1 < 8:
                load_head(h + 1)
            i = h % 2
            for qg in range(4):
                for m in range(2):
                    qT, kT, vv, bd = qTs[i][m], kTs[i][m], vs[i], bands[i]
                    pSq = {}

                    def emit_S(kc, qT=qT, kT=kT, qg=qg):
                        pS_ = pS[nS[0] % 4]
                        nS[0] += 1
                        P.op('tensor', lambda t, pS_=pS_, kc=kc: t.matmul(
                            pS_[:], lhsT=kT[:, kc * 128:(kc + 1) * 128], rhs=qT[:, qg * 512:(qg + 1) * 512],
                            start=True, stop=True), reads=[kT.s, qT.s], writes=[pS_.s])
                        pSq[kc] = pS_
                    for kc in range(LOOK):
                        emit_S(kc)
                    for kc in range(32):
                        pS_ = pSq.pop(kc)
                        E = Es[nE[0] % 4]
                        nE[0] += 1
                        r = kc - 4 * qg
                        if -1 <= r <= 4:
                            tm = tmps[nT[0] % 2]
                            nT[0] += 1
                            P.op('vector', lambda v, tm=tm, pS_=pS_, bd=bd, r=r: v.scalar_tensor_tensor(
                                out=tm[:], in0=pS_[:], scalar=SCALE, in1=bd[:, r + 1, :], op0=ALU.mult, op1=ALU.add),
                                reads=[pS_.s, bd.s], writes=[tm.s])
                            P.op('scalar', lambda a, E=E, tm=tm: a.activation(out=E[:], in_=tm[:], func=AF.Exp),
                                 reads=[tm.s], writes=[E.s])
                        else:
                            ci = 2 * h + (0 if r < -1 else 1)
                            P.op('scalar', lambda a, E=E, pS_=pS_, ci=ci: a.activation(
                                out=E[:], in_=pS_[:], func=AF.Exp, scale=SCALE, bias=cfar[:, ci:ci + 1]),
                                reads=[pS_.s, cfar.s], writes=[E.s])
                        if kc + LOOK < 32:
                            emit_S(kc + LOOK)

                        def av(t, E=E, vv=vv, kc=kc):
                            rr = None
                            for qb in range(4):
                                rr = t.matmul(pA[qb][:, 0:257], lhsT=E[:, qb * 128:(qb + 1) * 128], rhs=vv[:, kc, :],
                                              start=(kc == 0), stop=(kc == 31))
                            return rr
                        P.op('tensor', av, reads=[E.s] + vv.all(), writes=[pA[qb].s for qb in range(4)])
                        if kc == 10 and m == 0 and pending:
                            pending.pop(0)()
                    for qb in range(4):
                        o = osb2[m][qb]
                        P.op('scalar', lambda a, o=o, qb=qb: a.copy(out=o[:], in_=pA[qb][:, 0:257]), reads=[pA[qb].s],
                             writes=[o.s])
                pending.append(make_finalize(h, qg, nF[0] % 2))
                nF[0] += 1
        while pending:
            pending.pop(0)()
        P.flush("ph2")
    bT_d = dscr("bT_d", [8, 128, T], BF16)
    cT_d = dscr("cT_d", [8, 128, T], BF16)
    if upto >= 2.5:
      with ExitStack() as st:
        cw = sb(st, "cw", [128, 32, 5], F32)
        cb = sb(st, "cb", [128, 32], F32)
        P.op('sync', lambda e: e.dma_start(out=cw[:], in_=convw_d), writes=[cw.s], dma=P.sem("ld_cw"))
        P.op('sync', lambda e: e.dma_start(out=cb[:], in_=convb_d), writes=[cb.s], dma=P.sem("ld_cb"))
        ups = [sb(st, "up%d" % i, [128, T + 4], BF16) for i in range(2)]
        accs = [sb(st, "cacc%d" % i, [128, T], F32) for i in range(2)]
        cvos = [sb(st, "cvo%d" % i, [128, T], BF16) for i in range(2)]
        stg = [sb(st, "cstg%d" % i, [128, 32, 256], BF16) for i in range(2)]
        pT3 = [ps(st, "pT3_%d" % i, [128, 8, 128], BF16) for i in range(4)]
        ldu = [P.sem("ld_up%d" % i) for i in range(2)]
        stc = [P.sem("st_cvo%d" % i) for i in range(2)]
        sts = [P.sem("st_cstg%d" % i) for i in range(2)]
        for i in range(2):
            P.op('vector', lambda v, i=i: v.memset(ups[i][:, 0:2], 0.0), writes=[ups[i].sub('pl')])
            P.op('vector', lambda v, i=i: v.memset(ups[i][:, T + 2:T + 4], 0.0), writes=[ups[i].sub('pr')])

        def load_u(b):
            P.op('sync', lambda e: e.dma_start(out=ups[b % 2][:, 2:T + 2], in_=uT_d[b]), writes=[ups[b % 2].s],
                 dma=ldu[b % 2])
        load_u(0)
        cnt3 = [0]
        for b in range(32):
            if b + 1 < 32:
                load_u(b + 1)
            up, acc, cvo = ups[b % 2], accs[b % 2], cvos[b % 2]
            P.op('vector', lambda v, up=up, acc=acc, b=b: v.tensor_scalar(
                out=acc[:], in0=up[:, 0:T], scalar1=cw[:, b, 0:1], scalar2=None, op0=ALU.mult),
                reads=up.all() + [cw.s], writes=[acc.s])
            for j in range(1, 5):
                P.op('vector', lambda v, up=up, acc=acc, b=b, j=j: v.scalar_tensor_tensor(
                    out=acc[:], in0=up[:, j:j + T], scalar=cw[:, b, j:j + 1], in1=acc[:], op0=ALU.mult, op1=ALU.add),
                    reads=up.all() + [cw.s, acc.s], writes=[acc.s])
            P.op('scalar', lambda a, acc=acc, cvo=cvo, b=b: a.activation(out=cvo[:], in_=acc[:], func=AF.Silu,
                                                                       bias=cb[:, b:b + 1]),
                 reads=[acc.s, cb.s], writes=[cvo.s])
            if b >= 16:
                dst = bT_d if b < 24 else cT_d
                g = (b - 16) % 8
                P.op('gpsimd', lambda e, cvo=cvo, dst=dst, g=g: e.dma_start(out=dst[g], in_=cvo[:]), reads=[cvo.s],
                     dma=stc[b % 2])
            if b < 24:
                sg = stg[(b // 2) % 2]
                half = b % 2
                transpose_tile(cvo, lambda c0, nb, sg=sg, half=half: sg[:, c0:c0 + nb, half * 128:(half + 1) * 128],
                               T, pT3, cnt3, identb, lambda g8, sg=sg, half=half: sg.sub((half, g8)))
                if half == 1:
                    if b < 16:
                        dstv = xs_d.rearrange("(c p) n -> p c n", p=128)[:, :, (b - 1) * 128:(b + 1) * 128]
                    else:
                        dstv = bt_d.rearrange("(c p) n -> p c n", p=128)[:, :, (b - 17) * 128:(b - 15) * 128]
                    P.op('sync', lambda e, sg=sg, dstv=dstv: e.dma_start(out=dstv, in_=sg[:]), reads=sg.all(),
                         dma=sts[(b // 2) % 2])
        P.flush("ph3a")

    if upto >= 3:
      with ExitStack() as st:
        BTc = [sb(st, "BTc%d" % i, [128, 8, 128], BF16) for i in range(2)]
        CTc = [sb(st, "CTc%d" % i, [128, 8, 128], BF16) for i in range(2)]
        ldBT = [P.sem("ld_BTc%d" % i) for i in range(2)]
        ldCT = [P.sem("ld_CTc%d" % i) for i in range(2)]
        dtbb = bcast_load(st, "dtbb", dtb_d[0:1, :], 64)
        Ab = bcast_load(st, "Ab", alog_d[0:1, :], 64)
        dskb = bcast_load(st, "dskb", dskip_d[0:1, :], 32)
        ssmnb = bcast_load(st, "ssmnb", ssmn_d[0:1, :], 2048)
        P.op('scalar', lambda a: a.activation(out=Ab[:], in_=Ab[:], func=AF.Exp), reads=[Ab.s], writes=[Ab.s])
        P.op('vector', lambda v: v.tensor_scalar(out=Ab[:], in0=Ab[:], scalar1=-1.0, scalar2=None, op0=ALU.mult),
             reads=[Ab.s], writes=[Ab.s])
        xsc = [sb(st, "xsc%d" % i, [128, 32, 64], BF16) for i in range(2)]
        btc = [sb(st, "btc%d" % i, [128, 1024], BF16) for i in range(2)]
        dtr = [sb(st, "dtr%d" % i, [128, 32], F32) for i in range(2)]
        yfl = [sb(st, "yfl%d" % i, [128, 2048], F32) for i in range(2)]
        zsl = [sb(st, "zsl%d" % i, [128, 2048], BF16) for i in range(2)]
        ldxs = [P.sem("ld_xsc%d" % i) for i in range(2)]
        ldbt = [P.sem("ld_btc%d" % i) for i in range(2)]
        lddt = [P.sem("ld_dtr%d" % i) for i in range(2)]
        ldyf = [P.sem("ld_yfl%d" % i) for i in range(2)]
        ldzs = [P.sem("ld_zsl%d" % i) for i in range(2)]
        sm = {n: sb(st, "sm_" + n, [128, 32], F32) for n in ('t1', 'dt', 'a', 'eacs', 'cd', 'w')}
        acst = sb(st, "acst", [128, 64], F32)
        nacs = sb(st, "nacs", [128, 32], F32)
        mneg4 = [sb(st, "mneg4_%d" % i, [128, 4, 128], F32) for i in range(2)]
        for i_, mm_ in ((0, mneg_f), (1, mneg_b)):
            P.op('vector', lambda v, i_=i_, mm_=mm_: v.tensor_copy(out=mneg4[i_][:], in_=bc(mm_[:], [128, 4, 128], 1)),
                 reads=[mm_.s], writes=[mneg4[i_].s])
        xdt = sb(st, "xdt", [128, 32, 64], BF16)
        xw = sb(st, "xw", [128, 32, 64], BF16)
        rbig = sb(st, "rbig", [128, 32, 128], F32)
        dms = [sb(st, "dm%d" % i, [128, 4, 128], F32) for i in range(2)]
        decs = [sb(st, "dec%d" % i, [128, 4, 128], F32) for i in range(2)]
        MTs = [sb(st, "MT%d" % i, [128, 4, 128], BF16) for i in range(2)]
        yos = [sb(st, "yo%d" % i, [128, 4, 64], F32) for i in range(2)]
        ysb = [sb(st, "ysb%d" % i, [128, 32, 64], F32) for i in range(2)]
        Sst = sb(st, "Sst", [128, 32, 64], F32)
        Sbf = sb(st, "Sbf", [128, 32, 64], BF16)
        sq = sb(st, "sq", [128, 8, 256], F32)
        gss = sb(st, "gss", [128, 8], F32)
        gsd = sb(st, "gsd", [128, 8], F32)
        grs = sb(st, "grs", [128, 8], F32)
        yout = [sb(st, "yout%d" % i, [128, 8, 256], BF16) for i in range(2)]
        styf = [P.sem("st_ysb%d" % i) for i in range(2)]
        styo = [P.sem("st_yout%d" % i) for i in range(2)]
        pc = ps(st, "pc", [128, 512], F32)
        pR = [ps(st, "pR%d" % i, [128, 4, 128], F32) for i in range(2)]
        pB = [ps(st, "pB%d" % i, [128, 512], F32) for i in range(2)]
        pC = [ps(st, "pC%d" % i, [128, 512], F32) for i in range(2)]

        def bc(ap, shape, axis):
            return ap.unsqueeze(axis).to_broadcast(shape)

        seq = [(0, c, True) for c in range(16)] + [(1, c, False) for c in range(31, 15, -1)] + \
              [(1, c, True) for c in range(15, -1, -1)]
        import os as _os
        if _os.environ.get("SSD_MAXK"):
            seq = seq[:int(_os.environ["SSD_MAXK"])]
        if _os.environ.get("SSD_NOY"):
            seq = [(d_, c_, False) for (d_, c_, n_) in seq]

        def load_chunk(k):
            d, c, need_y = seq[k]
            i = k % 2
            r0 = c * 128
            P.op('sync', lambda e: e.dma_start(out=xsc[i][:], in_=xs_d[r0:r0 + 128, :].rearrange("p (h e) -> p h e", e=64)),
                 writes=[xsc[i].s], dma=ldxs[i])
            P.op('sync', lambda e: e.dma_start(out=btc[i][:], in_=bt_d[r0:r0 + 128, :]), writes=[btc[i].s], dma=ldbt[i])
            P.op('sync', lambda e: e.dma_start(out=dtr[i][:], in_=dt_d[r0:r0 + 128, d * 32:(d + 1) * 32]),
                 writes=[dtr[i].s], dma=lddt[i])
            if need_y:
                P.op('sync', lambda e: e.dma_start(out=BTc[i][:], in_=bT_d[:, :, r0:r0 + 128].rearrange("g p t -> p g t")),
                     writes=[BTc[i].s], dma=ldBT[i])
                P.op('sync', lambda e: e.dma_start(out=CTc[i][:], in_=cT_d[:, :, r0:r0 + 128].rearrange("g p t -> p g t")),
                     writes=[CTc[i].s], dma=ldCT[i])
            if d == 1 and need_y:
                P.op('sync', lambda e: e.dma_start(out=yfl[i][:], in_=yf_d[r0:r0 + 128, :]), writes=[yfl[i].s], dma=ldyf[i])
                P.op('sync', lambda e: e.dma_start(out=zsl[i][:], in_=zs_d[r0:r0 + 128, :]), writes=[zsl[i].s], dma=ldzs[i])
        yf_slot = Slot('yf_dram')
        load_chunk(0)
        ng = [0]
        for k, (d, c, need_y) in enumerate(seq):
            if k == 16:
                P.op('vector', lambda v: v.memset(Sst[:], 0.0), writes=[Sst.s])
                P.op('vector', lambda v: v.memset(Sbf[:], 0.0), writes=[Sbf.s])
            if k == 0:
                P.op('vector', lambda v: v.memset(Sst[:], 0.0), writes=[Sst.s])
                P.op('vector', lambda v: v.memset(Sbf[:], 0.0), writes=[Sbf.s])
            if k + 1 < len(seq):
                load_chunk(k + 1)
            i = k % 2
            xs_, bt_, dr = xsc[i], btc[i], dtr[i]
            BT, CT = BTc[i], CTc[i]
            hc = slice(d * 32, (d + 1) * 32)
            Tri = U_f if d == 0 else L_f
            mneg = mneg_f if d == 0 else mneg_b
            t1, dt, a_, eacs, cd, w_ = (sm[n] for n in ('t1', 'dt', 'a', 'eacs', 'cd', 'w'))
            acs = acst
            P.op('vector', lambda v, dr=dr, hc=hc: v.tensor_tensor(out=t1[:], in0=dr[:], in1=dtbb[:, hc], op=ALU.add),
                 reads=[dr.s, dtbb.s], writes=[t1.s])
            P.op('scalar', lambda a: a.activation(out=t1[:], in_=t1[:], func=AF.Exp), reads=[t1.s], writes=[t1.s])
            P.op('scalar', lambda a: a.activation(out=dt[:], in_=t1[:], func=AF.Ln, bias=1.0), reads=[t1.s],
                 writes=[dt.s])
            P.op('vector', lambda v, hc=hc: v.tensor_tensor(out=a_[:], in0=dt[:], in1=Ab[:, hc], op=ALU.mult),
                 reads=[dt.s, Ab.s], writes=[a_.s])

            def mmc(t, Tri=Tri):
                t.matmul(pc[:, 0:32], lhsT=Tri[:], rhs=a_[:], start=True, stop=True)
                return t.matmul(pc[:, 32:64], lhsT=ones_f[:], rhs=a_[:], start=True, stop=True)
            P.op('tensor', mmc, reads=[Tri.s, ones_f.s, a_.s], writes=[pc.s])
            P.op('vector', lambda v: v.tensor_copy(out=acst[:], in_=pc[:, 0:64]), reads=[pc.s], writes=[acst.s])
            P.op('vector', lambda v: v.tensor_scalar(out=nacs[:], in0=acst[:, 0:32], scalar1=-1.0, scalar2=None, op0=ALU.mult),
                 reads=[acst.s], writes=[nacs.s])
            P.op('scalar', lambda a: a.activation(out=eacs[:], in_=acst[:, 0:32], func=AF.Exp), reads=[acst.s], writes=[eacs.s])
            P.op('scalar', lambda a: a.activation(out=cd[:], in_=acst[:, 32:64], func=AF.Exp), reads=[acst.s], writes=[cd.s])
            P.op('vector', lambda v: v.tensor_tensor(out=w_[:], in0=acst[:, 32:64], in1=acst[:, 0:32], op=ALU.subtract),
                 reads=[acst.s], writes=[w_.s])
            P.op('scalar', lambda a: a.activation(out=w_[:], in_=w_[:], func=AF.Exp), reads=[w_.s], writes=[w_.s])
            P.op('vector', lambda v: v.tensor_tensor(out=w_[:], in0=w_[:], in1=dt[:], op=ALU.mult), reads=[w_.s, dt.s],
                 writes=[w_.s])
            P.op('gpsimd', lambda v, xs_=xs_: v.tensor_tensor(out=xw[:], in0=xs_[:], in1=bc(w_[:], [128, 32, 64], 2),
                                                             op=ALU.mult), reads=[xs_.s, w_.s], writes=[xw.s])
            if need_y:
                P.op('gpsimd', lambda v, xs_=xs_: v.tensor_tensor(out=xdt[:], in0=xs_[:], in1=bc(dt[:], [128, 32, 64], 2),
                                                                 op=ALU.mult), reads=[xs_.s, dt.s], writes=[xdt.s])
                P.op('gpsimd', lambda v, Tri=Tri: v.tensor_tensor(out=rbig[:], in0=bc(a_[:], [128, 32, 128], 2),
                                                                  in1=bc(Tri[:], [128, 32, 128], 1), op=ALU.mult),
                     reads=[a_.s, Tri.s], writes=[rbig.s])
            ys = ysb[i]
            for g in range(8):
                j = ng[0] % 2
                ng[0] += 1
                pR_, pB_, pC_ = pR[j], pB[j], pC[j]
                cs = slice(c * 128, (c + 1) * 128)
                if need_y:
                    m4 = mneg4[d]

                    def mmR(t, pR_=pR_, g=g, m4=m4):
                        t.matmul(pR_[:], lhsT=ones_f[:], rhs=rbig[:, 4 * g:4 * g + 4, :], start=True, stop=False)
                        return t.matmul(pR_[:], lhsT=identf[:], rhs=m4[:], start=False, stop=True)
                    P.op('tensor', mmR, reads=[ones_f.s, rbig.s, identf.s, m4.s], writes=[pR_.s])
                    P.op('tensor', lambda t, pB_=pB_, g=g, BT=BT, CT=CT: t.matmul(pB_[:, 0:128], lhsT=BT[:, g, :], rhs=CT[:, g, :],
                                                                           start=True, stop=True),
                         reads=[BT.s, CT.s], rw=[pB_.s])
                    dec, MT, yo = decs[j], MTs[j], yos[j]
                    for r in range(4):
                        P.op('scalar', lambda a, dec=dec, pR_=pR_, r=r, g=g: a.activation(
                            out=dec[:, r, :], in_=pR_[:, r, :], func=AF.Exp, bias=nacs[:, 4 * g + r:4 * g + r + 1]),
                            reads=[pR_.s, nacs.s], writes=[dec.sub(r)])
                    P.op('vector', lambda v, MT=MT, dec=dec, pB_=pB_: v.tensor_tensor(
                        out=MT[:], in0=dec[:], in1=bc(pB_[:, 0:128], [128, 4, 128], 1), op=ALU.mult),
                        reads=dec.all(), writes=[MT.s], rw=[pB_.s])

                    def mmy(t, pB_=pB_, MT=MT, g=g):
                        rr = None
                        for r in range(4):
                            rr = t.matmul(pB_[:, 128 + r * 64:128 + (r + 1) * 64], lhsT=MT[:, r, :], rhs=xdt[:, 4 * g + r, :],
                                          start=True, stop=True)
                        return rr
                    P.op('tensor', mmy, reads=[MT.s, xdt.s], rw=[pB_.s])
                    P.op('tensor', lambda t, pC_=pC_, g=g, CT=CT: t.matmul(
                        pC_[:, 0:256], lhsT=CT[:, g, :], rhs=Sbf[:, 4 * g:4 * g + 4, :], start=True, stop=True),
                        reads=[CT.s, Sbf.s, Sbf.sub(g)], rw=[pC_.s])
                    for r in range(4):
                        P.op('scalar', lambda a, yo=yo, pC_=pC_, g=g, r=r: a.activation(
                            out=yo[:, r, :], in_=pC_[:, r * 64:(r + 1) * 64], func=AF.Identity, scale=eacs[:, 4 * g + r:4 * g + r + 1]),
                            reads=[eacs.s], writes=[yo.sub(r)], rw=[pC_.s])
                    P.op('vector', lambda v, ys=ys, yo=yo, pB_=pB_, g=g: v.tensor_tensor(
                        out=ys[:, 4 * g:4 * g + 4, :], in0=yo[:],
                        in1=pB_[:, 128:384].rearrange("p (r e) -> p r e", e=64), op=ALU.add),
                        reads=yo.all(), writes=[ys.sub(g)], rw=[pB_.s])
                P.op('tensor', lambda t, pC_=pC_, bt_=bt_, g=g: t.matmul(
                    pC_[:, 256:512], lhsT=bt_[:, g * 128:(g + 1) * 128], rhs=xw[:, 4 * g:4 * g + 4, :], start=True, stop=True),
                    reads=[bt_.s, xw.s], rw=[pC_.s])
                P.op('vector', lambda v, g=g: v.tensor_tensor(
                    out=Sst[:, 4 * g:4 * g + 4, :], in0=Sst[:, 4 * g:4 * g + 4, :],
                    in1=bc(cd[:, 4 * g:4 * g + 4], [128, 4, 64], 2), op=ALU.mult),
                    reads=[cd.s, Sst.s, Sst.sub(g)], writes=[Sst.sub(g)])
                P.op('vector', lambda v, pC_=pC_, g=g: v.tensor_tensor(
                    out=Sst[:, 4 * g:4 * g + 4, :], in0=Sst[:, 4 * g:4 * g + 4, :],
                    in1=pC_[:, 256:512].rearrange("p (r e) -> p r e", e=64), op=ALU.add),
                    reads=[Sst.sub(g)], writes=[Sst.sub(g)], rw=[pC_.s])
                P.op('scalar', lambda a, g=g: a.copy(out=Sbf[:, 4 * g:4 * g + 4, :], in_=Sst[:, 4 * g:4 * g + 4, :]),
                     reads=[Sst.sub(g)], writes=[Sbf.sub(g)])
            if need_y and d == 0:
                P.op('vector', lambda v, xs_=xs_: v.tensor_tensor(out=xdt[:], in0=xs_[:], in1=bc(dskb[:], [128, 32, 64], 2),
                                                                 op=ALU.mult), reads=[xs_.s, dskb.s, xdt.s], writes=[xdt.s])
                P.op('vector', lambda v, ys=ys: v.tensor_tensor(out=ys[:], in0=ys[:], in1=xdt[:], op=ALU.add),
                     reads=ys.all() + [xdt.s], writes=[ys.s])
                r0 = c * 128
                P.op('gpsimd', lambda e, ys=ys, r0=r0: e.dma_start(out=yf_d[r0:r0 + 128, :].rearrange("p (h e) -> p h e", e=64),
                                                                   in_=ys[:]), reads=ys.all(), dma=styf[i])
            if need_y and d == 1:
                yf_, zs_ = yfl[i], zsl[i]
                yo_ = yout[i]
                ysf = ys[:].rearrange("p h e -> p (h e)")
                P.op('gpsimd', lambda v, ys=ys, yf_=yf_: v.tensor_tensor(out=ys[:].rearrange("p h e -> p (h e)"),
                                                                         in0=ys[:].rearrange("p h e -> p (h e)"),
                                                                         in1=yf_[:], op=ALU.add),
                     reads=ys.all() + [yf_.s], writes=[ys.s])
                P.op('gpsimd', lambda v, ys=ys, zs_=zs_: v.tensor_tensor(out=ys[:].rearrange("p h e -> p (h e)"),
                                                                         in0=ys[:].rearrange("p h e -> p (h e)"),
                                                                         in1=zs_[:], op=ALU.mult),
                     reads=[ys.s, zs_.s], writes=[ys.s])
                P.op('gpsimd', lambda v, ys=ys: v.tensor_tensor(out=sq[:].rearrange("p g e -> p (g e)"),
                                                                in0=ys[:].rearrange("p h e -> p (h e)"),
                                                                in1=ys[:].rearrange("p h e -> p (h e)"), op=ALU.mult),
                     reads=[ys.s], writes=[sq.s])
                P.op('vector', lambda v: v.tensor_reduce(out=gss[:], in_=sq[:], axis=AX.X, op=ALU.add), reads=[sq.s],
                     writes=[gss.s])
                P.op('scalar', lambda a: a.activation(out=gsd[:], in_=gss[:], func=AF.Sqrt, scale=1.0 / 256, bias=EPS),
                     reads=[gss.s], writes=[gsd.s])
                P.op('vector', lambda v: v.reciprocal(out=grs[:], in_=gsd[:]), reads=[gsd.s], writes=[grs.s])
                P.op('vector', lambda v, ys=ys: v.tensor_tensor(out=sq[:], in0=ys[:].rearrange("p (g r) e -> p g (r e)", r=4),
                                                                in1=bc(grs[:], [128, 8, 256], 2), op=ALU.mult),
                     reads=[ys.s, grs.s, sq.s], writes=[sq.s])
                P.op('vector', lambda v, yo_=yo_: v.tensor_tensor(out=yo_[:].rearrange("p g e -> p (g e)"),
                                                                  in0=sq[:].rearrange("p g e -> p (g e)"),
                                                                  in1=ssmnb[:], op=ALU.mult),
                     reads=[sq.s, ssmnb.s], writes=[yo_.s])
                r0 = c * 128
                P.op('gpsimd', lambda e, yo_=yo_, r0=r0: e.dma_start(
                    out=mix_d[r0:r0 + 128, 2048:4096].rearrange("p (g e) -> p g e", e=256), in_=yo_[:]),
                    reads=[yo_.s], dma=styo[i])
            if k == 15 and len(seq) > 16:
                P.flush("ph3f")
        P.flush("ph3b")
    if upto >= 4:
      with ExitStack() as st:
        mixT = sb(st, "mixT", [128, NCH, TO], BF16)
        mts = [sb(st, "mt%d" % i, [128, D], BF16) for i in range(2)]
        pT4 = [ps(st, "pT4_%d" % i, [128, 8, 128], BF16) for i in range(2)]
        ldm = [P.sem("ld_mt%d" % i) for i in range(2)]
        cnt4 = [0]

        def load_m(i):
            P.op('sync', lambda e: e.dma_start(out=mts[i % 2][:], in_=mix_d[i * 128:(i + 1) * 128, :]), writes=[mts[i % 2].s],
                 dma=ldm[i % 2])
        load_m(0)
        for i in range(16):
            if i + 1 < 16:
                load_m(i + 1)
            transpose_tile(mts[i % 2], lambda c0, nb, i=i: mixT[:, c0:c0 + nb, i * 128:(i + 1) * 128], D, pT4, cnt4, identb,
                           lambda g8, i=i: mixT.sub((i, g8)))
        Wo = [sb(st, "Wo%d" % i, [128, NCH, 256], BF16) for i in range(2)]
        ldWo = [P.sem("ld_Wo%d" % i) for i in range(2)]
        xr = [sb(st, "xr%d" % i, [128, 256], F32) for i in range(3)]
        ldxr = [P.sem("ld_xr%d" % i) for i in range(3)]
        x1s = [sb(st, "x1s%d" % i, [128, 256], F32) for i in range(3)]
        stx1 = [P.sem("st_x1s%d" % i) for i in range(3)]
        pO4 = [ps(st, "pO4_%d" % i, [128, 512], F32) for i in range(4)]
        wov = w_out.rearrange("(c p) n -> p c n", p=128)

        def load_Wo(s_):
            P.op('gpsimd', lambda g: g.dma_start(out=Wo[s_ % 2][:], in_=wov[:, :, s_ * 256:(s_ + 1) * 256]),
                 writes=[Wo[s_ % 2].s], dma=ldWo[s_ % 2])
        load_Wo(0)
        n4 = 0
        for s_ in range(16):
            if s_ + 1 < 16:
                load_Wo(s_ + 1)
            W = Wo[s_ % 2]
            for tt in range(16):
                xr_ = xr[n4 % 3]
                x1_ = x1s[n4 % 3]
                pa = pO4[n4 % 4]
                P.op('sync', lambda e, xr_=xr_, tt=tt, s_=s_: e.dma_start(
                    out=xr_[:], in_=x_d[tt * 128:(tt + 1) * 128, s_ * 256:(s_ + 1) * 256]), writes=[xr_.s], dma=ldxr[n4 % 3])

                def mm(t, pa=pa, W=W, tt=tt):
                    r = None
                    for c in range(NCH):
                        r = t.matmul(pa[:, 0:256], lhsT=mixT[:, c, tt * 128:(tt + 1) * 128], rhs=W[:, c, :],
                                     start=(c == 0), stop=(c == NCH - 1))
                    return r
                P.op('tensor', mm, reads=[W.s] + mixT.all(), writes=[pa.s])
                P.op('vector', lambda v, x1_=x1_, pa=pa, xr_=xr_: v.tensor_tensor(out=x1_[:], in0=pa[:, 0:256], in1=xr_[:],
                                                                                  op=ALU.add),
                     reads=[pa.s, xr_.s], writes=[x1_.s])
                P.op('sync', lambda e, x1_=x1_, tt=tt, s_=s_: e.dma_start(
                    out=x1_d[tt * 128:(tt + 1) * 128, s_ * 256:(s_ + 1) * 256], in_=x1_[:]), reads=[x1_.s], dma=stx1[n4 % 3])
                n4 += 1
        P.flush("ph4")

    if upto >= 5:
        idx_t = [[sb(top, "idx%d_%d" % (k, tt), [128, 1], I32) for tt in range(16)] for k in range(2)]
        gate_t = sb(top, "gate_t", [128, 2, 16], F32)
    if upto >= 5:
      with ExitStack() as st:
        w2b = bcast_load(st, "w2b", norm2_w[0:1, :], D)
        brt = bcast_load(st, "brt", b_rt[0:1, :], 36)
        Wr = sb(st, "Wr", [128, NCH, 36], F32)
        P.op('sync', lambda e: e.dma_start(out=Wr[:], in_=w_rt.rearrange("(c p) n -> p c n", p=128)), writes=[Wr.s],
             dma=P.sem("ld_Wr"))
        eidx = sb(st, "eidx", [128, 32], F32)
        pidx = sb(st, "pidx", [128, 1], F32)
        P.op('gpsimd', lambda g: g.iota(eidx[:], pattern=[[1, 32]], base=0, channel_multiplier=0,
                                        allow_small_or_imprecise_dtypes=True), writes=[eidx.s])
        P.op('gpsimd', lambda g: g.iota(pidx[:], pattern=[[1, 1]], base=NSLOT, channel_multiplier=1,
                                        allow_small_or_imprecise_dtypes=True), writes=[pidx.s])
        cum = sb(st, "cum", [128, 32], F32)
        P.op('vector', lambda v: v.memset(cum[:], 0.0), writes=[cum.s])
        zt = sb(st, "zt", [128, D], BF16)
        P.op('gpsimd', lambda g: g.memset(zt[:], 0.0), writes=[zt.s])
        P.op('sync', lambda e: e.dma_start(out=y_d[NSLOT:NSLOT + 128, :], in_=zt[:]), reads=[zt.s], dma=P.sem("st_zt"))
        x1t = [sb(st, "x1t%d" % i, [128, D], F32) for i in range(2)]
        ldx1 = [P.sem("ld_x1t%d" % i) for i in range(2)]
        h2f = sb(st, "h2f", [128, D], F32)
        h2b = [sb(st, "h2b%d" % i, [128, D], BF16) for i in range(2)]
        junk5 = sb(st, "junk5", [128, D], BF16)
        h2T = sb(st, "h2T", [128, NCH, 128], F32)
        ss5, sd5, rs5 = (sb(st, n, [128, 1], F32) for n in ("ss5", "sd5", "rs5"))
        pT5 = [ps(st, "pT5_%d" % i, [128, 4, 128], F32) for i in range(3)]
        pL = ps(st, "pL", [128, 512], F32)
        pP = ps(st, "pP", [128, 512], F32)
        R = {n: sb(st, "R_" + n, [128, sz], F32) for n, sz in (
            ('lg', 36), ('gmax', 1), ('ngmax', 1), ('gmask', 4), ('ge', 4), ('gsum', 1), ('pg', 1), ('pen', 4), ('mel', 32),
            ('top8', 8), ('sel', 32), ('is1', 32), ('is2', 32), ('dm', 1), ('ew', 1), ('den', 1), ('rden', 1), ('e1', 1),
            ('e2', 1), ('p1', 1), ('p2', 1), ('pos', 32), ('j32', 32), ('ok', 1), ('sf', 1), ('tf', 1))}
        selb = sb(st, "selb", [128, 32], BF16)
        scs = [P.sem("sc_h2b%d" % i) for i in range(2)]

        def load_x1(i):
            P.op('sync', lambda e: e.dma_start(out=x1t[i % 2][:], in_=x1_d[i * 128:(i + 1) * 128, :]), writes=[x1t[i % 2].s],
                 dma=ldx1[i % 2])
        load_x1(0)
        n5 = [0]

        def V(fn, reads, writes):
            P.op('vector', fn, reads=[R[n].s if isinstance(n, str) else n for n in reads],
                 writes=[R[n].s if isinstance(n, str) else n for n in writes])

        for tt in range(16):
            if tt + 1 < 16:
                load_x1(tt + 1)
            xt = x1t[tt % 2]
            hb_ = h2b[tt % 2]
            rmsnorm_tile(xt, w2b, h2f, junk5, ss5, sd5, rs5, D)
            P.op('scalar', lambda a, hb_=hb_: a.copy(out=hb_[:], in_=h2f[:]), reads=[h2f.s], writes=[hb_.s])
            for g4 in range(8):
                pt = pT5[n5[0] % 3]
                n5[0] += 1

                def tr(t, pt=pt, g4=g4):
                    r = None
                    for k in range(4):
                        c = g4 * 4 + k
                        r = t.transpose(out=pt[:, k, :], in_=h2f[:, c * 128:(c + 1) * 128], identity=identf[:])
                    return r
                P.op('tensor', tr, reads=[h2f.s, identf.s], writes=[pt.s])
                if g4 % 2 == 0:
                    P.op('scalar', lambda a, pt=pt, g4=g4: a.copy(out=h2T[:, g4 * 4:(g4 + 1) * 4, :], in_=pt[:]), reads=[pt.s],
                         writes=[h2T.sub(g4)])
                else:
                    P.op('vector', lambda v, pt=pt, g4=g4: v.tensor_copy(out=h2T[:, g4 * 4:(g4 + 1) * 4, :], in_=pt[:]),
                         reads=[pt.s], writes=[h2T.sub(g4)])

            def mml(t):
                r = None
                for c in range(NCH):
                    r = t.matmul(pL[:, 0:36], lhsT=h2T[:, c, :], rhs=Wr[:, c, :], start=(c == 0), stop=(c == NCH - 1))
                return r
            P.op('tensor', mml, reads=h2T.all() + [Wr.s], writes=[pL.s])
            lg, gl = R['lg'], R['lg']
            V(lambda v: v.tensor_tensor(out=R['lg'][:], in0=pL[:, 0:36], in1=brt[:], op=ALU.add), [pL.s, brt.s], ['lg'])
            V(lambda v: v.tensor_reduce(out=R['gmax'][:], in_=R['lg'][:, 0:4], axis=AX.X, op=ALU.max), ['lg'], ['gmax'])
            V(lambda v: v.tensor_scalar(out=R['ngmax'][:], in0=R['gmax'][:], scalar1=-1.0, scalar2=None, op0=ALU.mult),
              ['gmax'], ['ngmax'])
            V(lambda v: v.tensor_scalar(out=R['gmask'][:], in0=R['lg'][:, 0:4], scalar1=R['gmax'][:], scalar2=None,
                                        op0=ALU.is_equal), ['lg', 'gmax'], ['gmask'])
            P.op('scalar', lambda a: a.activation(out=R['ge'][:], in_=R['lg'][:, 0:4], func=AF.Exp, bias=R['ngmax'][:]),
                 reads=[R['lg'].s, R['ngmax'].s], writes=[R['ge'].s])
            V(lambda v: v.tensor_reduce(out=R['gsum'][:], in_=R['ge'][:], axis=AX.X, op=ALU.add), ['ge'], ['gsum'])
            V(lambda v: v.reciprocal(out=R['pg'][:], in_=R['gsum'][:]), ['gsum'], ['pg'])
            V(lambda v: v.tensor_scalar(out=R['pen'][:], in0=R['gmask'][:], scalar1=1.0, scalar2=1.0e9, op0=ALU.subtract,
                                        op1=ALU.mult), ['gmask'], ['pen'])
            V(lambda v: v.tensor_tensor(out=R['mel'][:].rearrange("p (g e) -> p g e", e=8),
                                        in0=R['lg'][:, 4:36].rearrange("p (g e) -> p g e", e=8),
                                        in1=R['pen'][:].unsqueeze(2).to_broadcast([128, 4, 8]), op=ALU.add),
              ['lg', 'pen'], ['mel'])
            V(lambda v: v.max(out=R['top8'][:], in_=R['mel'][:]), ['mel'], ['top8'])
            V(lambda v: v.tensor_scalar(out=R['sel'][:], in0=R['mel'][:], scalar1=R['top8'][:, 1:2], scalar2=None,
                                        op0=ALU.is_ge), ['mel', 'top8'], ['sel'])
            V(lambda v: v.tensor_scalar(out=R['is1'][:], in0=R['mel'][:], scalar1=R['top8'][:, 0:1], scalar2=None,
                                        op0=ALU.is_ge), ['mel', 'top8'], ['is1'])
            V(lambda v: v.tensor_tensor(out=R['is2'][:], in0=R['sel'][:], in1=R['is1'][:], op=ALU.subtract),
              ['sel', 'is1'], ['is2'])
            V(lambda v: v.tensor_copy(out=selb[:], in_=R['sel'][:]), ['sel'], [selb.s])
            V(lambda v: v.tensor_tensor(out=R['dm'][:], in0=R['top8'][:, 1:2], in1=R['top8'][:, 0:1], op=ALU.subtract),
              ['top8'], ['dm'])
            P.op('scalar', lambda a: a.activation(out=R['ew'][:], in_=R['dm'][:], func=AF.Exp), reads=[R['dm'].s],
                 writes=[R['ew'].s])
            V(lambda v: v.tensor_scalar(out=R['den'][:], in0=R['ew'][:], scalar1=1.0, scalar2=None, op0=ALU.add),
              ['ew'], ['den'])
            V(lambda v: v.reciprocal(out=R['rden'][:], in_=R['den'][:]), ['den'], ['rden'])
            V(lambda v, tt=tt: v.tensor_tensor(out=gate_t[:, 0, tt:tt + 1], in0=R['pg'][:], in1=R['rden'][:], op=ALU.mult),
              ['pg', 'rden'], [gate_t.sub((0, tt))])
            V(lambda v, tt=tt: v.tensor_tensor(out=gate_t[:, 1, tt:tt + 1], in0=gate_t[:, 0, tt:tt + 1], in1=R['ew'][:],
                                               op=ALU.mult), ['ew', gate_t.sub((0, tt))], [gate_t.sub((1, tt))])

            def mmp(t):
                t.matmul(pP[:, 0:32], lhsT=Ls_b[:], rhs=selb[:], start=True, stop=True)
                return t.matmul(pP[:, 32:64], lhsT=ones_b[:], rhs=selb[:], start=True, stop=True)
            P.op('tensor', mmp, reads=[Ls_b.s, ones_b.s, selb.s], writes=[pP.s])
            V(lambda v: v.tensor_tensor(out=R['pos'][:], in0=pP[:, 0:32], in1=cum[:], op=ALU.add), [pP.s, cum.s], ['pos'])
            V(lambda v: v.tensor_tensor(out=cum[:], in0=pP[:, 32:64], in1=cum[:], op=ALU.add), [pP.s, cum.s, 'pos'], [cum.s])
            for k, isn, en, pn in ((0, 'is1', 'e1', 'p1'), (1, 'is2', 'e2', 'p2')):
                V(lambda v, isn=isn: v.tensor_tensor(out=R['j32'][:], in0=R[isn][:], in1=eidx[:], op=ALU.mult),
                  [isn, eidx.s, 'j32'], ['j32'])
                V(lambda v, en=en: v.tensor_reduce(out=R[en][:], in_=R['j32'][:], axis=AX.X, op=ALU.add), ['j32'], [en])
                V(lambda v, isn=isn: v.tensor_tensor(out=R['j32'][:], in0=R[isn][:], in1=R['pos'][:], op=ALU.mult),
                  [isn, 'pos', 'j32'], ['j32'])
                V(lambda v, pn=pn: v.tensor_reduce(out=R[pn][:], in_=R['j32'][:], axis=AX.X, op=ALU.add), ['j32'], [pn])
                V(lambda v, pn=pn: v.tensor_scalar(out=R['ok'][:], in0=R[pn][:], scalar1=float(CAP), scalar2=None,
                                                   op0=ALU.is_lt), [pn], ['ok'])
                V(lambda v, en=en, pn=pn: v.scalar_tensor_tensor(out=R['sf'][:], in0=R[en][:], scalar=float(CAP), in1=R[pn][:],
                                                                 op0=ALU.mult, op1=ALU.add), [en, pn], ['sf'])
                V(lambda v: v.tensor_tensor(out=R['sf'][:], in0=R['sf'][:], in1=pidx[:], op=ALU.subtract), ['sf', pidx.s], ['sf'])
                V(lambda v: v.scalar_tensor_tensor(out=R['sf'][:], in0=R['sf'][:], scalar=R['ok'][:], in1=pidx[:],
                                                   op0=ALU.mult, op1=ALU.add), ['sf', 'ok', pidx.s], ['sf'])
                it = idx_t[k][tt]
                V(lambda v, it=it: v.tensor_copy(out=it[:], in_=R['sf'][:]), ['sf'], [it.s])
                P.op('gpsimd', lambda g, it=it, hb_=hb_: g.indirect_dma_start(
                    out=xg_d, out_offset=bass.IndirectOffsetOnAxis(ap=it[:, :], axis=0), in_=hb_[:, :], in_offset=None), reads=[it.s, hb_.s], dma=scs[tt % 2])
        P.flush("ph5")

    if upto >= 6:
      with ExitStack() as st:
        xgs = [sb(st, "xgs%d" % i, [128, D], BF16) for i in range(1)]
        ldxg = [P.sem("ld_xgs%d" % i) for i in range(1)]
        XgT = sb(st, "XgT", [128, NCH, CAP], BF16)
        WA = [[sb(st, "WA%d_%d" % (i, gu), [128, NCH, 512], BF16) for gu in range(2)] for i in range(2)]
        ldWA = [[P.sem("ld_WA%d_%d" % (i, gu)) for gu in range(2)] for i in range(2)]
        WD = [sb(st, "WD%d" % i, [128, 8, 512], BF16) for i in range(2)]
        ldWD = [P.sem("ld_WD%d" % i) for i in range(2)]
        actT = sb(st, "actT", [128, 8, CAP], BF16)
        sil = [sb(st, "sil%d" % i, [128, CAP], F32) for i in range(2)]
        ysg = [sb(st, "ysg%d" % i, [128, 512], BF16) for i in range(2)]
        stys = [P.sem("st_ysg%d" % i) for i in range(2)]
        pT6 = [ps(st, "pT6_%d" % i, [128, 8, 128], BF16) for i in range(2)]
        pHg = [ps(st, "pHg%d" % i, [128, 512], F32) for i in range(2)]
        pHu = [ps(st, "pHu%d" % i, [128, 512], F32) for i in range(2)]
        pY = [ps(st, "pY6_%d" % i, [128, 512], F32) for i in range(2)]
        cnt6 = [0]
        nxg = [0]
        nH = [0]
        nY = [0]
        nD = [0]

        def load_A(e, qf):
            for gu, wsrc in ((0, w_gate), (1, w_up)):
                W = WA[qf % 2][gu]
                P.op('gpsimd', lambda g, W=W, wsrc=wsrc: g.dma_start(
                    out=W[:], in_=wsrc[e].rearrange("(c p) n -> p c n", p=128)[:, :, qf * 512:(qf + 1) * 512]),
                    writes=[W.s], dma=ldWA[qf % 2][gu])

        def load_D(e, q):
            W = WD[nD[0] % 2]
            sem = ldWD[nD[0] % 2]
            nD[0] += 1
            P.op('gpsimd', lambda g, W=W: g.dma_start(
                out=W[:], in_=w_down[e].rearrange("(c p) n -> p c n", p=128)[:, :, q * 512:(q + 1) * 512]),
                writes=[W.s], dma=sem)
            return W

        def load_xg(e, t2):
            xg = xgs[0]
            sem = ldxg[0]
            nxg[0] += 1
            r0 = e * CAP + t2 * 128
            P.op('sync', lambda s_: s_.dma_start(out=xg[:], in_=xg_d[r0:r0 + 128, :]), writes=[xg.s], dma=sem)
            return xg

        load_A(0, 0)
        load_A(0, 1)
        for e in range(NE):
            for t2 in range(CAP // 128):
                xg = load_xg(e, t2)
                transpose_tile(xg, lambda c0, nb, t2=t2: XgT[:, c0:c0 + nb, t2 * 128:(t2 + 1) * 128], D, pT6, cnt6, identb,
                               lambda g8, t2=t2: XgT.sub((t2, g8)))
            WDs = [load_D(e, 0), load_D(e, 1)]
            for hf in range(2):
                Wg, Wu = WA[hf % 2]
                for fb in range(4):
                    phs = (pHg[nH[0] % 2], pHu[nH[0] % 2])
                    sl = sil[nH[0] % 2]
                    nH[0] += 1
                    for gu, W in ((0, Wg), (1, Wu)):
                        def mmA(t, ph=phs[gu], W=W, fb=fb):
                            r = None
                            for c in range(NCH):
                                r = t.matmul(ph[:, 0:CAP], lhsT=W[:, c, fb * 128:(fb + 1) * 128],
                                             rhs=XgT[:, c, :], start=(c == 0), stop=(c == NCH - 1))
                            return r
                        P.op('tensor', mmA, reads=[W.s] + XgT.all(), writes=[phs[gu].s])
                    P.op('scalar', lambda a, sl=sl, ph=phs[0]: a.activation(out=sl[:], in_=ph[:, 0:CAP], func=AF.Silu),
                         reads=[phs[0].s], writes=[sl.s])
                    fc = hf * 4 + fb
                    P.op('vector', lambda v, sl=sl, ph=phs[1], fc=fc: v.tensor_tensor(out=actT[:, fc, :], in0=sl[:], in1=ph[:, 0:CAP],
                                                                                     op=ALU.mult),
                         reads=[sl.s, phs[1].s], writes=[actT.sub(fc)])
                if e + 1 < NE:
                    load_A(e + 1, hf)
            for q in range(8):
                Wd = WDs[q] if q < 2 else load_D(e, q)
                for t2 in range(CAP // 128):
                    py = pY[nY[0] % 2]
                    ys_ = ysg[nY[0] % 2]
                    sem = stys[nY[0] % 2]
                    nY[0] += 1

                    def mmB(t, py=py, Wd=Wd, t2=t2):
                        r = None
                        for c in range(8):
                            r = t.matmul(py[:], lhsT=actT[:, c, t2 * 128:(t2 + 1) * 128], rhs=Wd[:, c, :],
                                         start=(c == 0), stop=(c == 7))
                        return r
                    P.op('tensor', mmB, reads=[Wd.s] + actT.all(), writes=[py.s])
                    if nY[0] % 2 == 0:
                        P.op('scalar', lambda a, ys_=ys_, py=py: a.copy(out=ys_[:], in_=py[:]), reads=[py.s], writes=[ys_.s])
                    else:
                        P.op('vector', lambda v, ys_=ys_, py=py: v.tensor_copy(out=ys_[:], in_=py[:]), reads=[py.s],
                             writes=[ys_.s])
                    r0 = e * CAP + t2 * 128
                    c0 = q * 512
                    P.op('sync', lambda s_, ys_=ys_, r0=r0, c0=c0: s_.dma_start(out=y_d[r0:r0 + 128, c0:c0 + 512], in_=ys_[:]),
                         reads=[ys_.s], dma=sem)
        P.flush("ph6")

    if upto >= 7:
      with ExitStack() as st:
        fnb = bcast_load(st, "fnb", fnorm_w[0:1, :], D)
        x1t = [sb(st, "x1u%d" % i, [128, D], F32) for i in range(2)]
        y1t = [sb(st, "y1t%d" % i, [128, D], BF16) for i in range(2)]
        y2t = [sb(st, "y2t%d" % i, [128, D], BF16) for i in range(2)]
        ot = [sb(st, "ot%d" % i, [128, D], F32) for i in range(2)]
        junk7 = sb(st, "junk7", [128, D], BF16)
        ss7, sd7, rs7 = (sb(st, n, [128, 1], F32) for n in ("ss7", "sd7", "rs7"))
        ldx = [P.sem("ld_x1u%d" % i) for i in range(2)]
        ldy1 = [P.sem("ld_y1t%d" % i) for i in range(2)]
        ldy2 = [P.sem("ld_y2t%d" % i) for i in range(2)]
        sto7 = [P.sem("st_ot%d" % i) for i in range(2)]

        def load7(tt):
            i = tt % 2
            P.op('sync', lambda e: e.dma_start(out=x1t[i][:], in_=x1_d[tt * 128:(tt + 1) * 128, :]), writes=[x1t[i].s], dma=ldx[i])
            for k, yt, sem in ((0, y1t[i], ldy1[i]), (1, y2t[i], ldy2[i])):
                it = idx_t[k][tt]
                P.op('gpsimd', lambda g, yt=yt, it=it: g.indirect_dma_start(
                    out=yt[:, :], out_offset=None, in_=y_d, in_offset=bass.IndirectOffsetOnAxis(ap=it[:, :], axis=0)), reads=[it.s], writes=[yt.s], dma=sem)
        load7(0)
        for tt in range(16):
            if tt + 1 < 16:
                load7(tt + 1)
            i = tt % 2
            xt, ya, yb, o = x1t[i], y1t[i], y2t[i], ot[i]
            P.op('vector', lambda v, xt=xt, ya=ya, tt=tt: v.scalar_tensor_tensor(
                out=xt[:], in0=ya[:], scalar=gate_t[:, 0, tt:tt + 1], in1=xt[:], op0=ALU.mult, op1=ALU.add),
                reads=[xt.s, ya.s] + gate_t.all(), writes=[xt.s])
            P.op('vector', lambda v, xt=xt, yb=yb, tt=tt: v.scalar_tensor_tensor(
                out=xt[:], in0=yb[:], scalar=gate_t[:, 1, tt:tt + 1], in1=xt[:], op0=ALU.mult, op1=ALU.add),
                reads=[xt.s, yb.s] + gate_t.all(), writes=[xt.s])
            rmsnorm_tile(xt, fnb, o, junk7, ss7, sd7, rs7, D)
            P.op('sync', lambda e, o=o, tt=tt: e.dma_start(out=out_d[tt * 128:(tt + 1) * 128, :], in_=o[:]), reads=[o.s],
                 dma=sto7[i])
        P.flush("ph7")
    else:
        if P.q['vector'] or P.q['sync'] or P.q['tensor']:
            P.flush("tail")
    return nc, top


def _t5_bucket_np(rel):
    half, max_exact = 16, 8
    n = np.abs(rel)
    large = max_exact + (np.log(np.maximum(n, 1).astype(np.float32) / max_exact)
                         / math.log(128 / max_exact) * (half - max_exact)).astype(np.int32)
    large = np.minimum(large, half - 1)
    return np.where(rel > 0, half, 0) + np.where(n < max_exact, n, large)


def _band_index(rev):
    r = np.arange(-1, 5)[:, None, None]
    k = np.arange(128)[None, :, None]
    q = np.arange(512)[None, None, :]
    rel = r * 128 + k - q
    if rev:
        rel = -rel
    return _t5_bucket_np(rel.astype(np.int32))


_NC_CACHE = {}


def kernel(x, rel_bias, norm1_w, w_in, lambda_q1, lambda_k1, lambda_q2, lambda_k2, subln_w, conv_w, conv_b,
           dt_bias_f, dt_bias_b, a_log_f, a_log_b, d_skip, ssm_norm_w, w_out, norm2_w, w_group_router,
           b_group_router, w_expert_router, b_expert_router, w_gate, w_up, w_down, final_norm_w, _upto=7, _debug=False,
           _cores=8):
    f = np.float32
    x = np.asarray(x, f)
    key = (_upto, _debug)
    if key not in _NC_CACHE:
        _NC_CACHE[key] = build_nc(_upto, _debug)
    nc = _NC_CACHE[key][0]
    w_in0 = np.asarray(w_in, f)[0]
    w_main = np.ascontiguousarray(w_in0[:, :12288])
    w_dt_n = np.ascontiguousarray(w_in0[:, 12288:12352])
    w_dt_r = np.ascontiguousarray(np.concatenate([w_in0[:, 12320:12352], w_in0[:, 12288:12320]], axis=1))
    rb = np.asarray(rel_bias, f)
    shared = {
        "w_main": w_main,
        "norm1_w": np.asarray(norm1_w, f).reshape(1, D),
        "lamv": np.stack([np.asarray(v, f)[0] for v in (lambda_q1, lambda_k1, lambda_q2, lambda_k2)]),
        "subln_w": np.asarray(subln_w, f).reshape(1, 256),
        "convb": np.ascontiguousarray(np.asarray(conv_b, f)[0].reshape(32, 128).T),
        "d_skip": np.asarray(d_skip, f).reshape(1, 32),
        "ssm_norm_w": np.asarray(ssm_norm_w, f).reshape(1, 2048),
    }
    if _upto >= 4:
        shared["w_out"] = np.ascontiguousarray(np.asarray(w_out, f)[0])
    if _upto >= 5:
        shared["norm2_w"] = np.asarray(norm2_w, f).reshape(1, D)
        shared["w_rt"] = np.ascontiguousarray(np.concatenate([np.asarray(w_group_router, f)[0],
                                                              np.asarray(w_expert_router, f)[0]], axis=1))
        shared["b_rt"] = np.concatenate([np.asarray(b_group_router, f)[0], np.asarray(b_expert_router, f)[0]]).reshape(1, 36)
    if _upto >= 6:
        shared["w_gate"] = np.ascontiguousarray(np.asarray(w_gate, f)[0])
        shared["w_up"] = np.ascontiguousarray(np.asarray(w_up, f)[0])
        shared["w_down"] = np.ascontiguousarray(np.asarray(w_down, f)[0])
    if _upto >= 7:
        shared["final_norm_w"] = np.asarray(final_norm_w, f).reshape(1, D)
    cw = np.asarray(conv_w, f)[0]
    per_kind = []
    for rev in (0, 1):
        bidx = _band_index(rev)
        band = np.ascontiguousarray(np.transpose(rb[bidx], (3, 0, 1, 2)))
        far = (rb[15], rb[31]) if not rev else (rb[31], rb[15])
        cfar = np.stack([far[0], far[1]], axis=1).reshape(1, 16)
        cfar = np.ascontiguousarray(np.broadcast_to(cfar, (128, 16)))
        cwk = cw[::-1] if rev else cw
        convw = np.ascontiguousarray(np.transpose(cwk.reshape(5, 32, 128), (2, 1, 0)))
        if not rev:
            dtb = np.concatenate([np.asarray(dt_bias_f, f)[0], np.asarray(dt_bias_b, f)[0]])
            al = np.concatenate([np.asarray(a_log_f, f)[0], np.asarray(a_log_b, f)[0]])
        else:
            dtb = np.concatenate([np.asarray(dt_bias_b, f)[0], np.asarray(dt_bias_f, f)[0]])
            al = np.concatenate([np.asarray(a_log_b, f)[0], np.asarray(a_log_f, f)[0]])
        per_kind.append({"band": band, "cfar": cfar, "convw": convw, "dt_bias": dtb.reshape(1, 64),
                         "a_log": al.reshape(1, 64), "w_dt": w_dt_r if rev else w_dt_n})
    in_maps = []
    for core in range(_cores):
        b, half = core // 2, core % 2
        xs = x[b] if half == 0 else x[b, ::-1]
        m = dict(shared)
        m.update(per_kind[half])
        m["xs"] = np.ascontiguousarray(xs)
        in_maps.append(m)
    res = run_bass_kernel_spmd(nc, in_maps, core_ids=list(range(_cores)))
    if _debug or _upto < 7:
        return res
    out = np.empty((4, 4096, D), f)
    for core in range(_cores):
        b, half = core // 2, core % 2
        o = np.asarray(res.results[core]["out"])
        if half == 0:
            out[b, :TO] = o
        else:
            out[b, TO:] = o[::-1]
    return out
```

```python
import math
from contextlib import ExitStack
import numpy as np
import concourse.bass as bass
import concourse.mybir as mybir
from concourse.bass_utils import run_bass_kernel_spmd

F32 = mybir.dt.float32
BF16 = mybir.dt.bfloat16
I32 = mybir.dt.int32
AF = mybir.ActivationFunctionType
ALU = mybir.AluOpType
AX = mybir.AxisListType

T = 4096
TO = 2048
D = 4096
NCH = 32
EPS = 1e-6
SCALE = 128 ** -0.5
LAM_INIT = 0.8 - 0.6 * math.exp(-0.3 * 0)
NE = 32
CAP = 512
NSLOT = NE * CAP
DFF = 1024
ENG = ['tensor', 'vector', 'scalar', 'gpsimd', 'sync']


class Sem:
    def __init__(self, h, name):
        self.h = h
        self.n = 0
        self.name = name


class Slot:
    def __init__(self, name=''):
        self.name = name
        self.wr = None
        self.rd = []


class Prog:
    def __init__(self, nc, stack):
        self.nc = nc
        self.stack = stack
        self.q = {e: [] for e in ENG}
        self.waited = {e: {} for e in ENG}
        self.sems = {}
        self.dma_evs = {e: [] for e in ENG}
        self.phase = 0
        self.engsem = {}
        self.new_phase_sems()

    def _raw_sem(self, name):
        if name not in self.sems:
            h = self.stack.enter_context(self.nc.semaphore(name))
            self.sems[name] = Sem(h, name)
        return self.sems[name]

    def sem(self, name):
        if not hasattr(self, 'pmap'):
            self.pmap = {}
        if name not in self.pmap:
            self.pmap[name] = self._raw_sem('dma%d' % len(self.pmap))
        return self.pmap[name]

    def new_phase_sems(self):
        self.pmap = {}
        self.semslot = {}
        if not self.engsem or max(s.n for s in self.engsem.values()) > 20000:
            self.gen = getattr(self, 'gen', -1) + 1
            self.engsem = {e: self._raw_sem('g%d_%s' % (self.gen, e)) for e in ENG}

    def op(self, eng, fn, reads=(), writes=(), dma=None, nsig=1, rw=()):
        waits = []
        writes = list(writes) + list(rw)
        for s in reads:
            if s.wr is not None:
                waits.append(s.wr)
        for s in writes:
            if s.wr is not None:
                waits.append(s.wr)
            waits.extend(s.rd)
        if dma is not None:
            sem, unit = dma, 16
            key_ = (list(writes) + list(reads))[0].name.split('/')[0] if (list(writes) + list(reads)) else None
            ss_ = self.__dict__.setdefault('semslot', {})
            if ss_.get(sem.name, key_) != key_:
                print("WARNING: DMA semaphore", sem.name, "shared by slots", ss_[sem.name], key_)
            ss_[sem.name] = key_
        else:
            sem, unit = self.engsem[eng], 1
        sem.n += unit * nsig
        assert sem.n < 60000, sem.name
        ev = (sem, sem.n, eng)
        w2 = []
        for (ws, wv, weng) in waits:
            if eng == 'tensor' and weng == 'tensor' and ws is self.engsem['tensor']:
                continue
            if self.waited[eng].get(ws.name, 0) >= wv:
                continue
            self.waited[eng][ws.name] = wv
            w2.append((ws, wv))
        self.q[eng].append((w2, fn, sem, unit, nsig))
        if dma is not None:
            self.dma_evs[eng].append(ev)
        for s in reads:
            s.rd.append(ev)
        for s in writes:
            s.wr = ev
            s.rd = []
        return ev

    def flush(self, name):
        for eng in ENG:
            best = {}
            for (s, v, _) in self.dma_evs[eng]:
                best[s.name] = (s, max(v, best.get(s.name, (s, 0))[1]))
            w = [(s, v) for (s, v) in best.values() if self.waited[eng].get(s.name, 0) < v]
            for (s, v) in w:
                self.waited[eng][s.name] = v
            if w:
                self.q[eng].append((w, None, None, 0, 0))
            self.dma_evs[eng] = []
        with self.nc.Block(name) as blk:
            for en in ENG:
                ops = self.q[en]
                if not ops:
                    continue

                def body(e, ops=ops):
                    for (waits, fn, sem, unit, nsig) in ops:
                        for (ws, wv) in waits:
                            e.wait_ge(ws.h, wv)
                        if fn is None:
                            continue
                        r = fn(e)
                        lst = list(r) if isinstance(r, (list, tuple)) else [r]
                        assert len(lst) == nsig, (len(lst), nsig)
                        for ins in lst:
                            ins.then_inc(sem.h, unit)
                getattr(blk, en)(body)
        self.q = {e: [] for e in ENG}
        self.phase += 1
        self.new_phase_sems()


class Tile:
    def __init__(self, t, name):
        self.t = t
        self.s = Slot(name)

    def __getitem__(self, k):
        return self.t[k]

    def sub(self, key):
        if not hasattr(self, '_sub'):
            self._sub = {}
        if key not in self._sub:
            self._sub[key] = Slot('%s/%s' % (self.s.name, key))
        return self._sub[key]

    def all(self):
        return [self.s] + list(getattr(self, '_sub', {}).values())


def build_nc(upto=99, debug=False):
    nc = bass.Bass("TRN2", target_bir_lowering=False)
    top = ExitStack()
    P = Prog(nc, top)

    def din(name, shape, dt=F32):
        return nc.dram_tensor(name, list(shape), dt, kind="ExternalInput").ap()

    def dscr(name, shape, dt, out=False):
        kind = "ExternalOutput" if (out and debug) else "Internal"
        return nc.dram_tensor(name, list(shape), dt, kind=kind).ap()

    x_d = din("xs", [T, D])
    w_main = din("w_main", [D, 12288])
    w_dt = din("w_dt", [D, 64])
    norm1_w = din("norm1_w", [1, D])
    band_d = din("band", [8, 6, 128, 512])
    cfar_d = din("cfar", [128, 16])
    lam_d = din("lamv", [4, 128])
    subln_d = din("subln_w", [1, 256])
    convw_d = din("convw", [128, 32, 5])
    convb_d = din("convb", [128, 32])
    dtb_d = din("dt_bias", [1, 64])
    alog_d = din("a_log", [1, 64])
    dskip_d = din("d_skip", [1, 32])
    ssmn_d = din("ssm_norm_w", [1, 2048])
    if upto >= 4:
        w_out = din("w_out", [D, D])
    if upto >= 5:
        norm2_w = din("norm2_w", [1, D])
        w_rt = din("w_rt", [D, 36])
        b_rt = din("b_rt", [1, 36])
    if upto >= 6:
        w_gate = din("w_gate", [NE, D, DFF])
        w_up = din("w_up", [NE, D, DFF])
        w_down = din("w_down", [NE, DFF, D])
    if upto >= 7:
        fnorm_w = din("final_norm_w", [1, D])
        out_d = nc.dram_tensor("out", [TO, D], F32, kind="ExternalOutput").ap()

    hT_d = dscr("hT_d", [8, 128, NCH, 512], BF16)
    qT_d = dscr("qT_d", [16, 128, TO], BF16, out=True)
    kT_d = dscr("kT_d", [16, 128, T], BF16, out=True)
    v_d = dscr("v_d", [T, 2048], BF16, out=True)
    zs_d = dscr("zs_d", [TO, 2048], BF16, out=True)
    uT_d = dscr("uT_d", [32, 128, T], BF16, out=True)
    dt_d = dscr("dt_d", [T, 64], F32, out=True)
    mix_d = dscr("mix_d", [TO, D], BF16, out=True)
    xs_d = dscr("xsc_d", [T, 2048], BF16, out=True)
    bt_d = dscr("bt_d", [T, 1024], BF16)
    yf_d = dscr("yf_d", [TO, 2048], F32, out=True)
    x1_d = dscr("x1_d", [TO, D], F32, out=True)
    xg_d = dscr("xg_d", [NSLOT + 128, D], BF16)
    y_d = dscr("y_d", [NSLOT + 128, D], BF16)

    def sb(stack, name, shape, dt):
        return Tile(stack.enter_context(nc.sbuf_tensor("s_" + name, list(shape), dt)), name)

    def ps(stack, name, shape, dt=F32):
        return Tile(stack.enter_context(nc.psum_tensor("p_" + name, list(shape), dt)), name)

    idf = sb(top, "idf", [128, 128], F32)
    identb = sb(top, "identb", [128, 128], BF16)
    identf = sb(top, "identf", [128, 128], F32)
    U_f = sb(top, "U_f", [128, 128], F32)
    L_f = sb(top, "L_f", [128, 128], F32)
    ones_f = sb(top, "ones_f", [128, 128], F32)
    mneg_f = sb(top, "mneg_f", [128, 128], F32)
    mneg_b = sb(top, "mneg_b", [128, 128], F32)
    Ls_b = sb(top, "Ls_b", [128, 128], BF16)
    ones_b = sb(top, "ones_b", [128, 128], BF16)

    P.op('gpsimd', lambda g: g.iota(idf[:], pattern=[[1, 128]], base=0, channel_multiplier=-1,
                                    allow_small_or_imprecise_dtypes=True), writes=[idf.s])
    P.op('vector', lambda v: v.tensor_single_scalar(out=identb[:], in_=idf[:], scalar=0.0, op=ALU.is_equal),
         reads=[idf.s], writes=[identb.s])
    P.op('vector', lambda v: v.tensor_single_scalar(out=identf[:], in_=idf[:], scalar=0.0, op=ALU.is_equal),
         reads=[idf.s], writes=[identf.s])
    P.op('vector', lambda v: v.tensor_single_scalar(out=U_f[:], in_=idf[:], scalar=0.0, op=ALU.is_ge),
         reads=[idf.s], writes=[U_f.s])
    P.op('vector', lambda v: v.tensor_single_scalar(out=L_f[:], in_=idf[:], scalar=0.0, op=ALU.is_le),
         reads=[idf.s], writes=[L_f.s])
    P.op('vector', lambda v: v.tensor_single_scalar(out=Ls_b[:], in_=idf[:], scalar=0.0, op=ALU.is_gt),
         reads=[idf.s], writes=[Ls_b.s])
    P.op('vector', lambda v: v.memset(ones_f[:], 1.0), writes=[ones_f.s])
    P.op('vector', lambda v: v.memset(ones_b[:], 1.0), writes=[ones_b.s])
    P.op('vector', lambda v: v.tensor_scalar(out=mneg_f[:], in0=U_f[:], scalar1=1.0, scalar2=30000.0,
                                             op0=ALU.subtract, op1=ALU.mult), reads=[U_f.s], writes=[mneg_f.s])
    P.op('vector', lambda v: v.tensor_scalar(out=mneg_b[:], in0=L_f[:], scalar1=1.0, scalar2=30000.0,
                                             op0=ALU.subtract, op1=ALU.mult), reads=[L_f.s], writes=[mneg_b.s])

    def bcast_load(stack, name, src_row, n, eng='sync'):
        t = sb(stack, name, [128, n], F32)
        P.op(eng, lambda e: e.dma_start(out=t[:], in_=src_row.partition_broadcast(128)), writes=[t.s],
             dma=P.sem('ld_' + name))
        return t

    def rmsnorm_tile(xt, wb, hb, junk, ss, std, rstd, ncols):
        P.op('scalar', lambda a: a.activation(out=junk[:], in_=xt[:], func=AF.Square, accum_out=ss[:]),
             reads=[xt.s], writes=[junk.s, ss.s])
        P.op('scalar', lambda a: a.activation(out=std[:], in_=ss[:], func=AF.Sqrt, scale=1.0 / ncols, bias=EPS),
             reads=[ss.s], writes=[std.s])
        P.op('vector', lambda v: v.reciprocal(out=rstd[:], in_=std[:]), reads=[std.s], writes=[rstd.s])
        P.op('vector', lambda v: v.scalar_tensor_tensor(out=hb[:], in0=xt[:], scalar=rstd[:], in1=wb[:],
                                                        op0=ALU.mult, op1=ALU.mult),
             reads=[xt.s, rstd.s, wb.s], writes=[hb.s])

    def transpose_tile(src, dst_fn, ncols, pTs, cnt, ident, dslot_fn):
        nblk = ncols // 128
        for g8 in range((nblk + 7) // 8):
            nb = min(8, nblk - g8 * 8)
            pt = pTs[cnt[0] % len(pTs)]
            cnt[0] += 1

            def tr(t, pt=pt, g8=g8, nb=nb):
                r = None
                for k in range(nb):
                    c = g8 * 8 + k
                    r = t.transpose(out=pt[:, k, :], in_=src[:, c * 128:(c + 1) * 128], identity=ident[:])
                return r
            P.op('tensor', tr, reads=[src.s, ident.s], writes=[pt.s])
            if cnt[0] % 2 == 0:
                P.op('scalar', lambda a, pt=pt, g8=g8, nb=nb: a.copy(out=dst_fn(g8 * 8, nb), in_=pt[:, 0:nb, :]),
                     reads=[pt.s], writes=[dslot_fn(g8)])
            else:
                P.op('vector', lambda v, pt=pt, g8=g8, nb=nb: v.tensor_copy(out=dst_fn(g8 * 8, nb), in_=pt[:, 0:nb, :]),
                     reads=[pt.s], writes=[dslot_fn(g8)])

    with ExitStack() as st:
        w1b = bcast_load(st, "w1b", norm1_w[0:1, :], D)
        xts = [sb(st, "xt%d" % i, [128, D], F32) for i in range(2)]
        junk = sb(st, "junk0", [128, D], BF16)
        hbs = [sb(st, "hb%d" % i, [128, D], BF16) for i in range(2)]
        hTs = [sb(st, "hTs%d" % i, [128, NCH, 512], BF16) for i in range(2)]
        ss = [sb(st, "ss%d" % i, [128, 1], F32) for i in range(2)]
        std = [sb(st, "std%d" % i, [128, 1], F32) for i in range(2)]
        rstd = [sb(st, "rstd%d" % i, [128, 1], F32) for i in range(2)]
        pT = [ps(st, "pT%d" % i, [128, 8, 128], BF16) for i in range(4)]
        ldx = [P.sem("ld_xt%d" % i) for i in range(2)]
        sthT = [P.sem("st_hT%d" % i) for i in range(2)]

        def load_x(i):
            xt = xts[i % 2]
            P.op('sync', lambda e: e.dma_start(out=xt[:], in_=x_d[i * 128:(i + 1) * 128, :]), writes=[xt.s],
                 dma=ldx[i % 2])
        load_x(0)
        cnt = [0]
        for i in range(32):
            if i + 1 < 32:
                load_x(i + 1)
            xt, hb = xts[i % 2], hbs[i % 2]
            rmsnorm_tile(xt, w1b, hb, junk, ss[i % 2], std[i % 2], rstd[i % 2], D)
            tb, j = i // 4, i % 4
            hT = hTs[tb % 2]
            transpose_tile(hb, lambda c0, nb, hT=hT, j=j: hT[:, c0:c0 + nb, j * 128:(j + 1) * 128], D, pT, cnt,
                           identb, lambda g, hT=hT, j=j: hT.sub((j, g)))
            if j == 3:
                P.op('sync', lambda e, hT=hT, tb=tb: e.dma_start(out=hT_d[tb], in_=hT[:]), reads=hT.all(),
                     dma=sthT[tb % 2])
        P.flush("ph0")

    with ExitStack() as st:
        Ws = [sb(st, "Wsl%d" % i, [128, NCH, 512], BF16) for i in range(2)]
        Wdt = sb(st, "Wdt", [128, NCH, 64], BF16)
        hTt = [sb(st, "hTt%d" % i, [128, NCH, 512], BF16) for i in range(2)]
        osb = [sb(st, "osb%d" % i, [128, 512], BF16) for i in range(4)]
        odt = [sb(st, "odt%d" % i, [128, 64], F32) for i in range(2)]
        pacc = [ps(st, "pacc%d" % i, [128, 512], F32) for i in range(6)]
        ldW = [P.sem("ld_W%d" % i) for i in range(2)]
        ldWdt = P.sem("ld_Wdt")
        ldh = [P.sem("ld_hTt%d" % i) for i in range(2)]
        sto = [P.sem("st_osb%d" % i) for i in range(4)]
        stdt = [P.sem("st_odt%d" % i) for i in range(2)]
        wv = w_main.rearrange("(c p) n -> p c n", p=128)
        wdtv = w_dt.rearrange("(c p) n -> p c n", p=128)
        slices = []
        for s_ in range(4):
            slices.append(('q', s_ * 512, 4))
        for s_ in range(4):
            slices.append(('k', 2048 + s_ * 512, 8))
        for s_ in range(4):
            slices.append(('v', 4096 + s_ * 512, 8))
        for s_ in range(4):
            slices.append(('z', 6144 + s_ * 512, 4))
        for s_ in range(8):
            slices.append(('u', 8192 + s_ * 512, 8))
        P.op('gpsimd', lambda g: g.dma_start(out=Wdt[:], in_=wdtv), writes=[Wdt.s], dma=ldWdt)

        def load_W(si):
            W = Ws[si % 2]
            c0 = slices[si][1]
            P.op('gpsimd', lambda g: g.dma_start(out=W[:], in_=wv[:, :, c0:c0 + 512]), writes=[W.s], dma=ldW[si % 2])
        its = [(si, tb) for si in range(len(slices)) for tb in range(slices[si][2])]

        def load_h(k):
            si, tb = its[k]
            h = hTt[k % 2]
            P.op('sync', lambda e: e.dma_start(out=h[:], in_=hT_d[tb]), writes=[h.s], dma=ldh[k % 2])
        load_W(0)
        load_h(0)
        npa = [0]
        nos = [0]
        nev = [0]

        def evac(pa, ob, silu=False):
            nev[0] += 1
            if silu:
                P.op('scalar', lambda a: a.activation(out=ob[:], in_=pa[:], func=AF.Silu), reads=[pa.s], writes=[ob.s])
            elif nev[0] % 2 == 0:
                P.op('scalar', lambda a: a.copy(out=ob[:], in_=pa[:]), reads=[pa.s], writes=[ob.s])
            else:
                P.op('vector', lambda v: v.tensor_copy(out=ob[:], in_=pa[:]), reads=[pa.s], writes=[ob.s])

        for k, (si, tb) in enumerate(its):
            kind, c0, ntb = slices[si]
            if tb == 0 and si + 1 < len(slices):
                load_W(si + 1)
            if k + 1 < len(its):
                load_h(k + 1)
            W, h = Ws[si % 2], hTt[k % 2]
            t0 = tb * 512
            if kind in ('q', 'k', 'u'):
                for j in range(4):
                    pa = pacc[npa[0] % 6]
                    npa[0] += 1

                    def mm(t, pa=pa, W=W, h=h, j=j):
                        r = None
                        for c in range(NCH):
                            r = t.matmul(pa[:], lhsT=W[:, c, j * 128:(j + 1) * 128], rhs=h[:, c, :],
                                         start=(c == 0), stop=(c == NCH - 1))
                        return r
                    P.op('tensor', mm, reads=[W.s, h.s], writes=[pa.s])
                    ob = osb[nos[0] % 4]
                    osem = sto[nos[0] % 4]
                    nos[0] += 1
                    evac(pa, ob)
                    blk = (c0 - {'q': 0, 'k': 2048, 'u': 8192}[kind]) // 128 + j
                    dst = {'q': qT_d, 'k': kT_d, 'u': uT_d}[kind]
                    P.op('gpsimd', lambda g, ob=ob, dst=dst, blk=blk, t0=t0: g.dma_start(
                        out=dst[blk, :, t0:t0 + 512], in_=ob[:]), reads=[ob.s], dma=osem)
            else:
                for t4 in range(4):
                    pa = pacc[npa[0] % 6]
                    npa[0] += 1

                    def mm(t, pa=pa, W=W, h=h, t4=t4):
                        r = None
                        for c in range(NCH):
                            r = t.matmul(pa[:], lhsT=h[:, c, t4 * 128:(t4 + 1) * 128], rhs=W[:, c, :],
                                         start=(c == 0), stop=(c == NCH - 1))
                        return r
                    P.op('tensor', mm, reads=[W.s, h.s], writes=[pa.s])
                    ob = osb[nos[0] % 4]
                    osem = sto[nos[0] % 4]
                    nos[0] += 1
                    evac(pa, ob, silu=(kind == 'z'))
                    cc = c0 - {'v': 4096, 'z': 6144}[kind]
                    dst = {'v': v_d, 'z': zs_d}[kind]
                    r0 = t0 + t4 * 128
                    P.op('gpsimd', lambda g, ob=ob, dst=dst, cc=cc, r0=r0: g.dma_start(
                        out=dst[r0:r0 + 128, cc:cc + 512], in_=ob[:]), reads=[ob.s], dma=osem)
            if kind == 'v' and c0 == 4096:
                for t4 in range(4):
                    pa = pacc[npa[0] % 6]
                    npa[0] += 1

                    def mm(t, pa=pa, h=h, t4=t4):
                        r = None
                        for c in range(NCH):
                            r = t.matmul(pa[:, 0:64], lhsT=h[:, c, t4 * 128:(t4 + 1) * 128], rhs=Wdt[:, c, :],
                                         start=(c == 0), stop=(c == NCH - 1))
                        return r
                    P.op('tensor', mm, reads=[Wdt.s, h.s], writes=[pa.s])
                    od = odt[t4 % 2]
                    P.op('vector', lambda v, od=od, pa=pa: v.tensor_copy(out=od[:], in_=pa[:, 0:64]), reads=[pa.s],
                         writes=[od.s])
                    r0 = t0 + t4 * 128
                    P.op('gpsimd', lambda g, od=od, r0=r0: g.dma_start(out=dt_d[r0:r0 + 128, :], in_=od[:]),
                         reads=[od.s], dma=stdt[t4 % 2])
        P.flush("ph1")
    if upto >= 2:
      with ExitStack() as st:
        qTs = [[sb(st, "qTs%d_%d" % (i, m), [128, TO], BF16) for m in range(2)] for i in range(2)]
        kTs = [[sb(st, "kTs%d_%d" % (i, m), [128, T], BF16) for m in range(2)] for i in range(2)]
        vs = [sb(st, "vs%d" % i, [128, 32, 257], BF16) for i in range(2)]
        bands = [sb(st, "band%d" % i, [128, 6, 512], F32) for i in range(2)]
        cfar = sb(st, "cfar", [128, 16], F32)
        lamv = [bcast_load(st, "lamv%d" % i, lam_d[i:i + 1, :], 128) for i in range(4)]
        sublnb = bcast_load(st, "sublnb", subln_d[0:1, :], 256)
        ldq = [[P.sem("ld_q%d_%d" % (i, m)) for m in range(2)] for i in range(2)]
        ldk = [[P.sem("ld_k%d_%d" % (i, m)) for m in range(2)] for i in range(2)]
        ldv = [P.sem("ld_v%d" % i) for i in range(2)]
        ldb = [P.sem("ld_band%d" % i) for i in range(2)]
        P.op('sync', lambda e: e.dma_start(out=cfar[:], in_=cfar_d), writes=[cfar.s], dma=P.sem("ld_cfar"))
        for i in range(2):
            P.op('vector', lambda v, i=i: v.memset(vs[i][:, :, 256:257], 1.0), writes=[vs[i].sub('ones')])
        lj = sb(st, "lj", [128, 128], F32)
        d1 = sb(st, "d1", [128, 1], F32)
        d2 = sb(st, "d2", [128, 1], F32)
        nlam = sb(st, "nlam", [128, 1], F32)
        for (ia, ib, dd) in ((0, 1, d1), (2, 3, d2)):
            P.op('vector', lambda v, ia=ia, ib=ib: v.tensor_tensor(out=lj[:], in0=lamv[ia][:], in1=lamv[ib][:], op=ALU.mult),
                 reads=[lamv[ia].s, lamv[ib].s, lj.s], writes=[lj.s])
            P.op('vector', lambda v, dd=dd: v.tensor_reduce(out=dd[:], in_=lj[:], axis=AX.X, op=ALU.add), reads=[lj.s],
                 writes=[dd.s])
        P.op('scalar', lambda a: a.activation(out=d1[:], in_=d1[:], func=AF.Exp), reads=[d1.s], writes=[d1.s])
        P.op('scalar', lambda a: a.activation(out=d2[:], in_=d2[:], func=AF.Exp), reads=[d2.s], writes=[d2.s])
        P.op('vector', lambda v: v.tensor_tensor(out=nlam[:], in0=d2[:], in1=d1[:], op=ALU.subtract),
             reads=[d1.s, d2.s], writes=[nlam.s])
        P.op('vector', lambda v: v.tensor_scalar(out=nlam[:], in0=nlam[:], scalar1=-LAM_INIT, scalar2=None, op0=ALU.add),
             reads=[nlam.s], writes=[nlam.s])
        P.op('vector', lambda v: v.tensor_scalar(out=sublnb[:], in0=sublnb[:], scalar1=(1.0 - LAM_INIT), scalar2=None,
                                                 op0=ALU.mult), reads=[sublnb.s], writes=[sublnb.s])

        pS = [ps(st, "pS%d" % i, [128, 512], F32) for i in range(4)]
        pA = [ps(st, "pA%d" % i, [128, 512], F32) for i in range(4)]
        Es = [sb(st, "Es%d" % i, [128, 512], BF16) for i in range(4)]
        tmps = [sb(st, "tmpS%d" % i, [128, 512], F32) for i in range(2)]
        osb2 = [[sb(st, "o%d_%d" % (m, qb), [128, 257], F32) for qb in range(4)] for m in range(2)]
        fin = {n: [sb(st, "fin_%s%d" % (n, i), [128, 1], F32) for i in range(2)] for n in ('r0', 'r1', 'ss', 'sd', 'rs')}
        ao = [sb(st, "ao%d" % i, [128, 256], F32) for i in range(2)]
        aj = sb(st, "aj", [128, 256], F32)
        ay = [sb(st, "ay%d" % i, [128, 256], BF16) for i in range(2)]
        sty = [P.sem("st_ay%d" % i) for i in range(2)]

        def load_head(h):
            i = h % 2
            for m in range(2):
                P.op('sync', lambda e, m=m: e.dma_start(out=qTs[i][m][:], in_=qT_d[2 * h + m]), writes=[qTs[i][m].s],
                     dma=ldq[i][m], nsig=1)
                P.op('sync', lambda e, m=m: e.dma_start(out=kTs[i][m][:], in_=kT_d[2 * h + m]), writes=[kTs[i][m].s],
                     dma=ldk[i][m], nsig=1)
            P.op('sync', lambda e: e.dma_start(out=vs[i][:, :, 0:256],
                                               in_=v_d[:, h * 256:(h + 1) * 256].rearrange("(c p) e -> p c e", p=128)),
                 writes=[vs[i].s], dma=ldv[i])
            P.op('sync', lambda e: e.dma_start(out=bands[i][:], in_=band_d[h].rearrange("r k q -> k r q")),
                 writes=[bands[i].s], dma=ldb[i])
        load_head(0)
        nS = [0]
        nE = [0]
        nT = [0]
        nF = [0]
        ss4 = [sb(st, "ss4_%d" % i, [128, 4], F32) for i in range(2)]
        sd4 = [sb(st, "sd4_%d" % i, [128, 4], F32) for i in range(2)]
        rs4 = [sb(st, "rs4_%d" % i, [128, 4], F32) for i in range(2)]
        ao4 = [[sb(st, "ao4_%d_%d" % (i, qb), [128, 256], F32) for qb in range(4)] for i in range(2)]
        ay4 = [[sb(st, "ay4_%d_%d" % (i, qb), [128, 256], BF16) for qb in range(4)] for i in range(2)]
        sty4 = [[P.sem("st_ay4_%d_%d" % (i, qb)) for qb in range(4)] for i in range(2)]
        pending = []

        def make_finalize(h, qg, f):
            def fin_():
                for qb in range(4):
                    o0, o1 = osb2[0][qb], osb2[1][qb]
                    r0, r1 = fin['r0'][qb % 2], fin['r1'][qb % 2]
                    a_o = ao4[f][qb]
                    P.op('vector', lambda v, r0=r0, o0=o0: v.reciprocal(out=r0[:], in_=o0[:, 256:257]), reads=[o0.s],
                         writes=[r0.s])
                    P.op('vector', lambda v, r1=r1, o1=o1: v.reciprocal(out=r1[:], in_=o1[:, 256:257]), reads=[o1.s],
                         writes=[r1.s])
                    P.op('vector', lambda v, r1=r1: v.tensor_tensor(out=r1[:], in0=r1[:], in1=nlam[:], op=ALU.mult),
                         reads=[r1.s, nlam.s], writes=[r1.s])
                    P.op('vector', lambda v, a_o=a_o, o0=o0, r0=r0: v.tensor_scalar(
                        out=a_o[:], in0=o0[:, 0:256], scalar1=r0[:], scalar2=None, op0=ALU.mult),
                        reads=[o0.s, r0.s], writes=[a_o.s])
                    P.op('vector', lambda v, a_o=a_o, o1=o1, r1=r1: v.scalar_tensor_tensor(
                        out=a_o[:], in0=o1[:, 0:256], scalar=r1[:], in1=a_o[:], op0=ALU.mult, op1=ALU.add),
                        reads=[o1.s, r1.s, a_o.s], writes=[a_o.s])
                    P.op('gpsimd', lambda g, a_o=a_o: g.tensor_tensor(out=aj[:], in0=a_o[:], in1=a_o[:], op=ALU.mult),
                         reads=[a_o.s, aj.s], writes=[aj.s])
                    P.op('vector', lambda v, qb=qb: v.tensor_reduce(out=ss4[f][:, qb:qb + 1], in_=aj[:], axis=AX.X, op=ALU.add),
                         reads=[aj.s], writes=[ss4[f].sub(qb)])
                P.op('scalar', lambda a: a.activation(out=sd4[f][:], in_=ss4[f][:], func=AF.Sqrt, scale=1.0 / 256, bias=EPS),
                     reads=ss4[f].all(), writes=[sd4[f].s])
                P.op('vector', lambda v: v.reciprocal(out=rs4[f][:], in_=sd4[f][:]), reads=[sd4[f].s], writes=[rs4[f].s])
                for qb in range(4):
                    a_o, a_y = ao4[f][qb], ay4[f][qb]
                    P.op('vector', lambda v, a_y=a_y, a_o=a_o, qb=qb: v.scalar_tensor_tensor(
                        out=a_y[:], in0=a_o[:], scalar=rs4[f][:, qb:qb + 1], in1=sublnb[:], op0=ALU.mult, op1=ALU.mult),
                        reads=[a_o.s, rs4[f].s, sublnb.s], writes=[a_y.s])
                    q0 = qg * 512 + qb * 128
                    P.op('gpsimd', lambda g, a_y=a_y, q0=q0: g.dma_start(
                        out=mix_d[q0:q0 + 128, h * 256:(h + 1) * 256], in_=a_y[:]), reads=[a_y.s], dma=sty4[f][qb])
            return fin_

        LOOK = 3
        for h in range(8):
            if h + 1 < 8:
                load_head(h + 1)
            i = h % 2
            for qg in range(4):
                for m in range(2):
                    qT, kT, vv, bd = qTs[i][m], kTs[i][m], vs[i], bands[i]
                    pSq = {}

                    def emit_S(kc, qT=qT, kT=kT, qg=qg):
                        pS_ = pS[nS[0] % 4]
                        nS[0] += 1
                        P.op('tensor', lambda t, pS_=pS_, kc=kc: t.matmul(
                            pS_[:], lhsT=kT[:, kc * 128:(kc + 1) * 128], rhs=qT[:, qg * 512:(qg + 1) * 512],
                            start=True, stop=True), reads=[kT.s, qT.s], writes=[pS_.s])
                        pSq[kc] = pS_
                    for kc in range(LOOK):
                        emit_S(kc)
                    for kc in range(32):
                        pS_ = pSq.pop(kc)
                        E = Es[nE[0] % 4]
                        nE[0] += 1
                        r = kc - 4 * qg
                        if -1 <= r <= 4:
                            tm = tmps[nT[0] % 2]
                            nT[0] += 1
                            P.op('vector', lambda v, tm=tm, pS_=pS_, bd=bd, r=r: v.scalar_tensor_tensor(
                                out=tm[:], in0=pS_[:], scalar=SCALE, in1=bd[:, r + 1, :], op0=ALU.mult, op1=ALU.add),
                                reads=[pS_.s, bd.s], writes=[tm.s])
                            P.op('scalar', lambda a, E=E, tm=tm: a.activation(out=E[:], in_=tm[:], func=AF.Exp),
                                 reads=[tm.s], writes=[E.s])
                        else:
                            ci = 2 * h + (0 if r < -1 else 1)
                            P.op('scalar', lambda a, E=E, pS_=pS_, ci=ci: a.activation(
                                out=E[:], in_=pS_[:], func=AF.Exp, scale=SCALE, bias=cfar[:, ci:ci + 1]),
                                reads=[pS_.s, cfar.s], writes=[E.s])
                        if kc + LOOK < 32:
                            emit_S(kc + LOOK)

                        def av(t, E=E, vv=vv, kc=kc):
                            rr = None
                            for qb in range(4):
                                rr = t.matmul(pA[qb][:, 0:257], lhsT=E[:, qb * 128:(qb + 1) * 128], rhs=vv[:, kc, :],
                                              start=(kc == 0), stop=(kc == 31))
                            return rr
                        P.op('tensor', av, reads=[E.s] + vv.all(), writes=[pA[qb].s for qb in range(4)])
                        if kc == 10 and m == 0 and pending:
                            pending.pop(0)()
                    for qb in range(4):
                        o = osb2[m][qb]
                        P.op('scalar', lambda a, o=o, qb=qb: a.copy(out=o[:], in_=pA[qb][:, 0:257]), reads=[pA[qb].s],
                             writes=[o.s])
                pending.append(make_finalize(h, qg, nF[0] % 2))
                nF[0] += 1
        while pending:
            pending.pop(0)()
        P.flush("ph2")
    bT_d = dscr("bT_d", [8, 128, T], BF16)
    cT_d = dscr("cT_d", [8, 128, T], BF16)
    if upto >= 2.5:
      with ExitStack() as st:
        cw = sb(st, "cw", [128, 32, 5], F32)
        cb = sb(st, "cb", [128, 32], F32)
        P.op('sync', lambda e: e.dma_start(out=cw[:], in_=convw_d), writes=[cw.s], dma=P.sem("ld_cw"))
        P.op('sync', lambda e: e.dma_start(out=cb[:], in_=convb_d), writes=[cb.s], dma=P.sem("ld_cb"))
        ups = [sb(st, "up%d" % i, [128, T + 4], BF16) for i in range(2)]
        accs = [sb(st, "cacc%d" % i, [128, T], F32) for i in range(2)]
        cvos = [sb(st, "cvo%d" % i, [128, T], BF16) for i in range(2)]
        stg = [sb(st, "cstg%d" % i, [128, 32, 256], BF16) for i in range(2)]
        pT3 = [ps(st, "pT3_%d" % i, [128, 8, 128], BF16) for i in range(4)]
        ldu = [P.sem("ld_up%d" % i) for i in range(2)]
        stc = [P.sem("st_cvo%d" % i) for i in range(2)]
        sts = [P.sem("st_cstg%d" % i) for i in range(2)]
        for i in range(2):
            P.op('vector', lambda v, i=i: v.memset(ups[i][:, 0:2], 0.0), writes=[ups[i].sub('pl')])
            P.op('vector', lambda v, i=i: v.memset(ups[i][:, T + 2:T + 4], 0.0), writes=[ups[i].sub('pr')])

        def load_u(b):
            P.op('sync', lambda e: e.dma_start(out=ups[b % 2][:, 2:T + 2], in_=uT_d[b]), writes=[ups[b % 2].s],
                 dma=ldu[b % 2])
        load_u(0)
        cnt3 = [0]
        for b in range(32):
            if b + 1 < 32:
                load_u(b + 1)
            up, acc, cvo = ups[b % 2], accs[b % 2], cvos[b % 2]
            P.op('vector', lambda v, up=up, acc=acc, b=b: v.tensor_scalar(
                out=acc[:], in0=up[:, 0:T], scalar1=cw[:, b, 0:1], scalar2=None, op0=ALU.mult),
                reads=up.all() + [cw.s], writes=[acc.s])
            for j in range(1, 5):
                P.op('vector', lambda v, up=up, acc=acc, b=b, j=j: v.scalar_tensor_tensor(
                    out=acc[:], in0=up[:, j:j + T], scalar=cw[:, b, j:j + 1], in1=acc[:], op0=ALU.mult, op1=ALU.add),
                    reads=up.all() + [cw.s, acc.s], writes=[acc.s])
            P.op('scalar', lambda a, acc=acc, cvo=cvo, b=b: a.activation(out=cvo[:], in_=acc[:], func=AF.Silu,
                                                                       bias=cb[:, b:b + 1]),
                 reads=[acc.s, cb.s], writes=[cvo.s])
            if b >= 16:
                dst = bT_d if b < 24 else cT_d
                g = (b - 16) % 8
                P.op('gpsimd', lambda e, cvo=cvo, dst=dst, g=g: e.dma_start(out=dst[g], in_=cvo[:]), reads=[cvo.s],
                     dma=stc[b % 2])
            if b < 24:
                sg = stg[(b // 2) % 2]
                half = b % 2
                transpose_tile(cvo, lambda c0, nb, sg=sg, half=half: sg[:, c0:c0 + nb, half * 128:(half + 1) * 128],
                               T, pT3, cnt3, identb, lambda g8, sg=sg, half=half: sg.sub((half, g8)))
                if half == 1:
                    if b < 16:
                        dstv = xs_d.rearrange("(c p) n -> p c n", p=128)[:, :, (b - 1) * 128:(b + 1) * 128]
                    else:
                        dstv = bt_d.rearrange("(c p) n -> p c n", p=128)[:, :, (b - 17) * 128:(b - 15) * 128]
                    P.op('sync', lambda e, sg=sg, dstv=dstv: e.dma_start(out=dstv, in_=sg[:]), reads=sg.all(),
                         dma=sts[(b // 2) % 2])
        P.flush("ph3a")

    if upto >= 3:
      with ExitStack() as st:
        BTc = [sb(st, "BTc%d" % i, [128, 8, 128], BF16) for i in range(2)]
        CTc = [sb(st, "CTc%d" % i, [128, 8, 128], BF16) for i in range(2)]
        ldBT = [P.sem("ld_BTc%d" % i) for i in range(2)]
        ldCT = [P.sem("ld_CTc%d" % i) for i in range(2)]
        dtbb = bcast_load(st, "dtbb", dtb_d[0:1, :], 64)
        Ab = bcast_load(st, "Ab", alog_d[0:1, :], 64)
        dskb = bcast_load(st, "dskb", dskip_d[0:1, :], 32)
        ssmnb = bcast_load(st, "ssmnb", ssmn_d[0:1, :], 2048)
        P.op('scalar', lambda a: a.activation(out=Ab[:], in_=Ab[:], func=AF.Exp), reads=[Ab.s], writes=[Ab.s])
        P.op('vector', lambda v: v.tensor_scalar(out=Ab[:], in0=Ab[:], scalar1=-1.0, scalar2=None, op0=ALU.mult),
             reads=[Ab.s], writes=[Ab.s])
        xsc = [sb(st, "xsc%d" % i, [128, 32, 64], BF16) for i in range(2)]
        btc = [sb(st, "btc%d" % i, [128, 1024], BF16) for i in range(2)]
        dtr = [sb(st, "dtr%d" % i, [128, 32], F32) for i in range(2)]
        yfl = [sb(st, "yfl%d" % i, [128, 2048], F32) for i in range(2)]
        zsl = [sb(st, "zsl%d" % i, [128, 2048], BF16) for i in range(2)]
        ldxs = [P.sem("ld_xsc%d" % i) for i in range(2)]
        ldbt = [P.sem("ld_btc%d" % i) for i in range(2)]
        lddt = [P.sem("ld_dtr%d" % i) for i in range(2)]
        ldyf = [P.sem("ld_yfl%d" % i) for i in range(2)]
        ldzs = [P.sem("ld_zsl%d" % i) for i in range(2)]
        sm = {n: sb(st, "sm_" + n, [128, 32], F32) for n in ('t1', 'dt', 'a', 'eacs', 'cd', 'w')}
        acst = sb(st, "acst", [128, 64], F32)
        nacs = sb(st, "nacs", [128, 32], F32)
        mneg4 = [sb(st, "mneg4_%d" % i, [128, 4, 128], F32) for i in range(2)]
        for i_, mm_ in ((0, mneg_f), (1, mneg_b)):
            P.op('vector', lambda v, i_=i_, mm_=mm_: v.tensor_copy(out=mneg4[i_][:], in_=bc(mm_[:], [128, 4, 128], 1)),
                 reads=[mm_.s], writes=[mneg4[i_].s])
        xdt = sb(st, "xdt", [128, 32, 64], BF16)
        xw = sb(st, "xw", [128, 32, 64], BF16)
        rbig = sb(st, "rbig", [128, 32, 128], F32)
        dms = [sb(st, "dm%d" % i, [128, 4, 128], F32) for i in range(2)]
        decs = [sb(st, "dec%d" % i, [128, 4, 128], F32) for i in range(2)]
        MTs = [sb(st, "MT%d" % i, [128, 4, 128], BF16) for i in range(2)]
        yos = [sb(st, "yo%d" % i, [128, 4, 64], F32) for i in range(2)]
        ysb = [sb(st, "ysb%d" % i, [128, 32, 64], F32) for i in range(2)]
        Sst = sb(st, "Sst", [128, 32, 64], F32)
        Sbf = sb(st, "Sbf", [128, 32, 64], BF16)
        sq = sb(st, "sq", [128, 8, 256], F32)
        gss = sb(st, "gss", [128, 8], F32)
        gsd = sb(st, "gsd", [128, 8], F32)
        grs = sb(st, "grs", [128, 8], F32)
        yout = [sb(st, "yout%d" % i, [128, 8, 256], BF16) for i in range(2)]
        styf = [P.sem("st_ysb%d" % i) for i in range(2)]
        styo = [P.sem("st_yout%d" % i) for i in range(2)]
        pc = ps(st, "pc", [128, 512], F32)
        pR = [ps(st, "pR%d" % i, [128, 4, 128], F32) for i in range(2)]
        pB = [ps(st, "pB%d" % i, [128, 512], F32) for i in range(2)]
        pC = [ps(st, "pC%d" % i, [128, 512], F32) for i in range(2)]

        def bc(ap, shape, axis):
            return ap.unsqueeze(axis).to_broadcast(shape)

        seq = [(0, c, True) for c in range(16)] + [(1, c, False) for c in range(31, 15, -1)] + \
              [(1, c, True) for c in range(15, -1, -1)]
        import os as _os
        if _os.environ.get("SSD_MAXK"):
            seq = seq[:int(_os.environ["SSD_MAXK"])]
        if _os.environ.get("SSD_NOY"):
            seq = [(d_, c_, False) for (d_, c_, n_) in seq]

        def load_chunk(k):
            d, c, need_y = seq[k]
            i = k % 2
            r0 = c * 128
            P.op('sync', lambda e: e.dma_start(out=xsc[i][:], in_=xs_d[r0:r0 + 128, :].rearrange("p (h e) -> p h e", e=64)),
                 writes=[xsc[i].s], dma=ldxs[i])
            P.op('sync', lambda e: e.dma_start(out=btc[i][:], in_=bt_d[r0:r0 + 128, :]), writes=[btc[i].s], dma=ldbt[i])
            P.op('sync', lambda e: e.dma_start(out=dtr[i][:], in_=dt_d[r0:r0 + 128, d * 32:(d + 1) * 32]),
                 writes=[dtr[i].s], dma=lddt[i])
            if need_y:
                P.op('sync', lambda e: e.dma_start(out=BTc[i][:], in_=bT_d[:, :, r0:r0 + 128].rearrange("g p t -> p g t")),
                     writes=[BTc[i].s], dma=ldBT[i])
                P.op('sync', lambda e: e.dma_start(out=CTc[i][:], in_=cT_d[:, :, r0:r0 + 128].rearrange("g p t -> p g t")),
                     writes=[CTc[i].s], dma=ldCT[i])
            if d == 1 and need_y:
                P.op('sync', lambda e: e.dma_start(out=yfl[i][:], in_=yf_d[r0:r0 + 128, :]), writes=[yfl[i].s], dma=ldyf[i])
                P.op('sync', lambda e: e.dma_start(out=zsl[i][:], in_=zs_d[r0:r0 + 128, :]), writes=[zsl[i].s], dma=ldzs[i])
        yf_slot = Slot('yf_dram')
        load_chunk(0)
        ng = [0]
        for k, (d, c, need_y) in enumerate(seq):
            if k == 16:
                P.op('vector', lambda v: v.memset(Sst[:], 0.0), writes=[Sst.s])
                P.op('vector', lambda v: v.memset(Sbf[:], 0.0), writes=[Sbf.s])
            if k == 0:
                P.op('vector', lambda v: v.memset(Sst[:], 0.0), writes=[Sst.s])
                P.op('vector', lambda v: v.memset(Sbf[:], 0.0), writes=[Sbf.s])
            if k + 1 < len(seq):
                load_chunk(k + 1)
            i = k % 2
            xs_, bt_, dr = xsc[i], btc[i], dtr[i]
            BT, CT = BTc[i], CTc[i]
            hc = slice(d * 32, (d + 1) * 32)
            Tri = U_f if d == 0 else L_f
            mneg = mneg_f if d == 0 else mneg_b
            t1, dt, a_, eacs, cd, w_ = (sm[n] for n in ('t1', 'dt', 'a', 'eacs', 'cd', 'w'))
            acs = acst
            P.op('vector', lambda v, dr=dr, hc=hc: v.tensor_tensor(out=t1[:], in0=dr[:], in1=dtbb[:, hc], op=ALU.add),
                 reads=[dr.s, dtbb.s], writes=[t1.s])
            P.op('scalar', lambda a: a.activation(out=t1[:], in_=t1[:], func=AF.Exp), reads=[t1.s], writes=[t1.s])
            P.op('scalar', lambda a: a.activation(out=dt[:], in_=t1[:], func=AF.Ln, bias=1.0), reads=[t1.s],
                 writes=[dt.s])
            P.op('vector', lambda v, hc=hc: v.tensor_tensor(out=a_[:], in0=dt[:], in1=Ab[:, hc], op=ALU.mult),
                 reads=[dt.s, Ab.s], writes=[a_.s])

            def mmc(t, Tri=Tri):
                t.matmul(pc[:, 0:32], lhsT=Tri[:], rhs=a_[:], start=True, stop=True)
                return t.matmul(pc[:, 32:64], lhsT=ones_f[:], rhs=a_[:], start=True, stop=True)
            P.op('tensor', mmc, reads=[Tri.s, ones_f.s, a_.s], writes=[pc.s])
            P.op('vector', lambda v: v.tensor_copy(out=acst[:], in_=pc[:, 0:64]), reads=[pc.s], writes=[acst.s])
            P.op('vector', lambda v: v.tensor_scalar(out=nacs[:], in0=acst[:, 0:32], scalar1=-1.0, scalar2=None, op0=ALU.mult),
                 reads=[acst.s], writes=[nacs.s])
            P.op('scalar', lambda a: a.activation(out=eacs[:], in_=acst[:, 0:32], func=AF.Exp), reads=[acst.s], writes=[eacs.s])
            P.op('scalar', lambda a: a.activation(out=cd[:], in_=acst[:, 32:64], func=AF.Exp), reads=[acst.s], writes=[cd.s])
            P.op('vector', lambda v: v.tensor_tensor(out=w_[:], in0=acst[:, 32:64], in1=acst[:, 0:32], op=ALU.subtract),
                 reads=[acst.s], writes=[w_.s])
            P.op('scalar', lambda a: a.activation(out=w_[:], in_=w_[:], func=AF.Exp), reads=[w_.s], writes=[w_.s])
            P.op('vector', lambda v: v.tensor_tensor(out=w_[:], in0=w_[:], in1=dt[:], op=ALU.mult), reads=[w_.s, dt.s],
                 writes=[w_.s])
            P.op('gpsimd', lambda v, xs_=xs_: v.tensor_tensor(out=xw[:], in0=xs_[:], in1=bc(w_[:], [128, 32, 64], 2),
                                                             op=ALU.mult), reads=[xs_.s, w_.s], writes=[xw.s])
            if need_y:
                P.op('gpsimd', lambda v, xs_=xs_: v.tensor_tensor(out=xdt[:], in0=xs_[:], in1=bc(dt[:], [128, 32, 64], 2),
                                                                 op=ALU.mult), reads=[xs_.s, dt.s], writes=[xdt.s])
                P.op('gpsimd', lambda v, Tri=Tri: v.tensor_tensor(out=rbig[:], in0=bc(a_[:], [128, 32, 128], 2),
                                                                  in1=bc(Tri[:], [128, 32, 128], 1), op=ALU.mult),
                     reads=[a_.s, Tri.s], writes=[rbig.s])
            ys = ysb[i]
            gctx = {}

            def stage1(g):
                j = ng[0] % 2
                ng[0] += 1
                pR_, pB_, pC_ = pR[j], pB[j], pC[j]
                dec, MT, yo = decs[j], MTs[j], yos[j]
                gctx[g] = (pB_, MT, yo)
                if need_y:
                    m4 = mneg4[d]

                    def mmR(t, pR_=pR_, g=g, m4=m4):
                        t.matmul(pR_[:], lhsT=ones_f[:], rhs=rbig[:, 4 * g:4 * g + 4, :], start=True, stop=False)
                        return t.matmul(pR_[:], lhsT=identf[:], rhs=m4[:], start=False, stop=True)
                    P.op('tensor', mmR, reads=[ones_f.s, rbig.s, identf.s, m4.s], writes=[pR_.s])
                    P.op('tensor', lambda t, pB_=pB_, g=g, BT=BT, CT=CT: t.matmul(pB_[:, 0:128], lhsT=BT[:, g, :], rhs=CT[:, g, :],
                                                                           start=True, stop=True),
                         reads=[BT.s, CT.s], rw=[pB_.s])
                    for r in range(4):
                        P.op('scalar', lambda a, dec=dec, pR_=pR_, r=r, g=g: a.activation(
                            out=dec[:, r, :], in_=pR_[:, r, :], func=AF.Exp, bias=nacs[:, 4 * g + r:4 * g + r + 1]),
                            reads=[pR_.s, nacs.s], writes=[dec.sub(r)])
                    P.op('vector', lambda v, MT=MT, dec=dec, pB_=pB_: v.tensor_tensor(
                        out=MT[:], in0=dec[:], in1=bc(pB_[:, 0:128], [128, 4, 128], 1), op=ALU.mult),
                        reads=dec.all(), writes=[MT.s], rw=[pB_.s])

                if need_y:
                    P.op('tensor', lambda t, pC_=pC_, g=g, CT=CT: t.matmul(
                        pC_[:, 0:256], lhsT=CT[:, g, :], rhs=Sbf[:, 4 * g:4 * g + 4, :], start=True, stop=True),
                        reads=[CT.s, Sbf.s, Sbf.sub(g)], rw=[pC_.s])
                    for r in range(4):
                        P.op('scalar', lambda a, yo=yo, pC_=pC_, g=g, r=r: a.activation(
                            out=yo[:, r, :], in_=pC_[:, r * 64:(r + 1) * 64], func=AF.Identity, scale=eacs[:, 4 * g + r:4 * g + r + 1]),
                            reads=[eacs.s], writes=[yo.sub(r)], rw=[pC_.s])
                P.op('tensor', lambda t, pC_=pC_, bt_=bt_, g=g: t.matmul(
                    pC_[:, 256:512], lhsT=bt_[:, g * 128:(g + 1) * 128], rhs=xw[:, 4 * g:4 * g + 4, :], start=True, stop=True),
                    reads=[bt_.s, xw.s], rw=[pC_.s])
                P.op('vector', lambda v, g=g: v.tensor_tensor(
                    out=Sst[:, 4 * g:4 * g + 4, :], in0=Sst[:, 4 * g:4 * g + 4, :],
                    in1=bc(cd[:, 4 * g:4 * g + 4], [128, 4, 64], 2), op=ALU.mult),
                    reads=[cd.s, Sst.s, Sst.sub(g)], writes=[Sst.sub(g)])
                P.op('vector', lambda v, pC_=pC_, g=g: v.tensor_tensor(
                    out=Sst[:, 4 * g:4 * g + 4, :], in0=Sst[:, 4 * g:4 * g + 4, :],
                    in1=pC_[:, 256:512].rearrange("p (r e) -> p r e", e=64), op=ALU.add),
                    reads=[Sst.sub(g)], writes=[Sst.sub(g)], rw=[pC_.s])
                P.op('scalar', lambda a, g=g: a.copy(out=Sbf[:, 4 * g:4 * g + 4, :], in_=Sst[:, 4 * g:4 * g + 4, :]),
                     reads=[Sst.sub(g)], writes=[Sbf.sub(g)])

            def stage2(g):
                pB_, MT, yo = gctx[g]
                if need_y:
                    def mmy(t, pB_=pB_, MT=MT, g=g):
                        rr = None
                        for r in range(4):
                            rr = t.matmul(pB_[:, 128 + r * 64:128 + (r + 1) * 64], lhsT=MT[:, r, :], rhs=xdt[:, 4 * g + r, :],
                                          start=True, stop=True)
                        return rr
                    P.op('tensor', mmy, reads=[MT.s, xdt.s], rw=[pB_.s])
                    P.op('vector', lambda v, ys=ys, yo=yo, pB_=pB_, g=g: v.tensor_tensor(
                        out=ys[:, 4 * g:4 * g + 4, :], in0=yo[:],
                        in1=pB_[:, 128:384].rearrange("p (r e) -> p r e", e=64), op=ALU.add),
                        reads=yo.all(), writes=[ys.sub(g)], rw=[pB_.s])

            stage1(0)
            for g in range(8):
                if g + 1 < 8:
                    stage1(g + 1)
                stage2(g)
            if need_y and d == 0:
                P.op('vector', lambda v, xs_=xs_: v.tensor_tensor(out=xdt[:], in0=xs_[:], in1=bc(dskb[:], [128, 32, 64], 2),
                                                                 op=ALU.mult), reads=[xs_.s, dskb.s, xdt.s], writes=[xdt.s])
                P.op('vector', lambda v, ys=ys: v.tensor_tensor(out=ys[:], in0=ys[:], in1=xdt[:], op=ALU.add),
                     reads=ys.all() + [xdt.s], writes=[ys.s])
                r0 = c * 128
                P.op('gpsimd', lambda e, ys=ys, r0=r0: e.dma_start(out=yf_d[r0:r0 + 128, :].rearrange("p (h e) -> p h e", e=64),
                                                                   in_=ys[:]), reads=ys.all(), dma=styf[i])
            if need_y and d == 1:
                yf_, zs_ = yfl[i], zsl[i]
                yo_ = yout[i]
                ysf = ys[:].rearrange("p h e -> p (h e)")
                P.op('gpsimd', lambda v, ys=ys, yf_=yf_: v.tensor_tensor(out=ys[:].rearrange("p h e -> p (h e)"),
                                                                         in0=ys[:].rearrange("p h e -> p (h e)"),
                                                                         in1=yf_[:], op=ALU.add),
                     reads=ys.all() + [yf_.s], writes=[ys.s])
                P.op('gpsimd', lambda v, ys=ys, zs_=zs_: v.tensor_tensor(out=ys[:].rearrange("p h e -> p (h e)"),
                                                                         in0=ys[:].rearrange("p h e -> p (h e)"),
                                                                         in1=zs_[:], op=ALU.mult),
                     reads=[ys.s, zs_.s], writes=[ys.s])
                P.op('gpsimd', lambda v, ys=ys: v.tensor_tensor(out=sq[:].rearrange("p g e -> p (g e)"),
                                                                in0=ys[:].rearrange("p h e -> p (h e)"),
                                                                in1=ys[:].rearrange("p h e -> p (h e)"), op=ALU.mult),
                     reads=[ys.s], writes=[sq.s])
                P.op('vector', lambda v: v.tensor_reduce(out=gss[:], in_=sq[:], axis=AX.X, op=ALU.add), reads=[sq.s],
                     writes=[gss.s])
                P.op('scalar', lambda a: a.activation(out=gsd[:], in_=gss[:], func=AF.Sqrt, scale=1.0 / 256, bias=EPS),
                     reads=[gss.s], writes=[gsd.s])
                P.op('vector', lambda v: v.reciprocal(out=grs[:], in_=gsd[:]), reads=[gsd.s], writes=[grs.s])
                P.op('vector', lambda v, ys=ys: v.tensor_tensor(out=sq[:], in0=ys[:].rearrange("p (g r) e -> p g (r e)", r=4),
                                                                in1=bc(grs[:], [128, 8, 256], 2), op=ALU.mult),
                     reads=[ys.s, grs.s, sq.s], writes=[sq.s])
                P.op('vector', lambda v, yo_=yo_: v.tensor_tensor(out=yo_[:].rearrange("p g e -> p (g e)"),
                                                                  in0=sq[:].rearrange("p g e -> p (g e)"),
                                                                  in1=ssmnb[:], op=ALU.mult),
                     reads=[sq.s, ssmnb.s], writes=[yo_.s])
                r0 = c * 128
                P.op('gpsimd', lambda e, yo_=yo_, r0=r0: e.dma_start(
                    out=mix_d[r0:r0 + 128, 2048:4096].rearrange("p (g e) -> p g e", e=256), in_=yo_[:]),
                    reads=[yo_.s], dma=styo[i])
            if k == 15 and len(seq) > 16:
                P.flush("ph3f")
        P.flush("ph3b")
    if upto >= 4:
      with ExitStack() as st:
        mixT = sb(st, "mixT", [128, NCH, TO], BF16)
        mts = [sb(st, "mt%d" % i, [128, D], BF16) for i in range(2)]
        pT4 = [ps(st, "pT4_%d" % i, [128, 8, 128], BF16) for i in range(2)]
        ldm = [P.sem("ld_mt%d" % i) for i in range(2)]
        cnt4 = [0]

        def load_m(i):
            P.op('sync', lambda e: e.dma_start(out=mts[i % 2][:], in_=mix_d[i * 128:(i + 1) * 128, :]), writes=[mts[i % 2].s],
                 dma=ldm[i % 2])
        load_m(0)
        for i in range(16):
            if i + 1 < 16:
                load_m(i + 1)
            transpose_tile(mts[i % 2], lambda c0, nb, i=i: mixT[:, c0:c0 + nb, i * 128:(i + 1) * 128], D, pT4, cnt4, identb,
                           lambda g8, i=i: mixT.sub((i, g8)))
        Wo = [sb(st, "Wo%d" % i, [128, NCH, 256], BF16) for i in range(2)]
        ldWo = [P.sem("ld_Wo%d" % i) for i in range(2)]
        xr = [sb(st, "xr%d" % i, [128, 256], F32) for i in range(3)]
        ldxr = [P.sem("ld_xr%d" % i) for i in range(3)]
        x1s = [sb(st, "x1s%d" % i, [128, 256], F32) for i in range(3)]
        stx1 = [P.sem("st_x1s%d" % i) for i in range(3)]
        pO4 = [ps(st, "pO4_%d" % i, [128, 512], F32) for i in range(4)]
        wov = w_out.rearrange("(c p) n -> p c n", p=128)

        def load_Wo(s_):
            P.op('gpsimd', lambda g: g.dma_start(out=Wo[s_ % 2][:], in_=wov[:, :, s_ * 256:(s_ + 1) * 256]),
                 writes=[Wo[s_ % 2].s], dma=ldWo[s_ % 2])
        load_Wo(0)
        n4 = 0
        for s_ in range(16):
            if s_ + 1 < 16:
                load_Wo(s_ + 1)
            W = Wo[s_ % 2]
            for tt in range(16):
                xr_ = xr[n4 % 3]
                x1_ = x1s[n4 % 3]
                pa = pO4[n4 % 4]
                P.op('sync', lambda e, xr_=xr_, tt=tt, s_=s_: e.dma_start(
                    out=xr_[:], in_=x_d[tt * 128:(tt + 1) * 128, s_ * 256:(s_ + 1) * 256]), writes=[xr_.s], dma=ldxr[n4 % 3])

                def mm(t, pa=pa, W=W, tt=tt):
                    r = None
                    for c in range(NCH):
                        r = t.matmul(pa[:, 0:256], lhsT=mixT[:, c, tt * 128:(tt + 1) * 128], rhs=W[:, c, :],
                                     start=(c == 0), stop=(c == NCH - 1))
                    return r
                P.op('tensor', mm, reads=[W.s] + mixT.all(), writes=[pa.s])
                P.op('vector', lambda v, x1_=x1_, pa=pa, xr_=xr_: v.tensor_tensor(out=x1_[:], in0=pa[:, 0:256], in1=xr_[:],
                                                                                  op=ALU.add),
                     reads=[pa.s, xr_.s], writes=[x1_.s])
                P.op('sync', lambda e, x1_=x1_, tt=tt, s_=s_: e.dma_start(
                    out=x1_d[tt * 128:(tt + 1) * 128, s_ * 256:(s_ + 1) * 256], in_=x1_[:]), reads=[x1_.s], dma=stx1[n4 % 3])
                n4 += 1
        P.flush("ph4")

    if upto >= 5:
        idx_t = [[sb(top, "idx%d_%d" % (k, tt), [128, 1], I32) for tt in range(16)] for k in range(2)]
        gate_t = sb(top, "gate_t", [128, 2, 16], F32)
    if upto >= 5:
      with ExitStack() as st:
        w2b = bcast_load(st, "w2b", norm2_w[0:1, :], D)
        brt = bcast_load(st, "brt", b_rt[0:1, :], 36)
        Wr = sb(st, "Wr", [128, NCH, 36], F32)
        P.op('sync', lambda e: e.dma_start(out=Wr[:], in_=w_rt.rearrange("(c p) n -> p c n", p=128)), writes=[Wr.s],
             dma=P.sem("ld_Wr"))
        eidx = sb(st, "eidx", [128, 32], F32)
        pidx = sb(st, "pidx", [128, 1], F32)
        P.op('gpsimd', lambda g: g.iota(eidx[:], pattern=[[1, 32]], base=0, channel_multiplier=0,
                                        allow_small_or_imprecise_dtypes=True), writes=[eidx.s])
        P.op('gpsimd', lambda g: g.iota(pidx[:], pattern=[[1, 1]], base=NSLOT, channel_multiplier=1,
                                        allow_small_or_imprecise_dtypes=True), writes=[pidx.s])
        cum = sb(st, "cum", [128, 32], F32)
        P.op('vector', lambda v: v.memset(cum[:], 0.0), writes=[cum.s])
        zt = sb(st, "zt", [128, D], BF16)
        P.op('gpsimd', lambda g: g.memset(zt[:], 0.0), writes=[zt.s])
        P.op('sync', lambda e: e.dma_start(out=y_d[NSLOT:NSLOT + 128, :], in_=zt[:]), reads=[zt.s], dma=P.sem("st_zt"))
        x1t = [sb(st, "x1t%d" % i, [128, D], F32) for i in range(2)]
        ldx1 = [P.sem("ld_x1t%d" % i) for i in range(2)]
        h2f = sb(st, "h2f", [128, D], F32)
        h2b = [sb(st, "h2b%d" % i, [128, D], BF16) for i in range(2)]
        junk5 = sb(st, "junk5", [128, D], BF16)
        h2T = sb(st, "h2T", [128, NCH, 128], F32)
        ss5, sd5, rs5 = (sb(st, n, [128, 1], F32) for n in ("ss5", "sd5", "rs5"))
        pT5 = [ps(st, "pT5_%d" % i, [128, 4, 128], F32) for i in range(3)]
        pL = ps(st, "pL", [128, 512], F32)
        pP = ps(st, "pP", [128, 512], F32)
        R = {n: sb(st, "R_" + n, [128, sz], F32) for n, sz in (
            ('lg', 36), ('gmax', 1), ('ngmax', 1), ('gmask', 4), ('ge', 4), ('gsum', 1), ('pg', 1), ('pen', 4), ('mel', 32),
            ('top8', 8), ('sel', 32), ('is1', 32), ('is2', 32), ('dm', 1), ('ew', 1), ('den', 1), ('rden', 1), ('e1', 1),
            ('e2', 1), ('p1', 1), ('p2', 1), ('pos', 32), ('j32', 32), ('ok', 1), ('sf', 1), ('tf', 1))}
        selb = sb(st, "selb", [128, 32], BF16)
        scs = [P.sem("sc_h2b%d" % i) for i in range(2)]

        def load_x1(i):
            P.op('sync', lambda e: e.dma_start(out=x1t[i % 2][:], in_=x1_d[i * 128:(i + 1) * 128, :]), writes=[x1t[i % 2].s],
                 dma=ldx1[i % 2])
        load_x1(0)
        n5 = [0]

        def V(fn, reads, writes):
            P.op('vector', fn, reads=[R[n].s if isinstance(n, str) else n for n in reads],
                 writes=[R[n].s if isinstance(n, str) else n for n in writes])

        for tt in range(16):
            if tt + 1 < 16:
                load_x1(tt + 1)
            xt = x1t[tt % 2]
            hb_ = h2b[tt % 2]
            rmsnorm_tile(xt, w2b, h2f, junk5, ss5, sd5, rs5, D)
            P.op('scalar', lambda a, hb_=hb_: a.copy(out=hb_[:], in_=h2f[:]), reads=[h2f.s], writes=[hb_.s])
            for g4 in range(8):
                pt = pT5[n5[0] % 3]
                n5[0] += 1

                def tr(t, pt=pt, g4=g4):
                    r = None
                    for k in range(4):
                        c = g4 * 4 + k
                        r = t.transpose(out=pt[:, k, :], in_=h2f[:, c * 128:(c + 1) * 128], identity=identf[:])
                    return r
                P.op('tensor', tr, reads=[h2f.s, identf.s], writes=[pt.s])
                if g4 % 2 == 0:
                    P.op('scalar', lambda a, pt=pt, g4=g4: a.copy(out=h2T[:, g4 * 4:(g4 + 1) * 4, :], in_=pt[:]), reads=[pt.s],
                         writes=[h2T.sub(g4)])
                else:
                    P.op('vector', lambda v, pt=pt, g4=g4: v.tensor_copy(out=h2T[:, g4 * 4:(g4 + 1) * 4, :], in_=pt[:]),
                         reads=[pt.s], writes=[h2T.sub(g4)])

            def mml(t):
                r = None
                for c in range(NCH):
                    r = t.matmul(pL[:, 0:36], lhsT=h2T[:, c, :], rhs=Wr[:, c, :], start=(c == 0), stop=(c == NCH - 1))
                return r
            P.op('tensor', mml, reads=h2T.all() + [Wr.s], writes=[pL.s])
            lg, gl = R['lg'], R['lg']
            V(lambda v: v.tensor_tensor(out=R['lg'][:], in0=pL[:, 0:36], in1=brt[:], op=ALU.add), [pL.s, brt.s], ['lg'])
            V(lambda v: v.tensor_reduce(out=R['gmax'][:], in_=R['lg'][:, 0:4], axis=AX.X, op=ALU.max), ['lg'], ['gmax'])
            V(lambda v: v.tensor_scalar(out=R['ngmax'][:], in0=R['gmax'][:], scalar1=-1.0, scalar2=None, op0=ALU.mult),
              ['gmax'], ['ngmax'])
            V(lambda v: v.tensor_scalar(out=R['gmask'][:], in0=R['lg'][:, 0:4], scalar1=R['gmax'][:], scalar2=None,
                                        op0=ALU.is_equal), ['lg', 'gmax'], ['gmask'])
            P.op('scalar', lambda a: a.activation(out=R['ge'][:], in_=R['lg'][:, 0:4], func=AF.Exp, bias=R['ngmax'][:]),
                 reads=[R['lg'].s, R['ngmax'].s], writes=[R['ge'].s])
            V(lambda v: v.tensor_reduce(out=R['gsum'][:], in_=R['ge'][:], axis=AX.X, op=ALU.add), ['ge'], ['gsum'])
            V(lambda v: v.reciprocal(out=R['pg'][:], in_=R['gsum'][:]), ['gsum'], ['pg'])
            V(lambda v: v.tensor_scalar(out=R['pen'][:], in0=R['gmask'][:], scalar1=1.0, scalar2=1.0e9, op0=ALU.subtract,
                                        op1=ALU.mult), ['gmask'], ['pen'])
            V(lambda v: v.tensor_tensor(out=R['mel'][:].rearrange("p (g e) -> p g e", e=8),
                                        in0=R['lg'][:, 4:36].rearrange("p (g e) -> p g e", e=8),
                                        in1=R['pen'][:].unsqueeze(2).to_broadcast([128, 4, 8]), op=ALU.add),
              ['lg', 'pen'], ['mel'])
            V(lambda v: v.max(out=R['top8'][:], in_=R['mel'][:]), ['mel'], ['top8'])
            V(lambda v: v.tensor_scalar(out=R['sel'][:], in0=R['mel'][:], scalar1=R['top8'][:, 1:2], scalar2=None,
                                        op0=ALU.is_ge), ['mel', 'top8'], ['sel'])
            V(lambda v: v.tensor_scalar(out=R['is1'][:], in0=R['mel'][:], scalar1=R['top8'][:, 0:1], scalar2=None,
                                        op0=ALU.is_ge), ['mel', 'top8'], ['is1'])
            V(lambda v: v.tensor_tensor(out=R['is2'][:], in0=R['sel'][:], in1=R['is1'][:], op=ALU.subtract),
              ['sel', 'is1'], ['is2'])
            V(lambda v: v.tensor_copy(out=selb[:], in_=R['sel'][:]), ['sel'], [selb.s])
            V(lambda v: v.tensor_tensor(out=R['dm'][:], in0=R['top8'][:, 1:2], in1=R['top8'][:, 0:1], op=ALU.subtract),
              ['top8'], ['dm'])
            P.op('scalar', lambda a: a.activation(out=R['ew'][:], in_=R['dm'][:], func=AF.Exp), reads=[R['dm'].s],
                 writes=[R['ew'].s])
            V(lambda v: v.tensor_scalar(out=R['den'][:], in0=R['ew'][:], scalar1=1.0, scalar2=None, op0=ALU.add),
              ['ew'], ['den'])
            V(lambda v: v.reciprocal(out=R['rden'][:], in_=R['den'][:]), ['den'], ['rden'])
            V(lambda v, tt=tt: v.tensor_tensor(out=gate_t[:, 0, tt:tt + 1], in0=R['pg'][:], in1=R['rden'][:], op=ALU.mult),
              ['pg', 'rden'], [gate_t.sub((0, tt))])
            V(lambda v, tt=tt: v.tensor_tensor(out=gate_t[:, 1, tt:tt + 1], in0=gate_t[:, 0, tt:tt + 1], in1=R['ew'][:],
                                               op=ALU.mult), ['ew', gate_t.sub((0, tt))], [gate_t.sub((1, tt))])

            def mmp(t):
                t.matmul(pP[:, 0:32], lhsT=Ls_b[:], rhs=selb[:], start=True, stop=True)
                return t.matmul(pP[:, 32:64], lhsT=ones_b[:], rhs=selb[:], start=True, stop=True)
            P.op('tensor', mmp, reads=[Ls_b.s, ones_b.s, selb.s], writes=[pP.s])
            V(lambda v: v.tensor_tensor(out=R['pos'][:], in0=pP[:, 0:32], in1=cum[:], op=ALU.add), [pP.s, cum.s], ['pos'])
            V(lambda v: v.tensor_tensor(out=cum[:], in0=pP[:, 32:64], in1=cum[:], op=ALU.add), [pP.s, cum.s, 'pos'], [cum.s])
            for k, isn, en, pn in ((0, 'is1', 'e1', 'p1'), (1, 'is2', 'e2', 'p2')):
                V(lambda v, isn=isn: v.tensor_tensor(out=R['j32'][:], in0=R[isn][:], in1=eidx[:], op=ALU.mult),
                  [isn, eidx.s, 'j32'], ['j32'])
                V(lambda v, en=en: v.tensor_reduce(out=R[en][:], in_=R['j32'][:], axis=AX.X, op=ALU.add), ['j32'], [en])
                V(lambda v, isn=isn: v.tensor_tensor(out=R['j32'][:], in0=R[isn][:], in1=R['pos'][:], op=ALU.mult),
                  [isn, 'pos', 'j32'], ['j32'])
                V(lambda v, pn=pn: v.tensor_reduce(out=R[pn][:], in_=R['j32'][:], axis=AX.X, op=ALU.add), ['j32'], [pn])
                V(lambda v, pn=pn: v.tensor_scalar(out=R['ok'][:], in0=R[pn][:], scalar1=float(CAP), scalar2=None,
                                                   op0=ALU.is_lt), [pn], ['ok'])
                V(lambda v, en=en, pn=pn: v.scalar_tensor_tensor(out=R['sf'][:], in0=R[en][:], scalar=float(CAP), in1=R[pn][:],
                                                                 op0=ALU.mult, op1=ALU.add), [en, pn], ['sf'])
                V(lambda v: v.tensor_tensor(out=R['sf'][:], in0=R['sf'][:], in1=pidx[:], op=ALU.subtract), ['sf', pidx.s], ['sf'])
                V(lambda v: v.scalar_tensor_tensor(out=R['sf'][:], in0=R['sf'][:], scalar=R['ok'][:], in1=pidx[:],
                                                   op0=ALU.mult, op1=ALU.add), ['sf', 'ok', pidx.s], ['sf'])
                it = idx_t[k][tt]
                V(lambda v, it=it: v.tensor_copy(out=it[:], in_=R['sf'][:]), ['sf'], [it.s])
                P.op('gpsimd', lambda g, it=it, hb_=hb_: g.indirect_dma_start(
                    out=xg_d, out_offset=bass.IndirectOffsetOnAxis(ap=it[:, :], axis=0), in_=hb_[:, :], in_offset=None), reads=[it.s, hb_.s], dma=scs[tt % 2])
        P.flush("ph5")

    if upto >= 6:
      with ExitStack() as st:
        xgs = [sb(st, "xgs%d" % i, [128, D], BF16) for i in range(1)]
        ldxg = [P.sem("ld_xgs%d" % i) for i in range(1)]
        XgT = sb(st, "XgT", [128, NCH, CAP], BF16)
        WA = [[sb(st, "WA%d_%d" % (i, gu), [128, NCH, 512], BF16) for gu in range(2)] for i in range(2)]
        ldWA = [[P.sem("ld_WA%d_%d" % (i, gu)) for gu in range(2)] for i in range(2)]
        WD = [sb(st, "WD%d" % i, [128, 8, 512], BF16) for i in range(2)]
        ldWD = [P.sem("ld_WD%d" % i) for i in range(2)]
        actT = sb(st, "actT", [128, 8, CAP], BF16)
        sil = [sb(st, "sil%d" % i, [128, CAP], F32) for i in range(2)]
        ysg = [sb(st, "ysg%d" % i, [128, 512], BF16) for i in range(2)]
        stys = [P.sem("st_ysg%d" % i) for i in range(2)]
        pT6 = [ps(st, "pT6_%d" % i, [128, 8, 128], BF16) for i in range(2)]
        pHg = [ps(st, "pHg%d" % i, [128, 512], F32) for i in range(2)]
        pHu = [ps(st, "pHu%d" % i, [128, 512], F32) for i in range(2)]
        pY = [ps(st, "pY6_%d" % i, [128, 512], F32) for i in range(2)]
        cnt6 = [0]
        nxg = [0]
        nH = [0]
        nY = [0]
        nD = [0]

        def load_A(e, qf):
            for gu, wsrc in ((0, w_gate), (1, w_up)):
                W = WA[qf % 2][gu]
                P.op('gpsimd', lambda g, W=W, wsrc=wsrc: g.dma_start(
                    out=W[:], in_=wsrc[e].rearrange("(c p) n -> p c n", p=128)[:, :, qf * 512:(qf + 1) * 512]),
                    writes=[W.s], dma=ldWA[qf % 2][gu])

        def load_D(e, q):
            W = WD[nD[0] % 2]
            sem = ldWD[nD[0] % 2]
            nD[0] += 1
            P.op('gpsimd', lambda g, W=W: g.dma_start(
                out=W[:], in_=w_down[e].rearrange("(c p) n -> p c n", p=128)[:, :, q * 512:(q + 1) * 512]),
                writes=[W.s], dma=sem)
            return W

        def load_xg(e, t2):
            xg = xgs[0]
            sem = ldxg[0]
            nxg[0] += 1
            r0 = e * CAP + t2 * 128
            P.op('sync', lambda s_: s_.dma_start(out=xg[:], in_=xg_d[r0:r0 + 128, :]), writes=[xg.s], dma=sem)
            return xg

        load_A(0, 0)
        load_A(0, 1)
        for e in range(NE):
            for t2 in range(CAP // 128):
                xg = load_xg(e, t2)
                transpose_tile(xg, lambda c0, nb, t2=t2: XgT[:, c0:c0 + nb, t2 * 128:(t2 + 1) * 128], D, pT6, cnt6, identb,
                               lambda g8, t2=t2: XgT.sub((t2, g8)))
            WDs = [load_D(e, 0), load_D(e, 1)]
            for hf in range(2):
                Wg, Wu = WA[hf % 2]
                for fb in range(4):
                    phs = (pHg[nH[0] % 2], pHu[nH[0] % 2])
                    sl = sil[nH[0] % 2]
                    nH[0] += 1
                    for gu, W in ((0, Wg), (1, Wu)):
                        def mmA(t, ph=phs[gu], W=W, fb=fb):
                            r = None
                            for c in range(NCH):
                                r = t.matmul(ph[:, 0:CAP], lhsT=W[:, c, fb * 128:(fb + 1) * 128],
                                             rhs=XgT[:, c, :], start=(c == 0), stop=(c == NCH - 1))
                            return r
                        P.op('tensor', mmA, reads=[W.s] + XgT.all(), writes=[phs[gu].s])
                    P.op('scalar', lambda a, sl=sl, ph=phs[0]: a.activation(out=sl[:], in_=ph[:, 0:CAP], func=AF.Silu),
                         reads=[phs[0].s], writes=[sl.s])
                    fc = hf * 4 + fb
                    P.op('vector', lambda v, sl=sl, ph=phs[1], fc=fc: v.tensor_tensor(out=actT[:, fc, :], in0=sl[:], in1=ph[:, 0:CAP],
                                                                                     op=ALU.mult),
                         reads=[sl.s, phs[1].s], writes=[actT.sub(fc)])
                if e + 1 < NE:
                    load_A(e + 1, hf)
            for q in range(8):
                Wd = WDs[q] if q < 2 else load_D(e, q)
                for t2 in range(CAP // 128):
                    py = pY[nY[0] % 2]
                    ys_ = ysg[nY[0] % 2]
                    sem = stys[nY[0] % 2]
                    nY[0] += 1

                    def mmB(t, py=py, Wd=Wd, t2=t2):
                        r = None
                        for c in range(8):
                            r = t.matmul(py[:], lhsT=actT[:, c, t2 * 128:(t2 + 1) * 128], rhs=Wd[:, c, :],
                                         start=(c == 0), stop=(c == 7))
                        return r
                    P.op('tensor', mmB, reads=[Wd.s] + actT.all(), writes=[py.s])
                    if nY[0] % 2 == 0:
                        P.op('scalar', lambda a, ys_=ys_, py=py: a.copy(out=ys_[:], in_=py[:]), reads=[py.s], writes=[ys_.s])
                    else:
                        P.op('vector', lambda v, ys_=ys_, py=py: v.tensor_copy(out=ys_[:], in_=py[:]), reads=[py.s],
                             writes=[ys_.s])
                    r0 = e * CAP + t2 * 128
                    c0 = q * 512
                    P.op('sync', lambda s_, ys_=ys_, r0=r0, c0=c0: s_.dma_start(out=y_d[r0:r0 + 128, c0:c0 + 512], in_=ys_[:]),
                         reads=[ys_.s], dma=sem)
        P.flush("ph6")

    if upto >= 7:
      with ExitStack() as st:
        fnb = bcast_load(st, "fnb", fnorm_w[0:1, :], D)
        x1t = [sb(st, "x1u%d" % i, [128, D], F32) for i in range(2)]
        y1t = [sb(st, "y1t%d" % i, [128, D], BF16) for i in range(2)]
        y2t = [sb(st, "y2t%d" % i, [128, D], BF16) for i in range(2)]
        ot = [sb(st, "ot%d" % i, [128, D], F32) for i in range(2)]
        junk7 = sb(st, "junk7", [128, D], BF16)
        ss7, sd7, rs7 = (sb(st, n, [128, 1], F32) for n in ("ss7", "sd7", "rs7"))
        ldx = [P.sem("ld_x1u%d" % i) for i in range(2)]
        ldy1 = [P.sem("ld_y1t%d" % i) for i in range(2)]
        ldy2 = [P.sem("ld_y2t%d" % i) for i in range(2)]
        sto7 = [P.sem("st_ot%d" % i) for i in range(2)]

        def load7(tt):
            i = tt % 2
            P.op('sync', lambda e: e.dma_start(out=x1t[i][:], in_=x1_d[tt * 128:(tt + 1) * 128, :]), writes=[x1t[i].s], dma=ldx[i])
            for k, yt, sem in ((0, y1t[i], ldy1[i]), (1, y2t[i], ldy2[i])):
                it = idx_t[k][tt]
                P.op('gpsimd', lambda g, yt=yt, it=it: g.indirect_dma_start(
                    out=yt[:, :], out_offset=None, in_=y_d, in_offset=bass.IndirectOffsetOnAxis(ap=it[:, :], axis=0)), reads=[it.s], writes=[yt.s], dma=sem)
        load7(0)
        for tt in range(16):
            if tt + 1 < 16:
                load7(tt + 1)
            i = tt % 2
            xt, ya, yb, o = x1t[i], y1t[i], y2t[i], ot[i]
            P.op('vector', lambda v, xt=xt, ya=ya, tt=tt: v.scalar_tensor_tensor(
                out=xt[:], in0=ya[:], scalar=gate_t[:, 0, tt:tt + 1], in1=xt[:], op0=ALU.mult, op1=ALU.add),
                reads=[xt.s, ya.s] + gate_t.all(), writes=[xt.s])
            P.op('vector', lambda v, xt=xt, yb=yb, tt=tt: v.scalar_tensor_tensor(
                out=xt[:], in0=yb[:], scalar=gate_t[:, 1, tt:tt + 1], in1=xt[:], op0=ALU.mult, op1=ALU.add),
                reads=[xt.s, yb.s] + gate_t.all(), writes=[xt.s])
            rmsnorm_tile(xt, fnb, o, junk7, ss7, sd7, rs7, D)
            P.op('sync', lambda e, o=o, tt=tt: e.dma_start(out=out_d[tt * 128:(tt + 1) * 128, :], in_=o[:]), reads=[o.s],
                 dma=sto7[i])
        P.flush("ph7")
    else:
        if P.q['vector'] or P.q['sync'] or P.q['tensor']:
            P.flush("tail")
    return nc, top


def _t5_bucket_np(rel):
    half, max_exact = 16, 8
    n = np.abs(rel)
    large = max_exact + (np.log(np.maximum(n, 1).astype(np.float32) / max_exact)
                         / math.log(128 / max_exact) * (half - max_exact)).astype(np.int32)
    large = np.minimum(large, half - 1)
    return np.where(rel > 0, half, 0) + np.where(n < max_exact, n, large)


def _band_index(rev):
    r = np.arange(-1, 5)[:, None, None]
    k = np.arange(128)[None, :, None]
    q = np.arange(512)[None, None, :]
    rel = r * 128 + k - q
    if rev:
        rel = -rel
    return _t5_bucket_np(rel.astype(np.int32))


_NC_CACHE = {}


def kernel(x, rel_bias, norm1_w, w_in, lambda_q1, lambda_k1, lambda_q2, lambda_k2, subln_w, conv_w, conv_b,
           dt_bias_f, dt_bias_b, a_log_f, a_log_b, d_skip, ssm_norm_w, w_out, norm2_w, w_group_router,
           b_group_router, w_expert_router, b_expert_router, w_gate, w_up, w_down, final_norm_w, _upto=7, _debug=False,
           _cores=8):
    f = np.float32
    x = np.asarray(x, f)
    key = (_upto, _debug)
    if key not in _NC_CACHE:
        _NC_CACHE[key] = build_nc(_upto, _debug)
    nc = _NC_CACHE[key][0]
    w_in0 = np.asarray(w_in, f)[0]
    w_main = np.ascontiguousarray(w_in0[:, :12288])
    w_dt_n = np.ascontiguousarray(w_in0[:, 12288:12352])
    w_dt_r = np.ascontiguousarray(np.concatenate([w_in0[:, 12320:12352], w_in0[:, 12288:12320]], axis=1))
    rb = np.asarray(rel_bias, f)
    shared = {
        "w_main": w_main,
        "norm1_w": np.asarray(norm1_w, f).reshape(1, D),
        "lamv": np.stack([np.asarray(v, f)[0] for v in (lambda_q1, lambda_k1, lambda_q2, lambda_k2)]),
        "subln_w": np.asarray(subln_w, f).reshape(1, 256),
        "convb": np.ascontiguousarray(np.asarray(conv_b, f)[0].reshape(32, 128).T),
        "d_skip": np.asarray(d_skip, f).reshape(1, 32),
        "ssm_norm_w": np.asarray(ssm_norm_w, f).reshape(1, 2048),
    }
    if _upto >= 4:
        shared["w_out"] = np.ascontiguousarray(np.asarray(w_out, f)[0])
    if _upto >= 5:
        shared["norm2_w"] = np.asarray(norm2_w, f).reshape(1, D)
        shared["w_rt"] = np.ascontiguousarray(np.concatenate([np.asarray(w_group_router, f)[0],
                                                              np.asarray(w_expert_router, f)[0]], axis=1))
        shared["b_rt"] = np.concatenate([np.asarray(b_group_router, f)[0], np.asarray(b_expert_router, f)[0]]).reshape(1, 36)
    if _upto >= 6:
        shared["w_gate"] = np.ascontiguousarray(np.asarray(w_gate, f)[0])
        shared["w_up"] = np.ascontiguousarray(np.asarray(w_up, f)[0])
        shared["w_down"] = np.ascontiguousarray(np.asarray(w_down, f)[0])
    if _upto >= 7:
        shared["final_norm_w"] = np.asarray(final_norm_w, f).reshape(1, D)
    cw = np.asarray(conv_w, f)[0]
    per_kind = []
    for rev in (0, 1):
        bidx = _band_index(rev)
        band = np.ascontiguousarray(np.transpose(rb[bidx], (3, 0, 1, 2)))
        far = (rb[15], rb[31]) if not rev else (rb[31], rb[15])
        cfar = np.stack([far[0], far[1]], axis=1).reshape(1, 16)
        cfar = np.ascontiguousarray(np.broadcast_to(cfar, (128, 16)))
        cwk = cw[::-1] if rev else cw
        convw = np.ascontiguousarray(np.transpose(cwk.reshape(5, 32, 128), (2, 1, 0)))
        if not rev:
            dtb = np.concatenate([np.asarray(dt_bias_f, f)[0], np.asarray(dt_bias_b, f)[0]])
            al = np.concatenate([np.asarray(a_log_f, f)[0], np.asarray(a_log_b, f)[0]])
        else:
            dtb = np.concatenate([np.asarray(dt_bias_b, f)[0], np.asarray(dt_bias_f, f)[0]])
            al = np.concatenate([np.asarray(a_log_b, f)[0], np.asarray(a_log_f, f)[0]])
        per_kind.append({"band": band, "cfar": cfar, "convw": convw, "dt_bias": dtb.reshape(1, 64),
                         "a_log": al.reshape(1, 64), "w_dt": w_dt_r if rev else w_dt_n})
    in_maps = []
    for core in range(_cores):
        b, half = core // 2, core % 2
        xs = x[b] if half == 0 else x[b, ::-1]
        m = dict(shared)
        m.update(per_kind[half])
        m["xs"] = np.ascontiguousarray(xs)
        in_maps.append(m)
    res = run_bass_kernel_spmd(nc, in_maps, core_ids=list(range(_cores)))
    if _debug or _upto < 7:
        return res
    out = np.empty((4, 4096, D), f)
    for core in range(_cores):
        b, half = core // 2, core % 2
        o = np.asarray(res.results[core]["out"])
        if half == 0:
            out[b, :TO] = o
        else:
            out[b, TO:] = o[::-1]
    return out
```

```python
import math
from contextlib import ExitStack
import numpy as np
import concourse.bass as bass
import concourse.mybir as mybir
from concourse.bass_utils import run_bass_kernel_spmd

F32 = mybir.dt.float32
BF16 = mybir.dt.bfloat16
I32 = mybir.dt.int32
AF = mybir.ActivationFunctionType
ALU = mybir.AluOpType
AX = mybir.AxisListType

T = 4096
TO = 2048
D = 4096
NCH = 32
EPS = 1e-6
SCALE = 128 ** -0.5
LAM_INIT = 0.8 - 0.6 * math.exp(-0.3 * 0)
NE = 32
CAP = 512
NSLOT = NE * CAP
DFF = 1024
ENG = ['tensor', 'vector', 'scalar', 'gpsimd', 'sync']


class Sem:
    def __init__(self, h, name):
        self.h = h
        self.n = 0
        self.name = name


class Slot:
    def __init__(self, name=''):
        self.name = name
        self.wr = None
        self.rd = []


class Prog:
    def __init__(self, nc, stack):
        self.nc = nc
        self.stack = stack
        self.q = {e: [] for e in ENG}
        self.waited = {e: {} for e in ENG}
        self.sems = {}
        self.dma_evs = {e: [] for e in ENG}
        self.phase = 0
        self.engsem = {}
        self.new_phase_sems()

    def _raw_sem(self, name):
        if name not in self.sems:
            h = self.stack.enter_context(self.nc.semaphore(name))
            self.sems[name] = Sem(h, name)
        return self.sems[name]

    def sem(self, name):
        if not hasattr(self, 'pmap'):
            self.pmap = {}
        if name not in self.pmap:
            self.pmap[name] = self._raw_sem('dma%d' % len(self.pmap))
        return self.pmap[name]

    def new_phase_sems(self):
        self.pmap = {}
        self.semslot = {}
        if not self.engsem or max(s.n for s in self.engsem.values()) > 20000:
            self.gen = getattr(self, 'gen', -1) + 1
            self.engsem = {e: self._raw_sem('g%d_%s' % (self.gen, e)) for e in ENG}

    def op(self, eng, fn, reads=(), writes=(), dma=None, nsig=1, rw=()):
        waits = []
        writes = list(writes) + list(rw)
        for s in reads:
            if s.wr is not None:
                waits.append(s.wr)
        for s in writes:
            if s.wr is not None:
                waits.append(s.wr)
            waits.extend(s.rd)
        if dma is not None:
            sem, unit = dma, 16
            key_ = (list(writes) + list(reads))[0].name.split('/')[0] if (list(writes) + list(reads)) else None
            ss_ = self.__dict__.setdefault('semslot', {})
            if ss_.get(sem.name, key_) != key_:
                print("WARNING: DMA semaphore", sem.name, "shared by slots", ss_[sem.name], key_)
            ss_[sem.name] = key_
        else:
            sem, unit = self.engsem[eng], 1
        sem.n += unit * nsig
        assert sem.n < 60000, sem.name
        ev = (sem, sem.n, eng)
        w2 = []
        for (ws, wv, weng) in waits:
            if eng == 'tensor' and weng == 'tensor' and ws is self.engsem['tensor']:
                continue
            if self.waited[eng].get(ws.name, 0) >= wv:
                continue
            self.waited[eng][ws.name] = wv
            w2.append((ws, wv))
        self.q[eng].append((w2, fn, sem, unit, nsig))
        if dma is not None:
            self.dma_evs[eng].append(ev)
        for s in reads:
            s.rd.append(ev)
        for s in writes:
            s.wr = ev
            s.rd = []
        return ev

    def flush(self, name):
        for eng in ENG:
            best = {}
            for (s, v, _) in self.dma_evs[eng]:
                best[s.name] = (s, max(v, best.get(s.name, (s, 0))[1]))
            w = [(s, v) for (s, v) in best.values() if self.waited[eng].get(s.name, 0) < v]
            for (s, v) in w:
                self.waited[eng][s.name] = v
            if w:
                self.q[eng].append((w, None, None, 0, 0))
            self.dma_evs[eng] = []
        with self.nc.Block(name) as blk:
            for en in ENG:
                ops = self.q[en]
                if not ops:
                    continue

                def body(e, ops=ops):
                    for (waits, fn, sem, unit, nsig) in ops:
                        for (ws, wv) in waits:
                            e.wait_ge(ws.h, wv)
                        if fn is None:
                            continue
                        r = fn(e)
                        lst = list(r) if isinstance(r, (list, tuple)) else [r]
                        assert len(lst) == nsig, (len(lst), nsig)
                        for ins in lst:
                            ins.then_inc(sem.h, unit)
                getattr(blk, en)(body)
        self.q = {e: [] for e in ENG}
        self.phase += 1
        self.new_phase_sems()


class Tile:
    def __init__(self, t, name):
        self.t = t
        self.s = Slot(name)

    def __getitem__(self, k):
        return self.t[k]

    def sub(self, key):
        if not hasattr(self, '_sub'):
            self._sub = {}
        if key not in self._sub:
            self._sub[key] = Slot('%s/%s' % (self.s.name, key))
        return self._sub[key]

    def all(self):
        return [self.s] + list(getattr(self, '_sub', {}).values())


def build_nc(upto=99, debug=False):
    nc = bass.Bass("TRN2", target_bir_lowering=False)
    top = ExitStack()
    P = Prog(nc, top)

    def din(name, shape, dt=F32):
        return nc.dram_tensor(name, list(shape), dt, kind="ExternalInput").ap()

    def dscr(name, shape, dt, out=False):
        kind = "ExternalOutput" if (out and debug) else "Internal"
        return nc.dram_tensor(name, list(shape), dt, kind=kind).ap()

    x_d = din("xs", [T, D])
    w_main = din("w_main", [D, 12288])
    w_dt = din("w_dt", [D, 64])
    norm1_w = din("norm1_w", [1, D])
    band_d = din("band", [8, 6, 128, 512])
    cfar_d = din("cfar", [128, 16])
    lam_d = din("lamv", [4, 128])
    subln_d = din("subln_w", [1, 256])
    convw_d = din("convw", [128, 32, 5])
    convb_d = din("convb", [128, 32])
    dtb_d = din("dt_bias", [1, 64])
    alog_d = din("a_log", [1, 64])
    dskip_d = din("d_skip", [1, 32])
    ssmn_d = din("ssm_norm_w", [1, 2048])
    if upto >= 4:
        w_out = din("w_out", [D, D])
    if upto >= 5:
        norm2_w = din("norm2_w", [1, D])
        w_rt = din("w_rt", [D, 36])
        b_rt = din("b_rt", [1, 36])
    if upto >= 6:
        w_gate = din("w_gate", [NE, D, DFF])
        w_up = din("w_up", [NE, D, DFF])
        w_down = din("w_down", [NE, DFF, D])
    if upto >= 7:
        fnorm_w = din("final_norm_w", [1, D])
        out_d = nc.dram_tensor("out", [TO, D], F32, kind="ExternalOutput").ap()

    hT_d = dscr("hT_d", [8, 128, NCH, 512], BF16)
    qT_d = dscr("qT_d", [16, 128, TO], BF16, out=True)
    kT_d = dscr("kT_d", [16, 128, T], BF16, out=True)
    v_d = dscr("v_d", [T, 2048], BF16, out=True)
    zs_d = dscr("zs_d", [TO, 2048], BF16, out=True)
    uT_d = dscr("uT_d", [32, 128, T], BF16, out=True)
    dt_d = dscr("dt_d", [T, 64], F32, out=True)
    mix_d = dscr("mix_d", [TO, D], BF16, out=True)
    xs_d = dscr("xsc_d", [T, 2048], BF16, out=True)
    bt_d = dscr("bt_d", [T, 1024], BF16)
    yf_d = dscr("yf_d", [TO, 2048], F32, out=True)
    x1_d = dscr("x1_d", [TO, D], F32, out=True)
    xg_d = dscr("xg_d", [NSLOT + 128, D], BF16)
    y_d = dscr("y_d", [NSLOT + 128, D], BF16)

    def sb(stack, name, shape, dt):
        return Tile(stack.enter_context(nc.sbuf_tensor("s_" + name, list(shape), dt)), name)

    def ps(stack, name, shape, dt=F32):
        return Tile(stack.enter_context(nc.psum_tensor("p_" + name, list(shape), dt)), name)

    idf = sb(top, "idf", [128, 128], F32)
    identb = sb(top, "identb", [128, 128], BF16)
    identf = sb(top, "identf", [128, 128], F32)
    U_f = sb(top, "U_f", [128, 128], F32)
    L_f = sb(top, "L_f", [128, 128], F32)
    ones_f = sb(top, "ones_f", [128, 128], F32)
    mneg_f = sb(top, "mneg_f", [128, 128], F32)
    mneg_b = sb(top, "mneg_b", [128, 128], F32)
    Ls_b = sb(top, "Ls_b", [128, 128], BF16)
    ones_b = sb(top, "ones_b", [128, 128], BF16)

    P.op('gpsimd', lambda g: g.iota(idf[:], pattern=[[1, 128]], base=0, channel_multiplier=-1,
                                    allow_small_or_imprecise_dtypes=True), writes=[idf.s])
    P.op('vector', lambda v: v.tensor_single_scalar(out=identb[:], in_=idf[:], scalar=0.0, op=ALU.is_equal),
         reads=[idf.s], writes=[identb.s])
    P.op('vector', lambda v: v.tensor_single_scalar(out=identf[:], in_=idf[:], scalar=0.0, op=ALU.is_equal),
         reads=[idf.s], writes=[identf.s])
    P.op('vector', lambda v: v.tensor_single_scalar(out=U_f[:], in_=idf[:], scalar=0.0, op=ALU.is_ge),
         reads=[idf.s], writes=[U_f.s])
    P.op('vector', lambda v: v.tensor_single_scalar(out=L_f[:], in_=idf[:], scalar=0.0, op=ALU.is_le),
         reads=[idf.s], writes=[L_f.s])
    P.op('vector', lambda v: v.tensor_single_scalar(out=Ls_b[:], in_=idf[:], scalar=0.0, op=ALU.is_gt),
         reads=[idf.s], writes=[Ls_b.s])
    P.op('vector', lambda v: v.memset(ones_f[:], 1.0), writes=[ones_f.s])
    P.op('vector', lambda v: v.memset(ones_b[:], 1.0), writes=[ones_b.s])
    P.op('vector', lambda v: v.tensor_scalar(out=mneg_f[:], in0=U_f[:], scalar1=1.0, scalar2=30000.0,
                                             op0=ALU.subtract, op1=ALU.mult), reads=[U_f.s], writes=[mneg_f.s])
    P.op('vector', lambda v: v.tensor_scalar(out=mneg_b[:], in0=L_f[:], scalar1=1.0, scalar2=30000.0,
                                             op0=ALU.subtract, op1=ALU.mult), reads=[L_f.s], writes=[mneg_b.s])

    def bcast_load(stack, name, src_row, n, eng='sync'):
        t = sb(stack, name, [128, n], F32)
        P.op(eng, lambda e: e.dma_start(out=t[:], in_=src_row.partition_broadcast(128)), writes=[t.s],
             dma=P.sem('ld_' + name))
        return t

    def rmsnorm_tile(xt, wb, hb, junk, ss, std, rstd, ncols):
        P.op('scalar', lambda a: a.activation(out=junk[:], in_=xt[:], func=AF.Square, accum_out=ss[:]),
             reads=[xt.s], writes=[junk.s, ss.s])
        P.op('scalar', lambda a: a.activation(out=std[:], in_=ss[:], func=AF.Sqrt, scale=1.0 / ncols, bias=EPS),
             reads=[ss.s], writes=[std.s])
        P.op('vector', lambda v: v.reciprocal(out=rstd[:], in_=std[:]), reads=[std.s], writes=[rstd.s])
        P.op('vector', lambda v: v.scalar_tensor_tensor(out=hb[:], in0=xt[:], scalar=rstd[:], in1=wb[:],
                                                        op0=ALU.mult, op1=ALU.mult),
             reads=[xt.s, rstd.s, wb.s], writes=[hb.s])

    def transpose_tile(src, dst_fn, ncols, pTs, cnt, ident, dslot_fn):
        nblk = ncols // 128
        for g8 in range((nblk + 7) // 8):
            nb = min(8, nblk - g8 * 8)
            pt = pTs[cnt[0] % len(pTs)]
            cnt[0] += 1

            def tr(t, pt=pt, g8=g8, nb=nb):
                r = None
                for k in range(nb):
                    c = g8 * 8 + k
                    r = t.transpose(out=pt[:, k, :], in_=src[:, c * 128:(c + 1) * 128], identity=ident[:])
                return r
            P.op('tensor', tr, reads=[src.s, ident.s], writes=[pt.s])
            if cnt[0] % 2 == 0:
                P.op('scalar', lambda a, pt=pt, g8=g8, nb=nb: a.copy(out=dst_fn(g8 * 8, nb), in_=pt[:, 0:nb, :]),
                     reads=[pt.s], writes=[dslot_fn(g8)])
            else:
                P.op('vector', lambda v, pt=pt, g8=g8, nb=nb: v.tensor_copy(out=dst_fn(g8 * 8, nb), in_=pt[:, 0:nb, :]),
                     reads=[pt.s], writes=[dslot_fn(g8)])

    with ExitStack() as st:
        w1b = bcast_load(st, "w1b", norm1_w[0:1, :], D)
        xts = [sb(st, "xt%d" % i, [128, D], F32) for i in range(2)]
        junk = sb(st, "junk0", [128, D], BF16)
        hbs = [sb(st, "hb%d" % i, [128, D], BF16) for i in range(2)]
        hTs = [sb(st, "hTs%d" % i, [128, NCH, 512], BF16) for i in range(2)]
        ss = [sb(st, "ss%d" % i, [128, 1], F32) for i in range(2)]
        std = [sb(st, "std%d" % i, [128, 1], F32) for i in range(2)]
        rstd = [sb(st, "rstd%d" % i, [128, 1], F32) for i in range(2)]
        pT = [ps(st, "pT%d" % i, [128, 8, 128], BF16) for i in range(4)]
        ldx = [P.sem("ld_xt%d" % i) for i in range(2)]
        sthT = [P.sem("st_hT%d" % i) for i in range(2)]

        def load_x(i):
            xt = xts[i % 2]
            P.op('sync', lambda e: e.dma_start(out=xt[:], in_=x_d[i * 128:(i + 1) * 128, :]), writes=[xt.s],
                 dma=ldx[i % 2])
        load_x(0)
        cnt = [0]
        for i in range(32):
            if i + 1 < 32:
                load_x(i + 1)
            xt, hb = xts[i % 2], hbs[i % 2]
            rmsnorm_tile(xt, w1b, hb, junk, ss[i % 2], std[i % 2], rstd[i % 2], D)
            tb, j = i // 4, i % 4
            hT = hTs[tb % 2]
            transpose_tile(hb, lambda c0, nb, hT=hT, j=j: hT[:, c0:c0 + nb, j * 128:(j + 1) * 128], D, pT, cnt,
                           identb, lambda g, hT=hT, j=j: hT.sub((j, g)))
            if j == 3:
                P.op('sync', lambda e, hT=hT, tb=tb: e.dma_start(out=hT_d[tb], in_=hT[:]), reads=hT.all(),
                     dma=sthT[tb % 2])
        P.flush("ph0")

    with ExitStack() as st:
        Ws = [sb(st, "Wsl%d" % i, [128, NCH, 512], BF16) for i in range(2)]
        Wdt = sb(st, "Wdt", [128, NCH, 64], BF16)
        hTt = [sb(st, "hTt%d" % i, [128, NCH, 512], BF16) for i in range(2)]
        osb = [sb(st, "osb%d" % i, [128, 512], BF16) for i in range(4)]
        odt = [sb(st, "odt%d" % i, [128, 64], F32) for i in range(2)]
        pacc = [ps(st, "pacc%d" % i, [128, 512], F32) for i in range(6)]
        ldW = [P.sem("ld_W%d" % i) for i in range(2)]
        ldWdt = P.sem("ld_Wdt")
        ldh = [P.sem("ld_hTt%d" % i) for i in range(2)]
        sto = [P.sem("st_osb%d" % i) for i in range(4)]
        stdt = [P.sem("st_odt%d" % i) for i in range(2)]
        wv = w_main.rearrange("(c p) n -> p c n", p=128)
        wdtv = w_dt.rearrange("(c p) n -> p c n", p=128)
        slices = []
        for s_ in range(4):
            slices.append(('q', s_ * 512, 4))
        for s_ in range(4):
            slices.append(('k', 2048 + s_ * 512, 8))
        for s_ in range(4):
            slices.append(('v', 4096 + s_ * 512, 8))
        for s_ in range(4):
            slices.append(('z', 6144 + s_ * 512, 4))
        for s_ in range(8):
            slices.append(('u', 8192 + s_ * 512, 8))
        P.op('gpsimd', lambda g: g.dma_start(out=Wdt[:], in_=wdtv), writes=[Wdt.s], dma=ldWdt)

        def load_W(si):
            W = Ws[si % 2]
            c0 = slices[si][1]
            P.op('gpsimd', lambda g: g.dma_start(out=W[:], in_=wv[:, :, c0:c0 + 512]), writes=[W.s], dma=ldW[si % 2])
        its = [(si, tb) for si in range(len(slices)) for tb in range(slices[si][2])]

        def load_h(k):
            si, tb = its[k]
            h = hTt[k % 2]
            P.op('sync', lambda e: e.dma_start(out=h[:], in_=hT_d[tb]), writes=[h.s], dma=ldh[k % 2])
        load_W(0)
        load_h(0)
        npa = [0]
        nos = [0]
        nev = [0]

        def evac(pa, ob, silu=False):
            nev[0] += 1
            if silu:
                P.op('scalar', lambda a: a.activation(out=ob[:], in_=pa[:], func=AF.Silu), reads=[pa.s], writes=[ob.s])
            elif nev[0] % 2 == 0:
                P.op('scalar', lambda a: a.copy(out=ob[:], in_=pa[:]), reads=[pa.s], writes=[ob.s])
            else:
                P.op('vector', lambda v: v.tensor_copy(out=ob[:], in_=pa[:]), reads=[pa.s], writes=[ob.s])

        for k, (si, tb) in enumerate(its):
            kind, c0, ntb = slices[si]
            if tb == 0 and si + 1 < len(slices):
                load_W(si + 1)
            if k + 1 < len(its):
                load_h(k + 1)
            W, h = Ws[si % 2], hTt[k % 2]
            t0 = tb * 512
            if kind in ('q', 'k', 'u'):
                for j in range(4):
                    pa = pacc[npa[0] % 6]
                    npa[0] += 1

                    def mm(t, pa=pa, W=W, h=h, j=j):
                        r = None
                        for c in range(NCH):
                            r = t.matmul(pa[:], lhsT=W[:, c, j * 128:(j + 1) * 128], rhs=h[:, c, :],
                                         start=(c == 0), stop=(c == NCH - 1))
                        return r
                    P.op('tensor', mm, reads=[W.s, h.s], writes=[pa.s])
                    ob = osb[nos[0] % 4]
                    osem = sto[nos[0] % 4]
                    nos[0] += 1
                    evac(pa, ob)
                    blk = (c0 - {'q': 0, 'k': 2048, 'u': 8192}[kind]) // 128 + j
                    dst = {'q': qT_d, 'k': kT_d, 'u': uT_d}[kind]
                    P.op('gpsimd', lambda g, ob=ob, dst=dst, blk=blk, t0=t0: g.dma_start(
                        out=dst[blk, :, t0:t0 + 512], in_=ob[:]), reads=[ob.s], dma=osem)
            else:
                for t4 in range(4):
                    pa = pacc[npa[0] % 6]
                    npa[0] += 1

                    def mm(t, pa=pa, W=W, h=h, t4=t4):
                        r = None
                        for c in range(NCH):
                            r = t.matmul(pa[:], lhsT=h[:, c, t4 * 128:(t4 + 1) * 128], rhs=W[:, c, :],
                                         start=(c == 0), stop=(c == NCH - 1))
                        return r
                    P.op('tensor', mm, reads=[W.s, h.s], writes=[pa.s])
                    ob = osb[nos[0] % 4]
                    osem = sto[nos[0] % 4]
                    nos[0] += 1
                    evac(pa, ob, silu=(kind == 'z'))
                    cc = c0 - {'v': 4096, 'z': 6144}[kind]
                    dst = {'v': v_d, 'z': zs_d}[kind]
                    r0 = t0 + t4 * 128
                    P.op('gpsimd', lambda g, ob=ob, dst=dst, cc=cc, r0=r0: g.dma_start(
                        out=dst[r0:r0 + 128, cc:cc + 512], in_=ob[:]), reads=[ob.s], dma=osem)
            if kind == 'v' and c0 == 4096:
                for t4 in range(4):
                    pa = pacc[npa[0] % 6]
                    npa[0] += 1

                    def mm(t, pa=pa, h=h, t4=t4):
                        r = None
                        for c in range(NCH):
                            r = t.matmul(pa[:, 0:64], lhsT=h[:, c, t4 * 128:(t4 + 1) * 128], rhs=Wdt[:, c, :],
                                         start=(c == 0), stop=(c == NCH - 1))
                        return r
                    P.op('tensor', mm, reads=[Wdt.s, h.s], writes=[pa.s])
                    od = odt[t4 % 2]
                    P.op('vector', lambda v, od=od, pa=pa: v.tensor_copy(out=od[:], in_=pa[:, 0:64]), reads=[pa.s],
                         writes=[od.s])
                    r0 = t0 + t4 * 128
                    P.op('gpsimd', lambda g, od=od, r0=r0: g.dma_start(out=dt_d[r0:r0 + 128, :], in_=od[:]),
                         reads=[od.s], dma=stdt[t4 % 2])
        P.flush("ph1")
    if upto >= 2:
      with ExitStack() as st:
        qTs = [[sb(st, "qTs%d_%d" % (i, m), [128, TO], BF16) for m in range(2)] for i in range(2)]
        kTs = [[sb(st, "kTs%d_%d" % (i, m), [128, T], BF16) for m in range(2)] for i in range(2)]
        vs = [sb(st, "vs%d" % i, [128, 32, 257], BF16) for i in range(2)]
        bands = [sb(st, "band%d" % i, [128, 6, 512], F32) for i in range(2)]
        cfar = sb(st, "cfar", [128, 16], F32)
        lamv = [bcast_load(st, "lamv%d" % i, lam_d[i:i + 1, :], 128) for i in range(4)]
        sublnb = bcast_load(st, "sublnb", subln_d[0:1, :], 256)
        ldq = [[P.sem("ld_q%d_%d" % (i, m)) for m in range(2)] for i in range(2)]
        ldk = [[P.sem("ld_k%d_%d" % (i, m)) for m in range(2)] for i in range(2)]
        ldv = [P.sem("ld_v%d" % i) for i in range(2)]
        ldb = [P.sem("ld_band%d" % i) for i in range(2)]
        P.op('sync', lambda e: e.dma_start(out=cfar[:], in_=cfar_d), writes=[cfar.s], dma=P.sem("ld_cfar"))
        for i in range(2):
            P.op('vector', lambda v, i=i: v.memset(vs[i][:, :, 256:257], 1.0), writes=[vs[i].sub('ones')])
        lj = sb(st, "lj", [128, 128], F32)
        d1 = sb(st, "d1", [128, 1], F32)
        d2 = sb(st, "d2", [128, 1], F32)
        nlam = sb(st, "nlam", [128, 1], F32)
        for (ia, ib, dd) in ((0, 1, d1), (2, 3, d2)):
            P.op('vector', lambda v, ia=ia, ib=ib: v.tensor_tensor(out=lj[:], in0=lamv[ia][:], in1=lamv[ib][:], op=ALU.mult),
                 reads=[lamv[ia].s, lamv[ib].s, lj.s], writes=[lj.s])
            P.op('vector', lambda v, dd=dd: v.tensor_reduce(out=dd[:], in_=lj[:], axis=AX.X, op=ALU.add), reads=[lj.s],
                 writes=[dd.s])
        P.op('scalar', lambda a: a.activation(out=d1[:], in_=d1[:], func=AF.Exp), reads=[d1.s], writes=[d1.s])
        P.op('scalar', lambda a: a.activation(out=d2[:], in_=d2[:], func=AF.Exp), reads=[d2.s], writes=[d2.s])
        P.op('vector', lambda v: v.tensor_tensor(out=nlam[:], in0=d2[:], in1=d1[:], op=ALU.subtract),
             reads=[d1.s, d2.s], writes=[nlam.s])
        P.op('vector', lambda v: v.tensor_scalar(out=nlam[:], in0=nlam[:], scalar1=-LAM_INIT, scalar2=None, op0=ALU.add),
             reads=[nlam.s], writes=[nlam.s])
        P.op('vector', lambda v: v.tensor_scalar(out=sublnb[:], in0=sublnb[:], scalar1=(1.0 - LAM_INIT), scalar2=None,
                                                 op0=ALU.mult), reads=[sublnb.s], writes=[sublnb.s])

        pS = [ps(st, "pS%d" % i, [128, 512], F32) for i in range(4)]
        pA = [ps(st, "pA%d" % i, [128, 512], F32) for i in range(4)]
        Es = [sb(st, "Es%d" % i, [128, 512], BF16) for i in range(4)]
        tmps = [sb(st, "tmpS%d" % i, [128, 512], F32) for i in range(2)]
        osb2 = [[sb(st, "o%d_%d" % (m, qb), [128, 257], F32) for qb in range(4)] for m in range(2)]
        fin = {n: [sb(st, "fin_%s%d" % (n, i), [128, 1], F32) for i in range(2)] for n in ('r0', 'r1', 'ss', 'sd', 'rs')}
        ao = [sb(st, "ao%d" % i, [128, 256], F32) for i in range(2)]
        aj = sb(st, "aj", [128, 256], F32)
        ay = [sb(st, "ay%d" % i, [128, 256], BF16) for i in range(2)]
        sty = [P.sem("st_ay%d" % i) for i in range(2)]

        def load_head(h):
            i = h % 2
            for m in range(2):
                P.op('sync', lambda e, m=m: e.dma_start(out=qTs[i][m][:], in_=qT_d[2 * h + m]), writes=[qTs[i][m].s],
                     dma=ldq[i][m], nsig=1)
                P.op('sync', lambda e, m=m: e.dma_start(out=kTs[i][m][:], in_=kT_d[2 * h + m]), writes=[kTs[i][m].s],
                     dma=ldk[i][m], nsig=1)
            P.op('sync', lambda e: e.dma_start(out=vs[i][:, :, 0:256],
                                               in_=v_d[:, h * 256:(h + 1) * 256].rearrange("(c p) e -> p c e", p=128)),
                 writes=[vs[i].s], dma=ldv[i])
            P.op('sync', lambda e: e.dma_start(out=bands[i][:], in_=band_d[h].rearrange("r k q -> k r q")),
                 writes=[bands[i].s], dma=ldb[i])
        load_head(0)
        nS = [0]
        nE = [0]
        nT = [0]
        nF = [0]
        ss4 = [sb(st, "ss4_%d" % i, [128, 4], F32) for i in range(2)]
        sd4 = [sb(st, "sd4_%d" % i, [128, 4], F32) for i in range(2)]
        rs4 = [sb(st, "rs4_%d" % i, [128, 4], F32) for i in range(2)]
        ao4 = [[sb(st, "ao4_%d_%d" % (i, qb), [128, 256], F32) for qb in range(4)] for i in range(2)]
        ay4 = [[sb(st, "ay4_%d_%d" % (i, qb), [128, 256], BF16) for qb in range(4)] for i in range(2)]
        sty4 = [[P.sem("st_ay4_%d_%d" % (i, qb)) for qb in range(4)] for i in range(2)]
        pending = []

        def make_finalize(h, qg, f):
            def fin_():
                for qb in range(4):
                    o0, o1 = osb2[0][qb], osb2[1][qb]
                    r0, r1 = fin['r0'][qb % 2], fin['r1'][qb % 2]
                    a_o = ao4[f][qb]
                    P.op('vector', lambda v, r0=r0, o0=o0: v.reciprocal(out=r0[:], in_=o0[:, 256:257]), reads=[o0.s],
                         writes=[r0.s])
                    P.op('vector', lambda v, r1=r1, o1=o1: v.reciprocal(out=r1[:], in_=o1[:, 256:257]), reads=[o1.s],
                         writes=[r1.s])
                    P.op('vector', lambda v, r1=r1: v.tensor_tensor(out=r1[:], in0=r1[:], in1=nlam[:], op=ALU.mult),
                         reads=[r1.s, nlam.s], writes=[r1.s])
                    P.op('vector', lambda v, a_o=a_o, o0=o0, r0=r0: v.tensor_scalar(
                        out=a_o[:], in0=o0[:, 0:256], scalar1=r0[:], scalar2=None, op0=ALU.mult),
                        reads=[o0.s, r0.s], writes=[a_o.s])
                    P.op('vector', lambda v, a_o=a_o, o1=o1, r1=r1: v.scalar_tensor_tensor(
                        out=a_o[:], in0=o1[:, 0:256], scalar=r1[:], in1=a_o[:], op0=ALU.mult, op1=ALU.add),
                        reads=[o1.s, r1.s, a_o.s], writes=[a_o.s])
                    P.op('gpsimd', lambda g, a_o=a_o: g.tensor_tensor(out=aj[:], in0=a_o[:], in1=a_o[:], op=ALU.mult),
                         reads=[a_o.s, aj.s], writes=[aj.s])
                    P.op('vector', lambda v, qb=qb: v.tensor_reduce(out=ss4[f][:, qb:qb + 1], in_=aj[:], axis=AX.X, op=ALU.add),
                         reads=[aj.s], writes=[ss4[f].sub(qb)])
                P.op('scalar', lambda a: a.activation(out=sd4[f][:], in_=ss4[f][:], func=AF.Sqrt, scale=1.0 / 256, bias=EPS),
                     reads=ss4[f].all(), writes=[sd4[f].s])
                P.op('vector', lambda v: v.reciprocal(out=rs4[f][:], in_=sd4[f][:]), reads=[sd4[f].s], writes=[rs4[f].s])
                for qb in range(4):
                    a_o, a_y = ao4[f][qb], ay4[f][qb]
                    P.op('vector', lambda v, a_y=a_y, a_o=a_o, qb=qb: v.scalar_tensor_tensor(
                        out=a_y[:], in0=a_o[:], scalar=rs4[f][:, qb:qb + 1], in1=sublnb[:], op0=ALU.mult, op1=ALU.mult),
                        reads=[a_o.s, rs4[f].s, sublnb.s], writes=[a_y.s])
                    q0 = qg * 512 + qb * 128
                    P.op('gpsimd', lambda g, a_y=a_y, q0=q0: g.dma_start(
                        out=mix_d[q0:q0 + 128, h * 256:(h + 1) * 256], in_=a_y[:]), reads=[a_y.s], dma=sty4[f][qb])
            return fin_

        LOOK = 3
        for h in range(8):
            if h + 1 < 8:
                load_head(h + 1)
            i = h % 2
            for qg in range(4):
                for m in range(2):
                    qT, kT, vv, bd = qTs[i][m], kTs[i][m], vs[i], bands[i]
                    pSq = {}

                    def emit_S(kc, qT=qT, kT=kT, qg=qg):
                        pS_ = pS[nS[0] % 4]
                        nS[0] += 1
                        P.op('tensor', lambda t, pS_=pS_, kc=kc: t.matmul(
                            pS_[:], lhsT=kT[:, kc * 128:(kc + 1) * 128], rhs=qT[:, qg * 512:(qg + 1) * 512],
                            start=True, stop=True), reads=[kT.s, qT.s], writes=[pS_.s])
                        pSq[kc] = pS_
                    for kc in range(LOOK):
                        emit_S(kc)
                    for kc in range(32):
                        pS_ = pSq.pop(kc)
                        E = Es[nE[0] % 4]
                        nE[0] += 1
                        r = kc - 4 * qg
                        if -1 <= r <= 4:
                            tm = tmps[nT[0] % 2]
                            nT[0] += 1
                            P.op('vector', lambda v, tm=tm, pS_=pS_, bd=bd, r=r: v.scalar_tensor_tensor(
                                out=tm[:], in0=pS_[:], scalar=SCALE, in1=bd[:, r + 1, :], op0=ALU.mult, op1=ALU.add),
                                reads=[pS_.s, bd.s], writes=[tm.s])
                            P.op('scalar', lambda a, E=E, tm=tm: a.activation(out=E[:], in_=tm[:], func=AF.Exp),
                                 reads=[tm.s], writes=[E.s])
                        else:
                            ci = 2 * h + (0 if r < -1 else 1)
                            P.op('scalar', lambda a, E=E, pS_=pS_, ci=ci: a.activation(
                                out=E[:], in_=pS_[:], func=AF.Exp, scale=SCALE, bias=cfar[:, ci:ci + 1]),
                                reads=[pS_.s, cfar.s], writes=[E.s])
                        if kc + LOOK < 32:
                            emit_S(kc + LOOK)

                        def av(t, E=E, vv=vv, kc=kc):
                            rr = None
                            for qb in range(4):
                                rr = t.matmul(pA[qb][:, 0:257], lhsT=E[:, qb * 128:(qb + 1) * 128], rhs=vv[:, kc, :],
                                              start=(kc == 0), stop=(kc == 31))
                            return rr
                        P.op('tensor', av, reads=[E.s] + vv.all(), writes=[pA[qb].s for qb in range(4)])
                        if kc == 10 and m == 0 and pending:
                            pending.pop(0)()
                    for qb in range(4):
                        o = osb2[m][qb]
                        P.op('scalar', lambda a, o=o, qb=qb: a.copy(out=o[:], in_=pA[qb][:, 0:257]), reads=[pA[qb].s],
                             writes=[o.s])
                pending.append(make_finalize(h, qg, nF[0] % 2))
                nF[0] += 1
        while pending:
            pending.pop(0)()
        P.flush("ph2")
    bT_d = dscr("bT_d", [8, 128, T], BF16)
    cT_d = dscr("cT_d", [8, 128, T], BF16)
    if upto >= 2.5:
      with ExitStack() as st:
        cw = sb(st, "cw", [128, 32, 5], F32)
        cb = sb(st, "cb", [128, 32], F32)
        P.op('sync', lambda e: e.dma_start(out=cw[:], in_=convw_d), writes=[cw.s], dma=P.sem("ld_cw"))
        P.op('sync', lambda e: e.dma_start(out=cb[:], in_=convb_d), writes=[cb.s], dma=P.sem("ld_cb"))
        ups = [sb(st, "up%d" % i, [128, T + 4], BF16) for i in range(2)]
        accs = [sb(st, "cacc%d" % i, [128, T], F32) for i in range(2)]
        cvos = [sb(st, "cvo%d" % i, [128, T], BF16) for i in range(2)]
        stg = [sb(st, "cstg%d" % i, [128, 32, 256], BF16) for i in range(2)]
        pT3 = [ps(st, "pT3_%d" % i, [128, 8, 128], BF16) for i in range(4)]
        ldu = [P.sem("ld_up%d" % i) for i in range(2)]
        stc = [P.sem("st_cvo%d" % i) for i in range(2)]
        sts = [P.sem("st_cstg%d" % i) for i in range(2)]
        for i in range(2):
            P.op('vector', lambda v, i=i: v.memset(ups[i][:, 0:2], 0.0), writes=[ups[i].sub('pl')])
            P.op('vector', lambda v, i=i: v.memset(ups[i][:, T + 2:T + 4], 0.0), writes=[ups[i].sub('pr')])

        def load_u(b):
            P.op('sync', lambda e: e.dma_start(out=ups[b % 2][:, 2:T + 2], in_=uT_d[b]), writes=[ups[b % 2].s],
                 dma=ldu[b % 2])
        load_u(0)
        cnt3 = [0]
        for b in range(32):
            if b + 1 < 32:
                load_u(b + 1)
            up, acc, cvo = ups[b % 2], accs[b % 2], cvos[b % 2]
            P.op('vector', lambda v, up=up, acc=acc, b=b: v.tensor_scalar(
                out=acc[:], in0=up[:, 0:T], scalar1=cw[:, b, 0:1], scalar2=None, op0=ALU.mult),
                reads=up.all() + [cw.s], writes=[acc.s])
            for j in range(1, 5):
                P.op('vector', lambda v, up=up, acc=acc, b=b, j=j: v.scalar_tensor_tensor(
                    out=acc[:], in0=up[:, j:j + T], scalar=cw[:, b, j:j + 1], in1=acc[:], op0=ALU.mult, op1=ALU.add),
                    reads=up.all() + [cw.s, acc.s], writes=[acc.s])
            P.op('scalar', lambda a, acc=acc, cvo=cvo, b=b: a.activation(out=cvo[:], in_=acc[:], func=AF.Silu,
                                                                       bias=cb[:, b:b + 1]),
                 reads=[acc.s, cb.s], writes=[cvo.s])
            if b >= 16:
                dst = bT_d if b < 24 else cT_d
                g = (b - 16) % 8
                P.op('gpsimd', lambda e, cvo=cvo, dst=dst, g=g: e.dma_start(out=dst[g], in_=cvo[:]), reads=[cvo.s],
                     dma=stc[b % 2])
            if b < 24:
                sg = stg[(b // 2) % 2]
                half = b % 2
                transpose_tile(cvo, lambda c0, nb, sg=sg, half=half: sg[:, c0:c0 + nb, half * 128:(half + 1) * 128],
                               T, pT3, cnt3, identb, lambda g8, sg=sg, half=half: sg.sub((half, g8)))
                if half == 1:
                    if b < 16:
                        dstv = xs_d.rearrange("(c p) n -> p c n", p=128)[:, :, (b - 1) * 128:(b + 1) * 128]
                    else:
                        dstv = bt_d.rearrange("(c p) n -> p c n", p=128)[:, :, (b - 17) * 128:(b - 15) * 128]
                    P.op('sync', lambda e, sg=sg, dstv=dstv: e.dma_start(out=dstv, in_=sg[:]), reads=sg.all(),
                         dma=sts[(b // 2) % 2])
        P.flush("ph3a")

    if upto >= 3:
      with ExitStack() as st:
        BTc = [sb(st, "BTc%d" % i, [128, 8, 128], BF16) for i in range(2)]
        CTc = [sb(st, "CTc%d" % i, [128, 8, 128], BF16) for i in range(2)]
        ldBT = [P.sem("ld_BTc%d" % i) for i in range(2)]
        ldCT = [P.sem("ld_CTc%d" % i) for i in range(2)]
        dtbb = bcast_load(st, "dtbb", dtb_d[0:1, :], 64)
        Ab = bcast_load(st, "Ab", alog_d[0:1, :], 64)
        dskb = bcast_load(st, "dskb", dskip_d[0:1, :], 32)
        ssmnb = bcast_load(st, "ssmnb", ssmn_d[0:1, :], 2048)
        P.op('scalar', lambda a: a.activation(out=Ab[:], in_=Ab[:], func=AF.Exp), reads=[Ab.s], writes=[Ab.s])
        P.op('vector', lambda v: v.tensor_scalar(out=Ab[:], in0=Ab[:], scalar1=-1.0, scalar2=None, op0=ALU.mult),
             reads=[Ab.s], writes=[Ab.s])
        xsc = [sb(st, "xsc%d" % i, [128, 32, 64], BF16) for i in range(2)]
        btc = [sb(st, "btc%d" % i, [128, 1024], BF16) for i in range(2)]
        dtr = [sb(st, "dtr%d" % i, [128, 32], F32) for i in range(2)]
        yfl = [sb(st, "yfl%d" % i, [128, 2048], F32) for i in range(2)]
        zsl = [sb(st, "zsl%d" % i, [128, 2048], BF16) for i in range(2)]
        ldxs = [P.sem("ld_xsc%d" % i) for i in range(2)]
        ldbt = [P.sem("ld_btc%d" % i) for i in range(2)]
        lddt = [P.sem("ld_dtr%d" % i) for i in range(2)]
        ldyf = [P.sem("ld_yfl%d" % i) for i in range(2)]
        ldzs = [P.sem("ld_zsl%d" % i) for i in range(2)]
        sm = {n: sb(st, "sm_" + n, [128, 32], F32) for n in ('t1', 'dt', 'a', 'eacs', 'cd', 'w')}
        acst = sb(st, "acst", [128, 64], F32)
        nacs = sb(st, "nacs", [128, 32], F32)
        mneg4 = [sb(st, "mneg4_%d" % i, [128, 4, 128], F32) for i in range(2)]
        for i_, mm_ in ((0, mneg_f), (1, mneg_b)):
            P.op('vector', lambda v, i_=i_, mm_=mm_: v.tensor_copy(out=mneg4[i_][:], in_=bc(mm_[:], [128, 4, 128], 1)),
                 reads=[mm_.s], writes=[mneg4[i_].s])
        xdt = sb(st, "xdt", [128, 32, 64], BF16)
        xw = sb(st, "xw", [128, 32, 64], BF16)
        rbig = sb(st, "rbig", [128, 32, 128], F32)
        dms = [sb(st, "dm%d" % i, [128, 4, 128], F32) for i in range(2)]
        decs = [sb(st, "dec%d" % i, [128, 4, 128], F32) for i in range(2)]
        MTs = [sb(st, "MT%d" % i, [128, 4, 128], BF16) for i in range(2)]
        yos = [sb(st, "yo%d" % i, [128, 4, 64], F32) for i in range(2)]
        ysb = [sb(st, "ysb%d" % i, [128, 32, 64], F32) for i in range(2)]
        Sst = sb(st, "Sst", [128, 32, 64], F32)
        Sbf = sb(st, "Sbf", [128, 32, 64], BF16)
        sq = sb(st, "sq", [128, 8, 256], F32)
        gss = sb(st, "gss", [128, 8], F32)
        gsd = sb(st, "gsd", [128, 8], F32)
        grs = sb(st, "grs", [128, 8], F32)
        yout = [sb(st, "yout%d" % i, [128, 8, 256], BF16) for i in range(2)]
        styf = [P.sem("st_ysb%d" % i) for i in range(2)]
        styo = [P.sem("st_yout%d" % i) for i in range(2)]
        pc = ps(st, "pc", [128, 512], F32)
        pR = [ps(st, "pR%d" % i, [128, 4, 128], F32) for i in range(2)]
        pB = [ps(st, "pB%d" % i, [128, 512], F32) for i in range(2)]
        pC = [ps(st, "pC%d" % i, [128, 512], F32) for i in range(2)]

        def bc(ap, shape, axis):
            return ap.unsqueeze(axis).to_broadcast(shape)

        seq = [(0, c, True) for c in range(16)] + [(1, c, False) for c in range(31, 15, -1)] + \
              [(1, c, True) for c in range(15, -1, -1)]
        import os as _os
        if _os.environ.get("SSD_MAXK"):
            seq = seq[:int(_os.environ["SSD_MAXK"])]
        if _os.environ.get("SSD_NOY"):
            seq = [(d_, c_, False) for (d_, c_, n_) in seq]

        def load_chunk(k):
            d, c, need_y = seq[k]
            i = k % 2
            r0 = c * 128
            P.op('sync', lambda e: e.dma_start(out=xsc[i][:], in_=xs_d[r0:r0 + 128, :].rearrange("p (h e) -> p h e", e=64)),
                 writes=[xsc[i].s], dma=ldxs[i])
            P.op('sync', lambda e: e.dma_start(out=btc[i][:], in_=bt_d[r0:r0 + 128, :]), writes=[btc[i].s], dma=ldbt[i])
            P.op('sync', lambda e: e.dma_start(out=dtr[i][:], in_=dt_d[r0:r0 + 128, d * 32:(d + 1) * 32]),
                 writes=[dtr[i].s], dma=lddt[i])
            if need_y:
                P.op('sync', lambda e: e.dma_start(out=BTc[i][:], in_=bT_d[:, :, r0:r0 + 128].rearrange("g p t -> p g t")),
                     writes=[BTc[i].s], dma=ldBT[i])
                P.op('sync', lambda e: e.dma_start(out=CTc[i][:], in_=cT_d[:, :, r0:r0 + 128].rearrange("g p t -> p g t")),
                     writes=[CTc[i].s], dma=ldCT[i])
            if d == 1 and need_y:
                P.op('sync', lambda e: e.dma_start(out=yfl[i][:], in_=yf_d[r0:r0 + 128, :]), writes=[yfl[i].s], dma=ldyf[i])
                P.op('sync', lambda e: e.dma_start(out=zsl[i][:], in_=zs_d[r0:r0 + 128, :]), writes=[zsl[i].s], dma=ldzs[i])
        yf_slot = Slot('yf_dram')
        load_chunk(0)
        ng = [0]
        for k, (d, c, need_y) in enumerate(seq):
            if k == 16:
                P.op('vector', lambda v: v.memset(Sst[:], 0.0), writes=[Sst.s])
                P.op('vector', lambda v: v.memset(Sbf[:], 0.0), writes=[Sbf.s])
            if k == 0:
                P.op('vector', lambda v: v.memset(Sst[:], 0.0), writes=[Sst.s])
                P.op('vector', lambda v: v.memset(Sbf[:], 0.0), writes=[Sbf.s])
            if k + 1 < len(seq):
                load_chunk(k + 1)
            i = k % 2
            xs_, bt_, dr = xsc[i], btc[i], dtr[i]
            BT, CT = BTc[i], CTc[i]
            hc = slice(d * 32, (d + 1) * 32)
            Tri = U_f if d == 0 else L_f
            mneg = mneg_f if d == 0 else mneg_b
            t1, dt, a_, eacs, cd, w_ = (sm[n] for n in ('t1', 'dt', 'a', 'eacs', 'cd', 'w'))
            acs = acst
            P.op('vector', lambda v, dr=dr, hc=hc: v.tensor_tensor(out=t1[:], in0=dr[:], in1=dtbb[:, hc], op=ALU.add),
                 reads=[dr.s, dtbb.s], writes=[t1.s])
            P.op('scalar', lambda a: a.activation(out=t1[:], in_=t1[:], func=AF.Exp), reads=[t1.s], writes=[t1.s])
            P.op('scalar', lambda a: a.activation(out=dt[:], in_=t1[:], func=AF.Ln, bias=1.0), reads=[t1.s],
                 writes=[dt.s])
            P.op('vector', lambda v, hc=hc: v.tensor_tensor(out=a_[:], in0=dt[:], in1=Ab[:, hc], op=ALU.mult),
                 reads=[dt.s, Ab.s], writes=[a_.s])

            def mmc(t, Tri=Tri):
                t.matmul(pc[:, 0:32], lhsT=Tri[:], rhs=a_[:], start=True, stop=True)
                return t.matmul(pc[:, 32:64], lhsT=ones_f[:], rhs=a_[:], start=True, stop=True)
            P.op('tensor', mmc, reads=[Tri.s, ones_f.s, a_.s], writes=[pc.s])
            P.op('vector', lambda v: v.tensor_copy(out=acst[:], in_=pc[:, 0:64]), reads=[pc.s], writes=[acst.s])
            P.op('vector', lambda v: v.tensor_scalar(out=nacs[:], in0=acst[:, 0:32], scalar1=-1.0, scalar2=None, op0=ALU.mult),
                 reads=[acst.s], writes=[nacs.s])
            P.op('scalar', lambda a: a.activation(out=eacs[:], in_=acst[:, 0:32], func=AF.Exp), reads=[acst.s], writes=[eacs.s])
            P.op('scalar', lambda a: a.activation(out=cd[:], in_=acst[:, 32:64], func=AF.Exp), reads=[acst.s], writes=[cd.s])
            P.op('vector', lambda v: v.tensor_tensor(out=w_[:], in0=acst[:, 32:64], in1=acst[:, 0:32], op=ALU.subtract),
                 reads=[acst.s], writes=[w_.s])
            P.op('scalar', lambda a: a.activation(out=w_[:], in_=w_[:], func=AF.Exp), reads=[w_.s], writes=[w_.s])
            P.op('vector', lambda v: v.tensor_tensor(out=w_[:], in0=w_[:], in1=dt[:], op=ALU.mult), reads=[w_.s, dt.s],
                 writes=[w_.s])
            P.op('gpsimd', lambda v, xs_=xs_: v.tensor_tensor(out=xw[:], in0=xs_[:], in1=bc(w_[:], [128, 32, 64], 2),
                                                             op=ALU.mult), reads=[xs_.s, w_.s], writes=[xw.s])
            if need_y:
                P.op('gpsimd', lambda v, xs_=xs_: v.tensor_tensor(out=xdt[:], in0=xs_[:], in1=bc(dt[:], [128, 32, 64], 2),
                                                                 op=ALU.mult), reads=[xs_.s, dt.s], writes=[xdt.s])
                P.op('gpsimd', lambda v, Tri=Tri: v.tensor_tensor(out=rbig[:], in0=bc(a_[:], [128, 32, 128], 2),
                                                                  in1=bc(Tri[:], [128, 32, 128], 1), op=ALU.mult),
                     reads=[a_.s, Tri.s], writes=[rbig.s])
            ys = ysb[i]
            for g in range(8):
                j = ng[0] % 2
                ng[0] += 1
                pR_, pB_, pC_ = pR[j], pB[j], pC[j]
                cs = slice(c * 128, (c + 1) * 128)
                if need_y:
                    m4 = mneg4[d]

                    def mmR(t, pR_=pR_, g=g, m4=m4):
                        t.matmul(pR_[:], lhsT=ones_f[:], rhs=rbig[:, 4 * g:4 * g + 4, :], start=True, stop=False)
                        return t.matmul(pR_[:], lhsT=identf[:], rhs=m4[:], start=False, stop=True)
                    P.op('tensor', mmR, reads=[ones_f.s, rbig.s, identf.s, m4.s], writes=[pR_.s])
                    P.op('tensor', lambda t, pB_=pB_, g=g, BT=BT, CT=CT: t.matmul(pB_[:, 0:128], lhsT=BT[:, g, :], rhs=CT[:, g, :],
                                                                           start=True, stop=True),
                         reads=[BT.s, CT.s], rw=[pB_.s])
                    dec, MT, yo = decs[j], MTs[j], yos[j]
                    for r in range(4):
                        P.op('scalar', lambda a, dec=dec, pR_=pR_, r=r, g=g: a.activation(
                            out=dec[:, r, :], in_=pR_[:, r, :], func=AF.Exp, bias=nacs[:, 4 * g + r:4 * g + r + 1]),
                            reads=[pR_.s, nacs.s], writes=[dec.sub(r)])
                    P.op('vector', lambda v, MT=MT, dec=dec, pB_=pB_: v.tensor_tensor(
                        out=MT[:], in0=dec[:], in1=bc(pB_[:, 0:128], [128, 4, 128], 1), op=ALU.mult),
                        reads=dec.all(), writes=[MT.s], rw=[pB_.s])

                    def mmy(t, pB_=pB_, MT=MT, g=g):
                        rr = None
                        for r in range(4):
                            rr = t.matmul(pB_[:, 128 + r * 64:128 + (r + 1) * 64], lhsT=MT[:, r, :], rhs=xdt[:, 4 * g + r, :],
                                          start=True, stop=True)
                        return rr
                    P.op('tensor', mmy, reads=[MT.s, xdt.s], rw=[pB_.s])
                    P.op('tensor', lambda t, pC_=pC_, g=g, CT=CT: t.matmul(
                        pC_[:, 0:256], lhsT=CT[:, g, :], rhs=Sbf[:, 4 * g:4 * g + 4, :], start=True, stop=True),
                        reads=[CT.s, Sbf.s, Sbf.sub(g)], rw=[pC_.s])
                    for r in range(4):
                        P.op('scalar', lambda a, yo=yo, pC_=pC_, g=g, r=r: a.activation(
                            out=yo[:, r, :], in_=pC_[:, r * 64:(r + 1) * 64], func=AF.Identity, scale=eacs[:, 4 * g + r:4 * g + r + 1]),
                            reads=[eacs.s], writes=[yo.sub(r)], rw=[pC_.s])
                    P.op('vector', lambda v, ys=ys, yo=yo, pB_=pB_, g=g: v.tensor_tensor(
                        out=ys[:, 4 * g:4 * g + 4, :], in0=yo[:],
                        in1=pB_[:, 128:384].rearrange("p (r e) -> p r e", e=64), op=ALU.add),
                        reads=yo.all(), writes=[ys.sub(g)], rw=[pB_.s])
                P.op('tensor', lambda t, pC_=pC_, bt_=bt_, g=g: t.matmul(
                    pC_[:, 256:512], lhsT=bt_[:, g * 128:(g + 1) * 128], rhs=xw[:, 4 * g:4 * g + 4, :], start=True, stop=True),
                    reads=[bt_.s, xw.s], rw=[pC_.s])
                P.op('vector', lambda v, g=g: v.tensor_tensor(
                    out=Sst[:, 4 * g:4 * g + 4, :], in0=Sst[:, 4 * g:4 * g + 4, :],
                    in1=bc(cd[:, 4 * g:4 * g + 4], [128, 4, 64], 2), op=ALU.mult),
                    reads=[cd.s, Sst.s, Sst.sub(g)], writes=[Sst.sub(g)])
                P.op('vector', lambda v, pC_=pC_, g=g: v.tensor_tensor(
                    out=Sst[:, 4 * g:4 * g + 4, :], in0=Sst[:, 4 * g:4 * g + 4, :],
                    in1=pC_[:, 256:512].rearrange("p (r e) -> p r e", e=64), op=ALU.add),
                    reads=[Sst.sub(g)], writes=[Sst.sub(g)], rw=[pC_.s])
                P.op('scalar', lambda a, g=g: a.copy(out=Sbf[:, 4 * g:4 * g + 4, :], in_=Sst[:, 4 * g:4 * g + 4, :]),
                     reads=[Sst.sub(g)], writes=[Sbf.sub(g)])
            if need_y and d == 0:
                P.op('vector', lambda v, xs_=xs_: v.tensor_tensor(out=xdt[:], in0=xs_[:], in1=bc(dskb[:], [128, 32, 64], 2),
                                                                 op=ALU.mult), reads=[xs_.s, dskb.s, xdt.s], writes=[xdt.s])
                P.op('vector', lambda v, ys=ys: v.tensor_tensor(out=ys[:], in0=ys[:], in1=xdt[:], op=ALU.add),
                     reads=ys.all() + [xdt.s], writes=[ys.s])
                r0 = c * 128
                P.op('gpsimd', lambda e, ys=ys, r0=r0: e.dma_start(out=yf_d[r0:r0 + 128, :].rearrange("p (h e) -> p h e", e=64),
                                                                   in_=ys[:]), reads=ys.all(), dma=styf[i])
            if need_y and d == 1:
                yf_, zs_ = yfl[i], zsl[i]
                yo_ = yout[i]
                ysf = ys[:].rearrange("p h e -> p (h e)")
                P.op('gpsimd', lambda v, ys=ys, yf_=yf_: v.tensor_tensor(out=ys[:].rearrange("p h e -> p (h e)"),
                                                                         in0=ys[:].rearrange("p h e -> p (h e)"),
                                                                         in1=yf_[:], op=ALU.add),
                     reads=ys.all() + [yf_.s], writes=[ys.s])
                P.op('gpsimd', lambda v, ys=ys, zs_=zs_: v.tensor_tensor(out=ys[:].rearrange("p h e -> p (h e)"),
                                                                         in0=ys[:].rearrange("p h e -> p (h e)"),
                                                                         in1=zs_[:], op=ALU.mult),
                     reads=[ys.s, zs_.s], writes=[ys.s])
                P.op('gpsimd', lambda v, ys=ys: v.tensor_tensor(out=sq[:].rearrange("p g e -> p (g e)"),
                                                                in0=ys[:].rearrange("p h e -> p (h e)"),
                                                                in1=ys[:].rearrange("p h e -> p (h e)"), op=ALU.mult),
                     reads=[ys.s], writes=[sq.s])
                P.op('vector', lambda v: v.tensor_reduce(out=gss[:], in_=sq[:], axis=AX.X, op=ALU.add), reads=[sq.s],
                     writes=[gss.s])
                P.op('scalar', lambda a: a.activation(out=gsd[:], in_=gss[:], func=AF.Sqrt, scale=1.0 / 256, bias=EPS),
                     reads=[gss.s], writes=[gsd.s])
                P.op('vector', lambda v: v.reciprocal(out=grs[:], in_=gsd[:]), reads=[gsd.s], writes=[grs.s])
                P.op('vector', lambda v, ys=ys: v.tensor_tensor(out=sq[:], in0=ys[:].rearrange("p (g r) e -> p g (r e)", r=4),
                                                                in1=bc(grs[:], [128, 8, 256], 2), op=ALU.mult),
                     reads=[ys.s, grs.s, sq.s], writes=[sq.s])
                P.op('vector', lambda v, yo_=yo_: v.tensor_tensor(out=yo_[:].rearrange("p g e -> p (g e)"),
                                                                  in0=sq[:].rearrange("p g e -> p (g e)"),
                                                                  in1=ssmnb[:], op=ALU.mult),
                     reads=[sq.s, ssmnb.s], writes=[yo_.s])
                r0 = c * 128
                P.op('gpsimd', lambda e, yo_=yo_, r0=r0: e.dma_start(
                    out=mix_d[r0:r0 + 128, 2048:4096].rearrange("p (g e) -> p g e", e=256), in_=yo_[:]),
                    reads=[yo_.s], dma=styo[i])
            if k == 15 and len(seq) > 16:
                P.flush("ph3f")
        P.flush("ph3b")
    if upto >= 4:
      with ExitStack() as st:
        mixT = sb(st, "mixT", [128, NCH, TO], BF16)
        mts = [sb(st, "mt%d" % i, [128, D], BF16) for i in range(2)]
        pT4 = [ps(st, "pT4_%d" % i, [128, 8, 128], BF16) for i in range(2)]
        ldm = [P.sem("ld_mt%d" % i) for i in range(2)]
        cnt4 = [0]

        def load_m(i):
            P.op('sync', lambda e: e.dma_start(out=mts[i % 2][:], in_=mix_d[i * 128:(i + 1) * 128, :]), writes=[mts[i % 2].s],
                 dma=ldm[i % 2])
        load_m(0)
        for i in range(16):
            if i + 1 < 16:
                load_m(i + 1)
            transpose_tile(mts[i % 2], lambda c0, nb, i=i: mixT[:, c0:c0 + nb, i * 128:(i + 1) * 128], D, pT4, cnt4, identb,
                           lambda g8, i=i: mixT.sub((i, g8)))
        Wo = [sb(st, "Wo%d" % i, [128, NCH, 256], BF16) for i in range(2)]
        ldWo = [P.sem("ld_Wo%d" % i) for i in range(2)]
        xr = [sb(st, "xr%d" % i, [128, 256], F32) for i in range(3)]
        ldxr = [P.sem("ld_xr%d" % i) for i in range(3)]
        x1s = [sb(st, "x1s%d" % i, [128, 256], F32) for i in range(3)]
        stx1 = [P.sem("st_x1s%d" % i) for i in range(3)]
        pO4 = [ps(st, "pO4_%d" % i, [128, 512], F32) for i in range(4)]
        wov = w_out.rearrange("(c p) n -> p c n", p=128)

        def load_Wo(s_):
            P.op('gpsimd', lambda g: g.dma_start(out=Wo[s_ % 2][:], in_=wov[:, :, s_ * 256:(s_ + 1) * 256]),
                 writes=[Wo[s_ % 2].s], dma=ldWo[s_ % 2])
        load_Wo(0)
        n4 = 0
        for s_ in range(16):
            if s_ + 1 < 16:
                load_Wo(s_ + 1)
            W = Wo[s_ % 2]
            for tt in range(16):
                xr_ = xr[n4 % 3]
                x1_ = x1s[n4 % 3]
                pa = pO4[n4 % 4]
                P.op('sync', lambda e, xr_=xr_, tt=tt, s_=s_: e.dma_start(
                    out=xr_[:], in_=x_d[tt * 128:(tt + 1) * 128, s_ * 256:(s_ + 1) * 256]), writes=[xr_.s], dma=ldxr[n4 % 3])

                def mm(t, pa=pa, W=W, tt=tt):
                    r = None
                    for c in range(NCH):
                        r = t.matmul(pa[:, 0:256], lhsT=mixT[:, c, tt * 128:(tt + 1) * 128], rhs=W[:, c, :],
                                     start=(c == 0), stop=(c == NCH - 1))
                    return r
                P.op('tensor', mm, reads=[W.s] + mixT.all(), writes=[pa.s])
                P.op('vector', lambda v, x1_=x1_, pa=pa, xr_=xr_: v.tensor_tensor(out=x1_[:], in0=pa[:, 0:256], in1=xr_[:],
                                                                                  op=ALU.add),
                     reads=[pa.s, xr_.s], writes=[x1_.s])
                P.op('sync', lambda e, x1_=x1_, tt=tt, s_=s_: e.dma_start(
                    out=x1_d[tt * 128:(tt + 1) * 128, s_ * 256:(s_ + 1) * 256], in_=x1_[:]), reads=[x1_.s], dma=stx1[n4 % 3])
                n4 += 1
        P.flush("ph4")

    if upto >= 5:
        idx_t = [[sb(top, "idx%d_%d" % (k, tt), [128, 1], I32) for tt in range(16)] for k in range(2)]
        gate_t = sb(top, "gate_t", [128, 2, 16], F32)
    if upto >= 5:
      with ExitStack() as st:
        w2b = bcast_load(st, "w2b", norm2_w[0:1, :], D)
        brt = bcast_load(st, "brt", b_rt[0:1, :], 36)
        Wr = sb(st, "Wr", [128, NCH, 36], F32)
        P.op('sync', lambda e: e.dma_start(out=Wr[:], in_=w_rt.rearrange("(c p) n -> p c n", p=128)), writes=[Wr.s],
             dma=P.sem("ld_Wr"))
        eidx = sb(st, "eidx", [128, 32], F32)
        pidx = sb(st, "pidx", [128, 1], F32)
        P.op('gpsimd', lambda g: g.iota(eidx[:], pattern=[[1, 32]], base=0, channel_multiplier=0,
                                        allow_small_or_imprecise_dtypes=True), writes=[eidx.s])
        P.op('gpsimd', lambda g: g.iota(pidx[:], pattern=[[1, 1]], base=NSLOT, channel_multiplier=1,
                                        allow_small_or_imprecise_dtypes=True), writes=[pidx.s])
        cum = sb(st, "cum", [128, 32], F32)
        P.op('vector', lambda v: v.memset(cum[:], 0.0), writes=[cum.s])
        zt = sb(st, "zt", [128, D], BF16)
        P.op('gpsimd', lambda g: g.memset(zt[:], 0.0), writes=[zt.s])
        P.op('sync', lambda e: e.dma_start(out=y_d[NSLOT:NSLOT + 128, :], in_=zt[:]), reads=[zt.s], dma=P.sem("st_zt"))
        x1t = [sb(st, "x1t%d" % i, [128, D], F32) for i in range(2)]
        ldx1 = [P.sem("ld_x1t%d" % i) for i in range(2)]
        h2f = sb(st, "h2f", [128, D], F32)
        h2b = [sb(st, "h2b%d" % i, [128, D], BF16) for i in range(2)]
        junk5 = sb(st, "junk5", [128, D], BF16)
        h2T = sb(st, "h2T", [128, NCH, 128], F32)
        ss5, sd5, rs5 = (sb(st, n, [128, 1], F32) for n in ("ss5", "sd5", "rs5"))
        pT5 = [ps(st, "pT5_%d" % i, [128, 4, 128], F32) for i in range(3)]
        pL = ps(st, "pL", [128, 512], F32)
        pP = ps(st, "pP", [128, 512], F32)
        R = {n: sb(st, "R_" + n, [128, sz], F32) for n, sz in (
            ('lg', 36), ('gmax', 1), ('ngmax', 1), ('gmask', 4), ('ge', 4), ('gsum', 1), ('pg', 1), ('pen', 4), ('mel', 32),
            ('top8', 8), ('sel', 32), ('is1', 32), ('is2', 32), ('dm', 1), ('ew', 1), ('den', 1), ('rden', 1), ('e1', 1),
            ('e2', 1), ('p1', 1), ('p2', 1), ('pos', 32), ('j32', 32), ('ok', 1), ('sf', 1), ('tf', 1))}
        selb = sb(st, "selb", [128, 32], BF16)
        scs = [P.sem("sc_h2b%d" % i) for i in range(2)]

        def load_x1(i):
            P.op('sync', lambda e: e.dma_start(out=x1t[i % 2][:], in_=x1_d[i * 128:(i + 1) * 128, :]), writes=[x1t[i % 2].s],
                 dma=ldx1[i % 2])
        load_x1(0)
        n5 = [0]

        def V(fn, reads, writes):
            P.op('vector', fn, reads=[R[n].s if isinstance(n, str) else n for n in reads],
                 writes=[R[n].s if isinstance(n, str) else n for n in writes])

        for tt in range(16):
            if tt + 1 < 16:
                load_x1(tt + 1)
            xt = x1t[tt % 2]
            hb_ = h2b[tt % 2]
            rmsnorm_tile(xt, w2b, h2f, junk5, ss5, sd5, rs5, D)
            P.op('scalar', lambda a, hb_=hb_: a.copy(out=hb_[:], in_=h2f[:]), reads=[h2f.s], writes=[hb_.s])
            for g4 in range(8):
                pt = pT5[n5[0] % 3]
                n5[0] += 1

                def tr(t, pt=pt, g4=g4):
                    r = None
                    for k in range(4):
                        c = g4 * 4 + k
                        r = t.transpose(out=pt[:, k, :], in_=h2f[:, c * 128:(c + 1) * 128], identity=identf[:])
                    return r
                P.op('tensor', tr, reads=[h2f.s, identf.s], writes=[pt.s])
                if g4 % 2 == 0:
                    P.op('scalar', lambda a, pt=pt, g4=g4: a.copy(out=h2T[:, g4 * 4:(g4 + 1) * 4, :], in_=pt[:]), reads=[pt.s],
                         writes=[h2T.sub(g4)])
                else:
                    P.op('vector', lambda v, pt=pt, g4=g4: v.tensor_copy(out=h2T[:, g4 * 4:(g4 + 1) * 4, :], in_=pt[:]),
                         reads=[pt.s], writes=[h2T.sub(g4)])

            def mml(t):
                r = None
                for c in range(NCH):
                    r = t.matmul(pL[:, 0:36], lhsT=h2T[:, c, :], rhs=Wr[:, c, :], start=(c == 0), stop=(c == NCH - 1))
                return r
            P.op('tensor', mml, reads=h2T.all() + [Wr.s], writes=[pL.s])
            lg, gl = R['lg'], R['lg']
            V(lambda v: v.tensor_tensor(out=R['lg'][:], in0=pL[:, 0:36], in1=brt[:], op=ALU.add), [pL.s, brt.s], ['lg'])
            V(lambda v: v.tensor_reduce(out=R['gmax'][:], in_=R['lg'][:, 0:4], axis=AX.X, op=ALU.max), ['lg'], ['gmax'])
            V(lambda v: v.tensor_scalar(out=R['ngmax'][:], in0=R['gmax'][:], scalar1=-1.0, scalar2=None, op0=ALU.mult),
              ['gmax'], ['ngmax'])
            V(lambda v: v.tensor_scalar(out=R['gmask'][:], in0=R['lg'][:, 0:4], scalar1=R['gmax'][:], scalar2=None,
                                        op0=ALU.is_equal), ['lg', 'gmax'], ['gmask'])
            P.op('scalar', lambda a: a.activation(out=R['ge'][:], in_=R['lg'][:, 0:4], func=AF.Exp, bias=R['ngmax'][:]),
                 reads=[R['lg'].s, R['ngmax'].s], writes=[R['ge'].s])
            V(lambda v: v.tensor_reduce(out=R['gsum'][:], in_=R['ge'][:], axis=AX.X, op=ALU.add), ['ge'], ['gsum'])
            V(lambda v: v.reciprocal(out=R['pg'][:], in_=R['gsum'][:]), ['gsum'], ['pg'])
            V(lambda v: v.tensor_scalar(out=R['pen'][:], in0=R['gmask'][:], scalar1=1.0, scalar2=1.0e9, op0=ALU.subtract,
                                        op1=ALU.mult), ['gmask'], ['pen'])
            V(lambda v: v.tensor_tensor(out=R['mel'][:].rearrange("p (g e) -> p g e", e=8),
                                        in0=R['lg'][:, 4:36].rearrange("p (g e) -> p g e", e=8),
                                        in1=R['pen'][:].unsqueeze(2).to_broadcast([128, 4, 8]), op=ALU.add),
              ['lg', 'pen'], ['mel'])
            V(lambda v: v.max(out=R['top8'][:], in_=R['mel'][:]), ['mel'], ['top8'])
            V(lambda v: v.tensor_scalar(out=R['sel'][:], in0=R['mel'][:], scalar1=R['top8'][:, 1:2], scalar2=None,
                                        op0=ALU.is_ge), ['mel', 'top8'], ['sel'])
            V(lambda v: v.tensor_scalar(out=R['is1'][:], in0=R['mel'][:], scalar1=R['top8'][:, 0:1], scalar2=None,
                                        op0=ALU.is_ge), ['mel', 'top8'], ['is1'])
            V(lambda v: v.tensor_tensor(out=R['is2'][:], in0=R['sel'][:], in1=R['is1'][:], op=ALU.subtract),
              ['sel', 'is1'], ['is2'])
            V(lambda v: v.tensor_copy(out=selb[:], in_=R['sel'][:]), ['sel'], [selb.s])
            V(lambda v: v.tensor_tensor(out=R['dm'][:], in0=R['top8'][:, 1:2], in1=R['top8'][:, 0:1], op=ALU.subtract),
              ['top8'], ['dm'])
            P.op('scalar', lambda a: a.activation(out=R['ew'][:], in_=R['dm'][:], func=AF.Exp), reads=[R['dm'].s],
                 writes=[R['ew'].s])
            V(lambda v: v.tensor_scalar(out=R['den'][:], in0=R['ew'][:], scalar1=1.0, scalar2=None, op0=ALU.add),
              ['ew'], ['den'])
            V(lambda v: v.reciprocal(out=R['rden'][:], in_=R['den'][:]), ['den'], ['rden'])
            V(lambda v, tt=tt: v.tensor_tensor(out=gate_t[:, 0, tt:tt + 1], in0=R['pg'][:], in1=R['rden'][:], op=ALU.mult),
              ['pg', 'rden'], [gate_t.sub((0, tt))])
            V(lambda v, tt=tt: v.tensor_tensor(out=gate_t[:, 1, tt:tt + 1], in0=gate_t[:, 0, tt:tt + 1], in1=R['ew'][:],
                                               op=ALU.mult), ['ew', gate_t.sub((0, tt))], [gate_t.sub((1, tt))])

            def mmp(t):
                t.matmul(pP[:, 0:32], lhsT=Ls_b[:], rhs=selb[:], start=True, stop=True)
                return t.matmul(pP[:, 32:64], lhsT=ones_b[:], rhs=selb[:], start=True, stop=True)
            P.op('tensor', mmp, reads=[Ls_b.s, ones_b.s, selb.s], writes=[pP.s])
            V(lambda v: v.tensor_tensor(out=R['pos'][:], in0=pP[:, 0:32], in1=cum[:], op=ALU.add), [pP.s, cum.s], ['pos'])
            V(lambda v: v.tensor_tensor(out=cum[:], in0=pP[:, 32:64], in1=cum[:], op=ALU.add), [pP.s, cum.s, 'pos'], [cum.s])
            for k, isn, en, pn in ((0, 'is1', 'e1', 'p1'), (1, 'is2', 'e2', 'p2')):
                V(lambda v, isn=isn: v.tensor_tensor(out=R['j32'][:], in0=R[isn][:], in1=eidx[:], op=ALU.mult),
                  [isn, eidx.s, 'j32'], ['j32'])
                V(lambda v, en=en: v.tensor_reduce(out=R[en][:], in_=R['j32'][:], axis=AX.X, op=ALU.add), ['j32'], [en])
                V(lambda v, isn=isn: v.tensor_tensor(out=R['j32'][:], in0=R[isn][:], in1=R['pos'][:], op=ALU.mult),
                  [isn, 'pos', 'j32'], ['j32'])
                V(lambda v, pn=pn: v.tensor_reduce(out=R[pn][:], in_=R['j32'][:], axis=AX.X, op=ALU.add), ['j32'], [pn])
                V(lambda v, pn=pn: v.tensor_scalar(out=R['ok'][:], in0=R[pn][:], scalar1=float(CAP), scalar2=None,
                                                   op0=ALU.is_lt), [pn], ['ok'])
                V(lambda v, en=en, pn=pn: v.scalar_tensor_tensor(out=R['sf'][:], in0=R[en][:], scalar=float(CAP), in1=R[pn][:],
                                                                 op0=ALU.mult, op1=ALU.add), [en, pn], ['sf'])
                V(lambda v: v.tensor_tensor(out=R['sf'][:], in0=R['sf'][:], in1=pidx[:], op=ALU.subtract), ['sf', pidx.s], ['sf'])
                V(lambda v: v.scalar_tensor_tensor(out=R['sf'][:], in0=R['sf'][:], scalar=R['ok'][:], in1=pidx[:],
                                                   op0=ALU.mult, op1=ALU.add), ['sf', 'ok', pidx.s], ['sf'])
                it = idx_t[k][tt]
                V(lambda v, it=it: v.tensor_copy(out=it[:], in_=R['sf'][:]), ['sf'], [it.s])
                P.op('gpsimd', lambda g, it=it, hb_=hb_: g.indirect_dma_start(
                    out=xg_d, out_offset=bass.IndirectOffsetOnAxis(ap=it[:, :], axis=0), in_=hb_[:, :], in_offset=None), reads=[it.s, hb_.s], dma=scs[tt % 2])
        P.flush("ph5")

    if upto >= 6:
      with ExitStack() as st:
        xgs = [sb(st, "xgs%d" % i, [128, D], BF16) for i in range(2)]
        ldxg = [P.sem("ld_xgs%d" % i) for i in range(2)]
        XgTs = [sb(st, "XgT%d" % i, [128, NCH, CAP], BF16) for i in range(2)]
        WA = [[sb(st, "WA%d_%d" % (i, gu), [128, NCH, 256], BF16) for gu in range(2)] for i in range(2)]
        ldWA = [[P.sem("ld_WA%d_%d" % (i, gu)) for gu in range(2)] for i in range(2)]
        WD = [sb(st, "WD%d" % i, [128, 8, 512], BF16) for i in range(2)]
        ldWD = [P.sem("ld_WD%d" % i) for i in range(2)]
        actT = sb(st, "actT", [128, 8, CAP], BF16)
        sil = [sb(st, "sil%d" % i, [128, CAP], F32) for i in range(2)]
        ysg = [sb(st, "ysg%d" % i, [128, 512], BF16) for i in range(2)]
        stys = [P.sem("st_ysg%d" % i) for i in range(2)]
        pT6 = [ps(st, "pT6_%d" % i, [128, 8, 128], BF16) for i in range(2)]
        pHg = [ps(st, "pHg%d" % i, [128, 512], F32) for i in range(2)]
        pHu = [ps(st, "pHu%d" % i, [128, 512], F32) for i in range(2)]
        pY = [ps(st, "pY6_%d" % i, [128, 512], F32) for i in range(2)]
        cnt6 = [0]
        nxg = [0]
        nH = [0]
        nY = [0]
        nD = [0]

        def load_A(e, qf):
            for gu, wsrc in ((0, w_gate), (1, w_up)):
                W = WA[qf % 2][gu]
                P.op('gpsimd', lambda g, W=W, wsrc=wsrc: g.dma_start(
                    out=W[:], in_=wsrc[e].rearrange("(c p) n -> p c n", p=128)[:, :, qf * 256:(qf + 1) * 256]),
                    writes=[W.s], dma=ldWA[qf % 2][gu])

        def load_D(e, q):
            W = WD[nD[0] % 2]
            sem = ldWD[nD[0] % 2]
            nD[0] += 1
            P.op('gpsimd', lambda g, W=W: g.dma_start(
                out=W[:], in_=w_down[e].rearrange("(c p) n -> p c n", p=128)[:, :, q * 512:(q + 1) * 512]),
                writes=[W.s], dma=sem)
            return W

        def load_xg(e, t2):
            xg = xgs[nxg[0] % 2]
            sem = ldxg[nxg[0] % 2]
            nxg[0] += 1
            r0 = e * CAP + t2 * 128
            P.op('sync', lambda s_: s_.dma_start(out=xg[:], in_=xg_d[r0:r0 + 128, :]), writes=[xg.s], dma=sem)
            return xg

        load_A(0, 0)
        load_A(0, 1)

        def prep_x(e):
            XgT = XgTs[e % 2]
            for t2 in range(CAP // 128):
                xg = load_xg(e, t2)
                transpose_tile(xg, lambda c0, nb, t2=t2, XgT=XgT: XgT[:, c0:c0 + nb, t2 * 128:(t2 + 1) * 128], D, pT6, cnt6, identb,
                               lambda g8, t2=t2, XgT=XgT: XgT.sub((t2, g8)))
        prep_x(0)
        for e in range(NE):
            XgT = XgTs[e % 2]
            WDs = [load_D(e, 0), load_D(e, 1)]
            for hf in range(4):
                Wg, Wu = WA[hf % 2]
                if hf == 1 and e + 1 < NE:
                    prep_x(e + 1)
                for fb in range(2):
                    phs = (pHg[nH[0] % 2], pHu[nH[0] % 2])
                    sl = sil[nH[0] % 2]
                    nH[0] += 1
                    for gu, W in ((0, Wg), (1, Wu)):
                        def mmA(t, ph=phs[gu], W=W, fb=fb, XgT=XgT):
                            r = None
                            for c in range(NCH):
                                r = t.matmul(ph[:, 0:CAP], lhsT=W[:, c, fb * 128:(fb + 1) * 128],
                                             rhs=XgT[:, c, :], start=(c == 0), stop=(c == NCH - 1))
                            return r
                        P.op('tensor', mmA, reads=[W.s] + XgT.all(), writes=[phs[gu].s])
                    P.op('scalar', lambda a, sl=sl, ph=phs[0]: a.activation(out=sl[:], in_=ph[:, 0:CAP], func=AF.Silu),
                         reads=[phs[0].s], writes=[sl.s])
                    fc = hf * 2 + fb
                    P.op('vector', lambda v, sl=sl, ph=phs[1], fc=fc: v.tensor_tensor(out=actT[:, fc, :], in0=sl[:], in1=ph[:, 0:CAP],
                                                                                     op=ALU.mult),
                         reads=[sl.s, phs[1].s], writes=[actT.sub(fc)])
                if hf < 2:
                    load_A(e, hf + 2)
                elif e + 1 < NE:
                    load_A(e + 1, hf - 2)
            for q in range(8):
                Wd = WDs[q] if q < 2 else load_D(e, q)
                for t2 in range(CAP // 128):
                    py = pY[nY[0] % 2]
                    ys_ = ysg[nY[0] % 2]
                    sem = stys[nY[0] % 2]
                    nY[0] += 1

                    def mmB(t, py=py, Wd=Wd, t2=t2):
                        r = None
                        for c in range(8):
                            r = t.matmul(py[:], lhsT=actT[:, c, t2 * 128:(t2 + 1) * 128], rhs=Wd[:, c, :],
                                         start=(c == 0), stop=(c == 7))
                        return r
                    P.op('tensor', mmB, reads=[Wd.s] + actT.all(), writes=[py.s])
                    if nY[0] % 2 == 0:
                        P.op('scalar', lambda a, ys_=ys_, py=py: a.copy(out=ys_[:], in_=py[:]), reads=[py.s], writes=[ys_.s])
                    else:
                        P.op('vector', lambda v, ys_=ys_, py=py: v.tensor_copy(out=ys_[:], in_=py[:]), reads=[py.s],
                             writes=[ys_.s])
                    r0 = e * CAP + t2 * 128
                    c0 = q * 512
                    P.op('sync', lambda s_, ys_=ys_, r0=r0, c0=c0: s_.dma_start(out=y_d[r0:r0 + 128, c0:c0 + 512], in_=ys_[:]),
                         reads=[ys_.s], dma=sem)
        P.flush("ph6")

    if upto >= 7:
      with ExitStack() as st:
        fnb = bcast_load(st, "fnb", fnorm_w[0:1, :], D)
        x1t = [sb(st, "x1u%d" % i, [128, D], F32) for i in range(2)]
        y1t = [sb(st, "y1t%d" % i, [128, D], BF16) for i in range(2)]
        y2t = [sb(st, "y2t%d" % i, [128, D], BF16) for i in range(2)]
        ot = [sb(st, "ot%d" % i, [128, D], F32) for i in range(2)]
        junk7 = sb(st, "junk7", [128, D], BF16)
        ss7, sd7, rs7 = (sb(st, n, [128, 1], F32) for n in ("ss7", "sd7", "rs7"))
        ldx = [P.sem("ld_x1u%d" % i) for i in range(2)]
        ldy1 = [P.sem("ld_y1t%d" % i) for i in range(2)]
        ldy2 = [P.sem("ld_y2t%d" % i) for i in range(2)]
        sto7 = [P.sem("st_ot%d" % i) for i in range(2)]

        def load7(tt):
            i = tt % 2
            P.op('sync', lambda e: e.dma_start(out=x1t[i][:], in_=x1_d[tt * 128:(tt + 1) * 128, :]), writes=[x1t[i].s], dma=ldx[i])
            for k, yt, sem in ((0, y1t[i], ldy1[i]), (1, y2t[i], ldy2[i])):
                it = idx_t[k][tt]
                P.op('gpsimd', lambda g, yt=yt, it=it: g.indirect_dma_start(
                    out=yt[:, :], out_offset=None, in_=y_d, in_offset=bass.IndirectOffsetOnAxis(ap=it[:, :], axis=0)), reads=[it.s], writes=[yt.s], dma=sem)
        load7(0)
        for tt in range(16):
            if tt + 1 < 16:
                load7(tt + 1)
            i = tt % 2
            xt, ya, yb, o = x1t[i], y1t[i], y2t[i], ot[i]
            P.op('vector', lambda v, xt=xt, ya=ya, tt=tt: v.scalar_tensor_tensor(
                out=xt[:], in0=ya[:], scalar=gate_t[:, 0, tt:tt + 1], in1=xt[:], op0=ALU.mult, op1=ALU.add),
                reads=[xt.s, ya.s] + gate_t.all(), writes=[xt.s])
            P.op('vector', lambda v, xt=xt, yb=yb, tt=tt: v.scalar_tensor_tensor(
                out=xt[:], in0=yb[:], scalar=gate_t[:, 1, tt:tt + 1], in1=xt[:], op0=ALU.mult, op1=ALU.add),
                reads=[xt.s, yb.s] + gate_t.all(), writes=[xt.s])
            rmsnorm_tile(xt, fnb, o, junk7, ss7, sd7, rs7, D)
            P.op('sync', lambda e, o=o, tt=tt: e.dma_start(out=out_d[tt * 128:(tt + 1) * 128, :], in_=o[:]), reads=[o.s],
                 dma=sto7[i])
        P.flush("ph7")
    else:
        if P.q['vector'] or P.q['sync'] or P.q['tensor']:
            P.flush("tail")
    return nc, top


def _t5_bucket_np(rel):
    half, max_exact = 16, 8
    n = np.abs(rel)
    large = max_exact + (np.log(np.maximum(n, 1).astype(np.float32) / max_exact)
                         / math.log(128 / max_exact) * (half - max_exact)).astype(np.int32)
    large = np.minimum(large, half - 1)
    return np.where(rel > 0, half, 0) + np.where(n < max_exact, n, large)


def _band_index(rev):
    r = np.arange(-1, 5)[:, None, None]
    k = np.arange(128)[None, :, None]
    q = np.arange(512)[None, None, :]
    rel = r * 128 + k - q
    if rev:
        rel = -rel
    return _t5_bucket_np(rel.astype(np.int32))


_NC_CACHE = {}


def kernel(x, rel_bias, norm1_w, w_in, lambda_q1, lambda_k1, lambda_q2, lambda_k2, subln_w, conv_w, conv_b,
           dt_bias_f, dt_bias_b, a_log_f, a_log_b, d_skip, ssm_norm_w, w_out, norm2_w, w_group_router,
           b_group_router, w_expert_router, b_expert_router, w_gate, w_up, w_down, final_norm_w, _upto=7, _debug=False,
           _cores=8):
    f = np.float32
    x = np.asarray(x, f)
    key = (_upto, _debug)
    if key not in _NC_CACHE:
        _NC_CACHE[key] = build_nc(_upto, _debug)
    nc = _NC_CACHE[key][0]
    w_in0 = np.asarray(w_in, f)[0]
    w_main = np.ascontiguousarray(w_in0[:, :12288])
    w_dt_n = np.ascontiguousarray(w_in0[:, 12288:12352])
    w_dt_r = np.ascontiguousarray(np.concatenate([w_in0[:, 12320:12352], w_in0[:, 12288:12320]], axis=1))
    rb = np.asarray(rel_bias, f)
    shared = {
        "w_main": w_main,
        "norm1_w": np.asarray(norm1_w, f).reshape(1, D),
        "lamv": np.stack([np.asarray(v, f)[0] for v in (lambda_q1, lambda_k1, lambda_q2, lambda_k2)]),
        "subln_w": np.asarray(subln_w, f).reshape(1, 256),
        "convb": np.ascontiguousarray(np.asarray(conv_b, f)[0].reshape(32, 128).T),
        "d_skip": np.asarray(d_skip, f).reshape(1, 32),
        "ssm_norm_w": np.asarray(ssm_norm_w, f).reshape(1, 2048),
    }
    if _upto >= 4:
        shared["w_out"] = np.ascontiguousarray(np.asarray(w_out, f)[0])
    if _upto >= 5:
        shared["norm2_w"] = np.asarray(norm2_w, f).reshape(1, D)
        shared["w_rt"] = np.ascontiguousarray(np.concatenate([np.asarray(w_group_router, f)[0],
                                                              np.asarray(w_expert_router, f)[0]], axis=1))
        shared["b_rt"] = np.concatenate([np.asarray(b_group_router, f)[0], np.asarray(b_expert_router, f)[0]]).reshape(1, 36)
    if _upto >= 6:
        shared["w_gate"] = np.ascontiguousarray(np.asarray(w_gate, f)[0])
        shared["w_up"] = np.ascontiguousarray(np.asarray(w_up, f)[0])
        shared["w_down"] = np.ascontiguousarray(np.asarray(w_down, f)[0])
    if _upto >= 7:
        shared["final_norm_w"] = np.asarray(final_norm_w, f).reshape(1, D)
    cw = np.asarray(conv_w, f)[0]
    per_kind = []
    for rev in (0, 1):
        bidx = _band_index(rev)
        band = np.ascontiguousarray(np.transpose(rb[bidx], (3, 0, 1, 2)))
        far = (rb[15], rb[31]) if not rev else (rb[31], rb[15])
        cfar = np.stack([far[0], far[1]], axis=1).reshape(1, 16)
        cfar = np.ascontiguousarray(np.broadcast_to(cfar, (128, 16)))
        cwk = cw[::-1] if rev else cw
        convw = np.ascontiguousarray(np.transpose(cwk.reshape(5, 32, 128), (2, 1, 0)))
        if not rev:
            dtb = np.concatenate([np.asarray(dt_bias_f, f)[0], np.asarray(dt_bias_b, f)[0]])
            al = np.concatenate([np.asarray(a_log_f, f)[0], np.asarray(a_log_b, f)[0]])
        else:
            dtb = np.concatenate([np.asarray(dt_bias_b, f)[0], np.asarray(dt_bias_f, f)[0]])
            al = np.concatenate([np.asarray(a_log_b, f)[0], np.asarray(a_log_f, f)[0]])
        per_kind.append({"band": band, "cfar": cfar, "convw": convw, "dt_bias": dtb.reshape(1, 64),
                         "a_log": al.reshape(1, 64), "w_dt": w_dt_r if rev else w_dt_n})
    in_maps = []
    for core in range(_cores):
        b, half = core // 2, core % 2
        xs = x[b] if half == 0 else x[b, ::-1]
        m = dict(shared)
        m.update(per_kind[half])
        m["xs"] = np.ascontiguousarray(xs)
        in_maps.append(m)
    res = run_bass_kernel_spmd(nc, in_maps, core_ids=list(range(_cores)))
    if _debug or _upto < 7:
        return res
    out = np.empty((4, 4096, D), f)
    for core in range(_cores):
        b, half = core // 2, core % 2
        o = np.asarray(res.results[core]["out"])
        if half == 0:
            out[b, :TO] = o
        else:
            out[b, TO:] = o[::-1]
    return out
```
